# Optimizing a Trainium2 kernel written in Bass

```python
import math
import jax, jax.numpy as jnp
from jax import lax
import numpy as np

D_MODEL = 2048
BATCH = 4
SEQ = 4096
DEPTH = 2

EPS = 1e-6
GLA_HEADS = 4
GLA_DV = D_MODEL // 2 // GLA_HEADS
GLA_DK = GLA_DV // 2
GLA_QK = GLA_HEADS * GLA_DK
GLA_V = GLA_HEADS * GLA_DV
GLA_RANK = 16
GLA_GATE_NORM = 16.0
GLA_CHUNK = 64
GMLP_HEADS = 8
GMLP_WIDTH = D_MODEL // 2
GMLP_DH = GMLP_WIDTH // GMLP_HEADS
GMLP_CHUNK = 128
IN_COLS = 2 * GLA_QK + 2 * GLA_V + GLA_RANK + 2 * GMLP_WIDTH
N_KEYS = 128
N_EXPERTS = N_KEYS * N_KEYS
PEER_HEADS = 8
PEER_TOPK = 16
PEER_DQ = 256
PEER_BLOCK = 64

kernel_name = "hybrid_gla_gmlp_peer_adaln"


def rmsnorm(x, g):
    xf = x.astype(jnp.float32)
    y = xf * lax.rsqrt(jnp.mean(xf * xf, axis=-1, keepdims=True) + EPS)
    return (y * g.astype(jnp.float32)).astype(x.dtype)


def gla_mixer(q, k, v, r, a_low, w_a2, b_a, norm_g):
    B, S, _ = q.shape
    n = S // GLA_CHUNK
    f32 = jnp.float32
    qh = q.reshape(B, S, GLA_HEADS, GLA_DK).astype(f32) * (GLA_DK ** -0.5)
    kh = k.reshape(B, S, GLA_HEADS, GLA_DK).astype(f32)
    vh = v.reshape(B, S, GLA_HEADS, GLA_DV).astype(f32)
    z = (a_low @ w_a2 + b_a).astype(f32)
    log_a = (jax.nn.log_sigmoid(z) / GLA_GATE_NORM).reshape(B, S, GLA_HEADS, GLA_DK)

    def to_chunks(t):
        return t.reshape(B, n, GLA_CHUNK, GLA_HEADS, t.shape[-1]).transpose(1, 0, 3, 2, 4)

    qc, kc, vc, lac = to_chunks(qh), to_chunks(kh), to_chunks(vh), to_chunks(log_a)
    G = jnp.cumsum(lac, axis=3)
    causal = jnp.tril(jnp.ones((GLA_CHUNK, GLA_CHUNK), dtype=bool))

    def step(state, inp):
        qi, ki, vi, Gi = inp
        diff = Gi[:, :, :, None, :] - Gi[:, :, None, :, :]
        decay = jnp.exp(jnp.where(causal[:, :, None], diff, -jnp.inf))
        attn = jnp.einsum('bhid,bhjd,bhijd->bhij', qi, ki, decay)
        o = attn @ vi + jnp.einsum('bhid,bhde->bhie', qi * jnp.exp(Gi), state)
        g_last = Gi[:, :, -1, :]
        k_dec = ki * jnp.exp(g_last[:, :, None, :] - Gi)
        state = jnp.exp(g_last)[..., None] * state + jnp.einsum('bhjd,bhje->bhde', k_dec, vi)
        return state, o

    state0 = jnp.zeros((B, GLA_HEADS, GLA_DK, GLA_DV), f32)
    _, o = lax.scan(step, state0, (qc, kc, vc, G))
    o = o.transpose(1, 0, 3, 2, 4).reshape(B, S, GLA_HEADS, GLA_DV)
    o = rmsnorm(o, norm_g)
    out = o.reshape(B, S, GLA_V) * jax.nn.silu(r.astype(f32))
    return out.astype(r.dtype)


def gmlp_mixer(u, v, vnorm_g, ws, b, out_g):
    B, S, _ = u.shape
    n = S // GMLP_CHUNK
    u = jax.nn.gelu(u).reshape(B, S, GMLP_HEADS, GMLP_DH)
    v = rmsnorm(jax.nn.gelu(v).reshape(B, S, GMLP_HEADS, GMLP_DH), vnorm_g)
    v = v.reshape(B, n, GMLP_CHUNK, GMLP_HEADS, GMLP_DH)
    w = ws * jnp.tril(jnp.ones((GMLP_CHUNK, GMLP_CHUNK), ws.dtype))
    sv = jnp.einsum('hts,bnshd->bnthd', w, v) + b.T[:, :, None]
    y = u * sv.reshape(B, S, GMLP_HEADS, GMLP_DH)
    y = rmsnorm(y, out_g)
    return y.reshape(B, S, GMLP_WIDTH)


def peer(h, wq, k1, k2, pu, pv):
    B, S, D = h.shape
    T = B * S
    hf = h.reshape(T, D)
    q = (hf @ wq).reshape(T, PEER_HEADS, PEER_DQ).astype(jnp.float32)
    half = PEER_DQ // 2
    s1 = jnp.einsum('thd,hkd->thk', q[..., :half], k1.astype(jnp.float32))
    s2 = jnp.einsum('thd,hkd->thk', q[..., half:], k2.astype(jnp.float32))
    v1, i1 = lax.top_k(s1, PEER_TOPK)
    v2, i2 = lax.top_k(s2, PEER_TOPK)
    cand = (v1[..., :, None] + v2[..., None, :]).reshape(T, PEER_HEADS, PEER_TOPK * PEER_TOPK)
    sc, ci = lax.top_k(cand, PEER_TOPK)
    e = (jnp.take_along_axis(i1, ci // PEER_TOPK, axis=-1) * N_KEYS
         + jnp.take_along_axis(i2, ci % PEER_TOPK, axis=-1))
    g = jax.nn.softmax(sc, axis=-1).astype(h.dtype)
    nb = T // PEER_BLOCK

    def block(args):
        hb, eb, gb = args
        ue = pu[eb]
        a = jax.nn.gelu(jnp.einsum('td,thkd->thk', hb, ue)) * gb
        return jnp.einsum('thk,thkd->td', a, pv[eb])

    y = lax.map(block, (hf.reshape(nb, PEER_BLOCK, D),
                        e.reshape(nb, PEER_BLOCK, PEER_HEADS, PEER_TOPK),
                        g.reshape(nb, PEER_BLOCK, PEER_HEADS, PEER_TOPK)))
    return y.reshape(B, S, D)


def setup_inputs(seed: int = 0) -> dict:
    key = jax.random.key(seed)
    ks = jax.random.split(key, 24)
    D = D_MODEL
    nrm = jax.random.normal
    f32 = jnp.float32
    return {
        "x": nrm(ks[0], (BATCH, SEQ, D), f32),
        "c": nrm(ks[1], (BATCH, D), f32),
        "ada_w": nrm(ks[2], (DEPTH, D, 6 * D), f32) * (0.5 * D ** -0.5),
        "ada_b": nrm(ks[3], (DEPTH, 6 * D), f32) * 0.01,
        "norm1_g": 1.0 + 0.01 * nrm(ks[4], (DEPTH, D), f32),
        "w_in": nrm(ks[5], (DEPTH, D, IN_COLS), f32) * D ** -0.5,
        "gla_w_a2": nrm(ks[6], (DEPTH, GLA_RANK, GLA_QK), f32) * GLA_RANK ** -0.5,
        "gla_b_a": nrm(ks[7], (DEPTH, GLA_QK), f32) * 0.1,
        "gla_norm_g": 1.0 + 0.01 * nrm(ks[8], (DEPTH, GLA_HEADS, GLA_DV), f32),
        "gmlp_vnorm_g": 1.0 + 0.01 * nrm(ks[9], (DEPTH, GMLP_HEADS, GMLP_DH), f32),
        "gmlp_ws": nrm(ks[10], (DEPTH, GMLP_HEADS, GMLP_CHUNK, GMLP_CHUNK), f32) * GMLP_CHUNK ** -0.5,
        "gmlp_b": 1.0 + 0.1 * nrm(ks[11], (DEPTH, GMLP_HEADS, GMLP_CHUNK), f32),
        "gmlp_out_g": 1.0 + 0.01 * nrm(ks[12], (DEPTH, GMLP_HEADS, GMLP_DH), f32),
        "w_out": nrm(ks[13], (DEPTH, D, D), f32) * D ** -0.5,
        "norm2_g": 1.0 + 0.01 * nrm(ks[14], (DEPTH, D), f32),
        "peer_wq": nrm(ks[15], (DEPTH, D, PEER_HEADS * PEER_DQ), f32) * D ** -0.5,
        "peer_k1": nrm(ks[16], (DEPTH, PEER_HEADS, N_KEYS, PEER_DQ // 2), f32) * (PEER_DQ // 2) ** -0.5,
        "peer_k2": nrm(ks[17], (DEPTH, PEER_HEADS, N_KEYS, PEER_DQ // 2), f32) * (PEER_DQ // 2) ** -0.5,
        "peer_u": nrm(ks[18], (DEPTH, N_EXPERTS, D), f32) * D ** -0.5,
        "peer_v": nrm(ks[19], (DEPTH, N_EXPERTS, D), f32) * PEER_HEADS ** -0.5,
        "final_g": 1.0 + 0.01 * nrm(ks[20], (D,), f32),
    }


def reference(x, c, ada_w, ada_b, norm1_g, w_in, gla_w_a2, gla_b_a, gla_norm_g,
              gmlp_vnorm_g, gmlp_ws, gmlp_b, gmlp_out_g, w_out, norm2_g,
              peer_wq, peer_k1, peer_k2, peer_u, peer_v, final_g):
    widths = [GLA_QK, GLA_QK, GLA_V, GLA_V, GLA_RANK, GMLP_WIDTH]
    splits = [int(s) for s in np.cumsum(widths)]
    cond = jax.nn.silu(c)
    for l in range(DEPTH):
        mod = (cond @ ada_w[l] + ada_b[l])[:, None, :]
        sh1, sc1, gt1, sh2, sc2, gt2 = jnp.split(mod, 6, axis=-1)
        h = rmsnorm(x, norm1_g[l]) * (1 + sc1) + sh1
        proj = h @ w_in[l]
        q, k, v, r, a_low, u_sp, v_sp = jnp.split(proj, splits, axis=-1)
        y_gla = gla_mixer(q, k, v, r, a_low, gla_w_a2[l], gla_b_a[l], gla_norm_g[l])
        y_gmlp = gmlp_mixer(u_sp, v_sp, gmlp_vnorm_g[l], gmlp_ws[l], gmlp_b[l], gmlp_out_g[l])
        mix = jnp.concatenate([y_gla, y_gmlp], axis=-1) @ w_out[l]
        x = x + gt1 * mix
        h = rmsnorm(x, norm2_g[l]) * (1 + sc2) + sh2
        x = x + gt2 * peer(h, peer_wq[l], peer_k1[l], peer_k2[l], peer_u[l], peer_v[l])
    return rmsnorm(x, final_g)
```

```python
import numpy as np
from contextlib import ExitStack
import concourse.bass as bass
import concourse.mybir as mybir
from concourse.bass_utils import run_bass_kernel_spmd

F32 = mybir.dt.float32
BF16 = mybir.dt.bfloat16
AF = mybir.ActivationFunctionType
ALU = mybir.AluOpType
AX = mybir.AxisListType

D = 2048
DC = 16
SEQ = 4096
BATCH = 4
DEPTH = 2
IN_COLS = 5136
NEXP = 16384
EPS = 1e-6
C_Q, C_K, C_V, C_R, C_AL, C_U, C_VS = 0, 512, 1024, 2048, 3072, 3088, 4112
NEG = -1.0e30


class Buf:
    __slots__ = ("lw", "rd")

    def __init__(self):
        self.lw = None
        self.rd = {}


class X:
    def __init__(self, t):
        self.a = t
        self.b = Buf()


def _b(x):
    return x.b if isinstance(x, X) else x


class Prog:
    ENGS = ("sp", "act", "dve", "pool", "pe")
    NDMA = 12

    def __init__(self, nc, self_wait=True):
        self.nc = nc
        self.h = {"sp": nc.sync, "act": nc.scalar, "dve": nc.vector,
                  "pool": nc.gpsimd, "pe": nc.tensor}
        self.ops = {e: [] for e in self.ENGS}
        self.cnt = {e: 0 for e in self.ENGS}
        self.waited = {e: {} for e in self.ENGS}
        self.dma_next = {e: 0 for e in self.ENGS}
        self.dma_val = {}
        self.self_wait = self_wait
        self.sems = {}

    def op(self, eng, fn, reads=(), writes=(), dma=False):
        waits = {}

        def need(kv):
            if kv is None:
                return
            k, v = kv
            if v > 0 and waits.get(k, 0) < v:
                waits[k] = v
        reads = [_b(x) for x in reads]
        writes = [_b(x) for x in writes]
        for b in reads:
            need(b.lw)
        for b in writes:
            need(b.lw)
            for k, v in b.rd.items():
                need((k, v))
        if dma:
            slot = self.dma_next[eng]
            self.dma_next[eng] = (slot + 1) % self.NDMA
            key = ("dma", eng, slot)
            prev = self.dma_val.get(key, 0)
            need((key, prev))
            myval = prev + 16
            self.dma_val[key] = myval
        else:
            key = ("eng", eng)
            self.cnt[eng] += 1
            myval = self.cnt[eng]
        fw = []
        for k, v in waits.items():
            if k == ("eng", eng) and (eng == "pe" or not self.self_wait):
                continue
            if self.waited[eng].get(k, 0) >= v:
                continue
            self.waited[eng][k] = v
            fw.append((k, v))
        self.ops[eng].append((fw, fn, key, 16 if dma else 1))
        for b in reads:
            if b.rd.get(key, 0) < myval:
                b.rd[key] = myval
        for b in writes:
            b.lw = (key, myval)
            b.rd = {}

    def fence(self):
        for e in self.ENGS:
            fw = []
            for key, v in self.dma_val.items():
                if self.waited[e].get(key, 0) < v:
                    self.waited[e][key] = v
                    fw.append((key, v))
            for e2 in self.ENGS:
                k = ("eng", e2)
                if self.cnt[e2] > 0 and self.waited[e].get(k, 0) < self.cnt[e2]:
                    self.waited[e][k] = self.cnt[e2]
                    fw.append((k, self.cnt[e2]))
            if fw:
                self.ops[e].append((fw, None, None, 0))

    def finish(self, eng="sp"):
        fw = []
        for key, v in self.dma_val.items():
            if self.waited[eng].get(key, 0) < v:
                fw.append((key, v))
        for e in self.ENGS:
            if e != eng and self.cnt[e] > 0:
                fw.append((("eng", e), self.cnt[e]))
        self.ops[eng].append((fw, None, None, 0))

    def emit(self):
        nc = self.nc
        keys = set()
        for e in self.ENGS:
            for fw, fn, key, inc in self.ops[e]:
                if key is not None:
                    keys.add(key)
                for k, v in fw:
                    keys.add(k)
        with ExitStack() as st:
            for k in sorted(keys):
                self.sems[k] = st.enter_context(
                    nc.semaphore("s_" + "_".join(str(x) for x in k)))
            block = st.enter_context(nc.Block())

            def run(e):
                h = self.h[e]
                for fw, fn, key, inc in self.ops[e]:
                    for k, v in fw:
                        h.wait_ge(self.sems[k], v)
                    if fn is not None:
                        fn().then_inc(self.sems[key], inc)

            @block.sync
            def _(x):
                run("sp")

            @block.scalar
            def _(x):
                run("act")

            @block.vector
            def _(x):
                run("dve")

            @block.gpsimd
            def _(x):
                run("pool")

            @block.tensor
            def _(x):
                run("pe")


class Arena:
    def __init__(self, t, nwords):
        self.t = t
        self.n = nwords
        self.off = 0

    def reset(self):
        self.off = 0

    def take(self, shape, dt=F32, parts=128):
        n = 1
        for s in shape[1:]:
            n *= s
        four = dt in (F32, mybir.dt.uint32, mybir.dt.int32)
        words = n if four else (n + 1) // 2
        words = (words + 15) // 16 * 16
        assert self.off + words <= self.n, ("arena overflow", self.off, words, self.n)
        ap = self.t[0:shape[0], self.off:self.off + words]
        self.off += words
        if dt != F32:
            ap = ap.bitcast(dt)
        ap = ap[:, 0:n]
        if len(shape) == 3:
            ap = ap.rearrange("p (a b) -> p a b", b=shape[2])
        elif len(shape) == 4:
            ap = ap.rearrange("p (a b c) -> p a b c", b=shape[2], c=shape[3])
        return X(ap)


def build(NT=32, NL=DEPTH, stop_after=None):
    NTOK = NT * 128
    assert NT % 4 == 0
    nc = bass.Bass("TRN2", target_bir_lowering=False)
    P = Prog(nc)

    def din(name, shape):
        return nc.dram_tensor(name, shape, F32, kind="ExternalInput")

    x_d = din("x", [NTOK, D])
    c_d = din("c", [16, 128])
    adaw_d = din("ada_w", [DEPTH, D, 6 * D])
    adab_d = din("ada_b", [DEPTH, 96, 128])
    n1g_d = din("norm1_g", [DEPTH, 16, 128])
    win_d = din("w_in", [DEPTH, D, IN_COLS])
    wa2_d = din("gla_w_a2", [DEPTH, 16, 512])
    ba_d = din("gla_b_a", [DEPTH, 1, 512])
    gng_d = din("gla_norm_g", [DEPTH, 1, 1024])
    vng_d = din("gmlp_vnorm_g", [DEPTH, 1, 1024])
    ws_d = din("gmlp_ws", [DEPTH, 8, 128, 128])
    gb_d = din("gmlp_b", [DEPTH, 8, 128])
    og_d = din("gmlp_out_g", [DEPTH, 1, 1024])
    wout_d = din("w_out", [DEPTH, D, D])
    n2g_d = din("norm2_g", [DEPTH, 16, 128])
    wq_d = din("peer_wq", [DEPTH, D, D])
    k1_d = din("peer_k1", [DEPTH, 8, 128, 128])
    k2_d = din("peer_k2", [DEPTH, 8, 128, 128])
    pu_d = din("peer_u", [DEPTH, NEXP, D])
    pv_d = din("peer_v", [DEPTH, NEXP, D])
    fg_d = din("final_g", [1, D])
    cst_d = din("consts", [128, 5, 128])
    out_d = nc.dram_tensor("out", [NTOK, D], F32, kind="ExternalOutput")

    def dscr(name, shape, dt=F32):
        return nc.dram_tensor(name, shape, dt, kind="Internal")

    xa_d = dscr("xa", [NTOK, D])
    xb_d = dscr("xb", [NTOK, D])
    modrow_d = dscr("modrow", [DEPTH, 32, 128])
    s2_d = dscr("s2s", [NTOK, 1024])
    ut_d = dscr("uts", [128, 128, DC * 128], BF16)
    vb_d = dscr("vbs", [128, 128, D], BF16)

    db = {}

    def dbuf(*key):
        if key not in db:
            db[key] = Buf()
        return db[key]

    st = ExitStack()
    with st:
        def SB(name, shape, dt=F32):
            return X(st.enter_context(nc.sbuf_tensor(name, shape, dt)))

        def DMA(q, out, in_, r=(), w=()):
            h = P.h[q]
            P.op(q, lambda: h.dma_start(out=out, in_=in_), reads=r, writes=w, dma=True)

        def MM(out, lhsT, rhs, start=True, stop=True, r=(), w=()):
            P.op("pe", lambda: nc.tensor.matmul(out, lhsT=lhsT, rhs=rhs, start=start, stop=stop),
                 reads=r, writes=w)

        def TR(out, in_, ident, r=(), w=()):
            P.op("pe", lambda: nc.tensor.transpose(out=out, in_=in_, identity=ident),
                 reads=r, writes=w)

        def ACT(out, in_, func, bias=None, scale=None, accum=None, r=(), w=()):
            kw = {}
            if bias is not None:
                kw["bias"] = bias
            if scale is not None:
                kw["scale"] = scale
            if accum is not None:
                kw["accum_out"] = accum
            P.op("act", lambda: nc.scalar.activation(out=out, in_=in_, func=func, **kw),
                 reads=r, writes=w)

        def TS(e, out, in0, s1, s2, op0, op1=None, r=(), w=()):
            h = P.h[e]
            if op1 is None:
                P.op(e, lambda: h.tensor_scalar(out=out, in0=in0, scalar1=s1, scalar2=None, op0=op0),
                     reads=r, writes=w)
            else:
                P.op(e, lambda: h.tensor_scalar(out=out, in0=in0, scalar1=s1, scalar2=s2, op0=op0, op1=op1),
                     reads=r, writes=w)

        def TT(e, out, in0, in1, op, r=(), w=()):
            h = P.h[e]
            P.op(e, lambda: h.tensor_tensor(out=out, in0=in0, in1=in1, op=op), reads=r, writes=w)

        def STT(out, in0, scalar, in1, op0, op1, r=(), w=()):
            P.op("dve", lambda: nc.vector.scalar_tensor_tensor(out=out, in0=in0, scalar=scalar, in1=in1,
                                                               op0=op0, op1=op1), reads=r, writes=w)

        def CP(e, out, in_, r=(), w=()):
            h = P.h[e]
            if e == "act":
                P.op(e, lambda: nc.scalar.copy(out=out, in_=in_), reads=r, writes=w)
            else:
                P.op(e, lambda: h.tensor_copy(out=out, in_=in_), reads=r, writes=w)

        def MSET(e, out, val, w=()):
            h = P.h[e]
            P.op(e, lambda: h.memset(out, val), writes=w)

        def RED(out, in_, op, r=(), w=()):
            P.op("dve", lambda: nc.vector.tensor_reduce(out=out, in_=in_, axis=AX.X, op=op), reads=r, writes=w)

        def MAX8(out, in_, r=(), w=()):
            P.op("dve", lambda: nc.vector.max(out=out, in_=in_), reads=r, writes=w)

        def MIDX(out, mx, vals, r=(), w=()):
            P.op("dve", lambda: nc.vector.max_index(out=out, in_max=mx, in_values=vals), reads=r, writes=w)

        def MREP(out, rep, vals, r=(), w=()):
            P.op("dve", lambda: nc.vector.match_replace(out=out, in_to_replace=rep, in_values=vals,
                                                        imm_value=NEG), reads=r, writes=w)

        def RECIP(out, in_, r=(), w=()):
            P.op("dve", lambda: nc.vector.reciprocal(out=out, in_=in_), reads=r, writes=w)

        def v3(ap, b):
            return ap.rearrange("p (a b) -> p a b", b=b)

        PSB = [X(st.enter_context(nc.psum_tensor("psb%d" % i, [128, 512], F32))) for i in range(8)]

        def psbf(i):
            return PSB[i].a[:, :].bitcast(BF16)

        cst = SB("cst", [128, 5, 128])
        identb = SB("identb", [128, 128], BF16)
        epsc = SB("epsc", [128, 1])
        onec = SB("onec", [128, 1])
        smallT = SB("smallT", [128, 128])
        condT = SB("condT", [128, 16, 2])
        modT = SB("modT", [128, 96])
        AB = SB("AB", [128, 2, 16])
        GTR = SB("GTR", [128, D])
        st8 = SB("st8", [128, 64])
        st8b = SB("st8b", [128, 64])
        ARW = 48128
        AR = Arena(st.enter_context(nc.sbuf_tensor("arena", [128, ARW], F32)), ARW)

        def new_phase():
            P.fence()
            AR.reset()

        ident = cst.a[:, 0, :]
        M_G = cst.a[:, 2, :]
        M_D = cst.a[:, 3, :]
        iota = cst.a[:, 4, :]

        def rstd_of(ss, inv_n, tmp, r, w):
            ACT(tmp, ss, AF.Sqrt, bias=epsc.a[:, 0:1], scale=inv_n, r=r + [epsc], w=w)
            RECIP(tmp, tmp, r=w, w=w)

        stage = AR.take([128, 128])
        DMA("sp", cst.a[:], cst_d.ap(), w=[cst])
        CP("dve", identb.a[:], ident, r=[cst], w=[identb])
        MSET("dve", epsc.a[:], EPS, w=[epsc])
        MSET("dve", onec.a[:], 1.0, w=[onec])
        DMA("sp", stage.a[0:16, :], c_d.ap(), w=[stage])
        TR(PSB[0].a[:, 0:16], stage.a[0:16, :], cst.a[0:16, 0, 0:16], r=[stage, cst], w=[PSB[0]])
        ACT(condT.a[:, :, 0], PSB[0].a[:, 0:16], AF.Silu, r=[PSB[0]], w=[condT])
        ACT(condT.a[:, :, 1], PSB[0].a[:, 0:16], AF.Silu, r=[PSB[0]], w=[condT])

        def setup_layer(l):
            new_phase()
            stage = AR.take([128, 128])
            gtT = AR.take([128, 32])
            gtrow = AR.take([32, 128])
            aw = [AR.take([128, DC, 256]) for _ in range(2)]
            DMA("sp", stage.a[0:96, :], adab_d.ap()[l], w=[stage])
            DMA("sp", stage.a[96:112, :], n1g_d.ap()[l], w=[stage])
            DMA("sp", stage.a[112:128, :], n2g_d.ap()[l], w=[stage])
            TR(PSB[0].a[:, 0:128], stage.a[:, :], ident, r=[stage, cst], w=[PSB[0]])
            CP("dve", smallT.a[:], PSB[0].a[:, 0:128], r=[PSB[0]], w=[smallT])
            src = adaw_d.ap()[l].rearrange("(dc p) c -> p dc c", p=128)
            for cc in range(48):
                t = aw[cc % 2]
                DMA("sp" if cc % 2 == 0 else "act", t.a, src[:, :, cc * 256:(cc + 1) * 256], w=[t])
                for mm in range(2):
                    m = cc * 2 + mm
                    for dc in range(DC):
                        MM(PSB[1].a[:, 2 * m:2 * m + 2], t.a[:, dc, mm * 128:(mm + 1) * 128],
                           condT.a[:, dc, :], start=(dc == 0), stop=(dc == DC - 1),
                           r=[t, condT], w=[PSB[1]])
            TT("dve", modT.a[:], v3(PSB[1].a[:, 0:192], 2)[:, :, 0], smallT.a[:, 0:96], ALU.add,
               r=[PSB[1], smallT], w=[modT])
            STT(AB.a[:, 0, :], modT.a[:, 16:32], 1.0, smallT.a[:, 96:112], ALU.add, ALU.mult,
                r=[modT, smallT], w=[AB])
            STT(AB.a[:, 1, :], modT.a[:, 64:80], 1.0, smallT.a[:, 112:128], ALU.add, ALU.mult,
                r=[modT, smallT], w=[AB])
            CP("dve", gtT.a[:, 0:16], modT.a[:, 32:48], r=[modT], w=[gtT])
            CP("dve", gtT.a[:, 16:32], modT.a[:, 80:96], r=[modT], w=[gtT])
            TR(PSB[0].a[0:32, 0:128], gtT.a[:, :], ident, r=[gtT, cst], w=[PSB[0]])
            CP("dve", gtrow.a[:, :], PSB[0].a[0:32, 0:128], r=[PSB[0]], w=[gtrow])
            DMA("sp", modrow_d.ap()[l], gtrow.a[:, :], r=[gtrow], w=[dbuf("modrow", l)])

        def tr8(srcd_ap, dst, masked, kin):
            DMA("sp", kin.a, srcd_ap, w=[kin])
            for h in range(8):
                bk = PSB[2 + h // 4]
                TR(bk.a[:, (h % 4) * 128:(h % 4 + 1) * 128], kin.a[:, h, :], ident, r=[kin, cst], w=[bk])
            for hb in range(2):
                bk = PSB[2 + hb]
                if masked:
                    TT("dve", dst.a[:, hb * 4:(hb + 1) * 4, :], v3(bk.a[:, :], 128),
                       cst.a[:, 1:2, :].to_broadcast([128, 4, 128]), ALU.mult, r=[bk, cst], w=[dst])
                else:
                    CP("dve", dst.a[:, hb * 4:(hb + 1) * 4, :], v3(bk.a[:, :], 128), r=[bk], w=[dst])

        def norm_T(x_ap, xdep, which, bshift, dst, dst_fn, xnb, junk):
            ACT(junk.a[:, :], x_ap, AF.Square, accum=st8.a[:, 0:1], r=[xdep], w=[junk, st8])
            rstd_of(st8.a[:, 0:1], 1.0 / D, st8.a[:, 1:2], r=[st8], w=[st8])
            TS("dve", xnb.a[:, :], x_ap, st8.a[:, 1:2], None, ALU.mult, r=[xdep, st8], w=[xnb])
            for dc in range(DC):
                bk = PSB[dc // 8]
                TR(psbf(dc // 8)[:, (dc % 8) * 128:(dc % 8 + 1) * 128], xnb.a[:, dc * 128:(dc + 1) * 128],
                   identb.a[:], r=[xnb, identb], w=[bk])
            for dc in range(DC):
                bk = PSB[dc // 8]
                src = psbf(dc // 8)[:, (dc % 8) * 128:(dc % 8 + 1) * 128]
                if dc % 2 == 0:
                    TS("dve", dst_fn(dc), src, AB.a[:, which, dc:dc + 1],
                       modT.a[:, bshift + dc:bshift + dc + 1], ALU.mult, ALU.add,
                       r=[bk, AB, modT], w=[dst])
                else:
                    ACT(dst_fn(dc), src, AF.Identity, bias=modT.a[:, bshift + dc:bshift + dc + 1],
                        scale=AB.a[:, which, dc:dc + 1], r=[bk, AB, modT], w=[dst])

        def mixer_layer(l, xsrc, xdst):
            new_phase()
            ngr = AR.take([128, 1024])
            vgr = AR.take([128, 1024])
            ogr = AR.take([128, 1024])
            W2a = AR.take([17, 512])
            wsT = AR.take([128, 8, 128], BF16)
            bT = AR.take([128, 8])
            S32 = AR.take([128, 4, 256])
            Sbf = AR.take([128, 4, 256], BF16)
            xt = [AR.take([128, D]) for _ in range(2)]
            xnb = AR.take([128, D], BF16)
            junk = AR.take([128, D], BF16)
            wc = [AR.take([128, DC, 512], BF16) for _ in range(2)]
            hTg = AR.take([128, DC, 512], BF16)
            ymT = hTg
            alT = AR.take([17, 512])
            GTg = AR.take([128, 4, 4, 128])
            EGl = AR.take([128, 4, 4])
            EDg = AR.take([128, 4, 512], BF16)
            qtg = AR.take([128, 4, 512], BF16)
            ktg = AR.take([128, 4, 512], BF16)
            kdg = AR.take([128, 4, 512], BF16)
            vg_ = AR.take([128, 4, 1024], BF16)
            srg = AR.take([128, 4, 1024], BF16)
            gug = AR.take([128, 4, 1024], BF16)
            gvg = AR.take([128, 4, 1024], BF16)
            lt = AR.take([128, 512])
            et = AR.take([128, 512])
            attm = AR.take([128, 4, 128], BF16)
            ymix = AR.take([128, D], BF16)
            f1 = AR.take([128, 1024])
            f2 = AR.take([128, 1024])
            vnb = AR.take([128, 1024], BF16)
            xp = [AR.take([128, 512]) for _ in range(2)]
            xo = [AR.take([128, 512]) for _ in range(2)]
            stage = AR.take([8, 128])
            kin = X(f1.a[:, :].rearrange("p (a b) -> p a b", b=128))
            kin.b = f1.b

            DMA("sp", GTR.a[:], modrow_d.ap()[l, 0:16, :].rearrange("a b -> (a b)").unsqueeze(0).to_broadcast([128, D]),
                r=[dbuf("modrow", l)], w=[GTR])
            DMA("sp", W2a.a[0:16, :], wa2_d.ap()[l], w=[W2a])
            DMA("sp", W2a.a[16:17, :], ba_d.ap()[l], w=[W2a])
            DMA("sp", ngr.a[:, :], gng_d.ap()[l].to_broadcast([128, 1024]), w=[ngr])
            DMA("sp", vgr.a[:, :], vng_d.ap()[l].to_broadcast([128, 1024]), w=[vgr])
            DMA("sp", ogr.a[:, :], og_d.ap()[l].to_broadcast([128, 1024]), w=[ogr])
            tr8(ws_d.ap()[l].rearrange("h t s -> t h s"), wsT, True, kin)
            DMA("sp", stage.a[0:8, :], gb_d.ap()[l], w=[stage])
            TR(PSB[0].a[:, 0:8], stage.a[0:8, :], cst.a[0:8, 0, 0:8], r=[stage, cst], w=[PSB[0]])
            CP("dve", bT.a[:, :], PSB[0].a[:, 0:8], r=[PSB[0]], w=[bT])
            MSET("dve", S32.a[:], 0.0, w=[S32])
            MSET("pool", Sbf.a[:], 0.0, w=[Sbf])
            MSET("dve", alT.a[:, :], 1.0, w=[alT])

            wcnt = [0]

            def load_w(src_ap3, ncols):
                t = wc[wcnt[0] % 2]
                wcnt[0] += 1
                DMA("pool", t.a[:, :, 0:ncols], src_ap3, w=[t])
                return t

            win_v = win_d.ap()[l].rearrange("(dc p) c -> p dc c", p=128)
            wo_src = wout_d.ap()[l].rearrange("(mc p) c -> p mc c", p=128)

            def win_cols(c0, n):
                return win_v[:, :, c0:c0 + n]

            for g in range(NT // 4):
                t0 = g * 4
                DMA("sp", xt[0].a[:, :], xsrc.ap()[t0 * 128:(t0 + 1) * 128, :], r=[dbuf(xsrc.name, t0)], w=[xt[0]])
                for j in range(4):
                    if j + 1 < 4:
                        s = (j + 1) % 2
                        DMA("sp", xt[s].a[:, :], xsrc.ap()[(t0 + j + 1) * 128:(t0 + j + 2) * 128, :],
                            r=[dbuf(xsrc.name, t0 + j + 1)], w=[xt[s]])
                    norm_T(xt[j % 2].a[:, :], xt[j % 2], 0, 0, hTg,
                           lambda dc, j=j: hTg.a[:, dc, j * 128:(j + 1) * 128], xnb, junk)
                wal = load_w(win_cols(C_AL, 16), 16)
                wq_ = load_w(win_cols(C_Q, 512), 512)
                for dc in range(DC):
                    MM(PSB[2].a[0:16, :], wal.a[:, dc, 0:16], hTg.a[:, dc, :], start=(dc == 0), stop=(dc == DC - 1),
                       r=[wal, hTg], w=[PSB[2]])
                CP("dve", alT.a[0:16, :], PSB[2].a[0:16, :], r=[PSB[2]], w=[alT])
                for j in range(4):
                    tk = slice(j * 128, (j + 1) * 128)
                    MM(PSB[4].a[:, :], alT.a[:, tk], W2a.a[:, :], r=[alT, W2a], w=[PSB[4]])
                    ACT(et.a[:, :], PSB[4].a[:, :], AF.Exp, scale=-1.0, r=[PSB[4]], w=[et])
                    ACT(lt.a[:, :], et.a[:, :], AF.Ln, bias=onec.a[:, 0:1], scale=1.0, r=[et, onec], w=[lt])
                    MM(PSB[4].a[:, :], M_D, lt.a[:, :], r=[cst, lt], w=[PSB[4]])
                    ACT(EDg.a[:, j, :], PSB[4].a[:, :], AF.Exp, r=[PSB[4]], w=[EDg])
                    for h in range(4):
                        MM(PSB[5].a[:, h * 128:(h + 1) * 128], lt.a[:, h * 128:(h + 1) * 128], M_G,
                           r=[lt, cst], w=[PSB[5]])
                    CP("dve", GTg.a[:, j, :, :], v3(PSB[5].a[:, :], 128), r=[PSB[5]], w=[GTg])
                ACT(EGl.a[:, :, :], GTg.a[:, :, :, 127], AF.Exp, r=[GTg], w=[EGl])
                wk_ = load_w(win_cols(C_K, 512), 512)
                for h in range(4):
                    bk = PSB[2 + h % 2]
                    for dc in range(DC):
                        MM(bk.a[:, :], wq_.a[:, dc, h * 128:(h + 1) * 128], hTg.a[:, dc, :],
                           start=(dc == 0), stop=(dc == DC - 1), r=[wq_, hTg], w=[bk])
                    ACT(v3(et.a[:, :], 128), GTg.a[:, :, h, :], AF.Exp, r=[GTg], w=[et])
                    STT(qtg.a[:, h, :], bk.a[:, :], 128.0 ** -0.5, et.a[:, :], ALU.mult, ALU.mult,
                        r=[bk, et], w=[qtg])
                wv0 = load_w(win_cols(C_V, 512), 512)
                for h in range(4):
                    bk = PSB[2 + h % 2]
                    for dc in range(DC):
                        MM(bk.a[:, :], wk_.a[:, dc, h * 128:(h + 1) * 128], hTg.a[:, dc, :],
                           start=(dc == 0), stop=(dc == DC - 1), r=[wk_, hTg], w=[bk])
                    ACT(v3(et.a[:, :], 128), GTg.a[:, :, h, :], AF.Exp, scale=-1.0, r=[GTg], w=[et])
                    TT("dve", ktg.a[:, h, :], bk.a[:, :], et.a[:, :], ALU.mult, r=[bk, et], w=[ktg])
                for j in range(4):
                    bk = PSB[2 + j % 2]
                    for dc in range(DC):
                        MM(bk.a[:, :], hTg.a[:, dc, j * 128:(j + 1) * 128], wk_.a[:, dc, :],
                           start=(dc == 0), stop=(dc == DC - 1), r=[wk_, hTg], w=[bk])
                    TT("dve", kdg.a[:, j, :], bk.a[:, :], EDg.a[:, j, :], ALU.mult, r=[bk, EDg], w=[kdg])
                chunks = [(C_V, vg_, 0, None), (C_V + 512, vg_, 512, None),
                          (C_R, srg, 0, AF.Silu), (C_R + 512, srg, 512, AF.Silu),
                          (C_U, gug, 0, AF.Gelu_apprx_tanh), (C_U + 512, gug, 512, AF.Gelu_apprx_tanh),
                          (C_VS, gvg, 0, AF.Gelu_apprx_tanh), (C_VS + 512, gvg, 512, AF.Gelu_apprx_tanh)]
                wcur = wv0
                for ci, (c0, dstt, off, fn) in enumerate(chunks):
                    if ci + 1 < len(chunks):
                        nxt = load_w(win_cols(chunks[ci + 1][0], 512), 512)
                    else:
                        nxt = load_w(wo_src[:, :, 0:512], 512)
                    for j in range(4):
                        bk = PSB[2 + j % 2]
                        for dc in range(DC):
                            MM(bk.a[:, :], hTg.a[:, dc, j * 128:(j + 1) * 128], wcur.a[:, dc, :],
                               start=(dc == 0), stop=(dc == DC - 1), r=[wcur, hTg], w=[bk])
                        if fn is None:
                            CP("act", dstt.a[:, j, off:off + 512], bk.a[:, :], r=[bk], w=[dstt])
                        else:
                            ACT(dstt.a[:, j, off:off + 512], bk.a[:, :], fn, r=[bk], w=[dstt])
                    wcur = nxt
                wo = wcur
                for j in range(4):
                    tk = slice(j * 128, (j + 1) * 128)
                    for h in range(4):
                        MM(PSB[5].a[:, h * 128:(h + 1) * 128], ktg.a[:, h, tk], qtg.a[:, h, tk],
                           r=[ktg, qtg], w=[PSB[5]])
                    TT("dve", attm.a[:, :, :], v3(PSB[5].a[:, :], 128), cst.a[:, 1:2, :].to_broadcast([128, 4, 128]),
                       ALU.mult, r=[PSB[5], cst], w=[attm])
                    for h in range(4):
                        bk = PSB[6 + h // 2]
                        oc = slice((h % 2) * 256, (h % 2 + 1) * 256)
                        MM(bk.a[:, oc], attm.a[:, h, :], vg_.a[:, j, h * 256:(h + 1) * 256], start=True, stop=False,
                           r=[attm, vg_], w=[bk])
                        MM(bk.a[:, oc], qtg.a[:, h, tk], Sbf.a[:, h, :], start=False, stop=True,
                           r=[qtg, Sbf], w=[bk])
                    for h in range(4):
                        bk = PSB[h // 2]
                        oc = slice((h % 2) * 256, (h % 2 + 1) * 256)
                        MM(bk.a[:, oc], kdg.a[:, j, h * 128:(h + 1) * 128], vg_.a[:, j, h * 256:(h + 1) * 256],
                           r=[kdg, vg_], w=[bk])
                    for h in range(4):
                        bk = PSB[h // 2]
                        oc = slice((h % 2) * 256, (h % 2 + 1) * 256)
                        STT(S32.a[:, h, :], S32.a[:, h, :], EGl.a[:, j, h:h + 1], bk.a[:, oc], ALU.mult, ALU.add,
                            r=[S32, EGl, bk], w=[S32])
                    CP("pool", Sbf.a[:, :, :], S32.a[:, :, :], r=[S32], w=[Sbf])
                    for h in range(4):
                        bk = PSB[6 + h // 2]
                        oc = slice((h % 2) * 256, (h % 2 + 1) * 256)
                        ACT(junk.a[:, 0:256], bk.a[:, oc], AF.Square, accum=st8b.a[:, h:h + 1], r=[bk], w=[junk, st8b])
                    rstd_of(st8b.a[:, 0:4], 1.0 / 256, st8b.a[:, 4:8], r=[st8b], w=[st8b])
                    for h in range(4):
                        bk = PSB[6 + h // 2]
                        oc = slice((h % 2) * 256, (h % 2 + 1) * 256)
                        STT(f1.a[:, h * 256:(h + 1) * 256], bk.a[:, oc], st8b.a[:, 4 + h:5 + h],
                            ngr.a[:, h * 256:(h + 1) * 256], ALU.mult, ALU.mult, r=[bk, st8b, ngr], w=[f1])
                    TT("dve", ymix.a[:, 0:1024], f1.a[:, :], srg.a[:, j, :], ALU.mult, r=[f1, srg], w=[ymix])
                    TT("dve", f1.a[:, :], gvg.a[:, j, :], gvg.a[:, j, :], ALU.mult, r=[gvg], w=[f1])
                    RED(st8b.a[:, 8:16], v3(f1.a[:, :], 128), ALU.add, r=[f1], w=[st8b])
                    rstd_of(st8b.a[:, 8:16], 1.0 / 128, st8b.a[:, 16:24], r=[st8b], w=[st8b])
                    TT("dve", v3(f1.a[:, :], 128), v3(gvg.a[:, j, :], 128),
                       st8b.a[:, 16:24].unsqueeze(2).to_broadcast([128, 8, 128]), ALU.mult, r=[gvg, st8b], w=[f1])
                    TT("dve", vnb.a[:, :], f1.a[:, :], vgr.a[:, :], ALU.mult, r=[f1, vgr], w=[vnb])
                    for h in range(8):
                        bk = PSB[6 + h // 4]
                        MM(bk.a[:, (h % 4) * 128:(h % 4 + 1) * 128], wsT.a[:, h, :], vnb.a[:, h * 128:(h + 1) * 128],
                           r=[wsT, vnb], w=[bk])
                    for hb in range(2):
                        bk = PSB[6 + hb]
                        TT("dve", v3(f2.a[:, hb * 512:(hb + 1) * 512], 128), v3(bk.a[:, :], 128),
                           bT.a[:, hb * 4:(hb + 1) * 4].unsqueeze(2).to_broadcast([128, 4, 128]), ALU.add,
                           r=[bk, bT], w=[f2])
                    TT("dve", f2.a[:, :], f2.a[:, :], gug.a[:, j, :], ALU.mult, r=[f2, gug], w=[f2])
                    TT("dve", f1.a[:, :], f2.a[:, :], f2.a[:, :], ALU.mult, r=[f2], w=[f1])
                    RED(st8b.a[:, 24:32], v3(f1.a[:, :], 128), ALU.add, r=[f1], w=[st8b])
                    rstd_of(st8b.a[:, 24:32], 1.0 / 128, st8b.a[:, 32:40], r=[st8b], w=[st8b])
                    TT("dve", v3(f2.a[:, :], 128), v3(f2.a[:, :], 128),
                       st8b.a[:, 32:40].unsqueeze(2).to_broadcast([128, 8, 128]), ALU.mult, r=[f2, st8b], w=[f2])
                    TT("dve", ymix.a[:, 1024:2048], f2.a[:, :], ogr.a[:, :], ALU.mult, r=[f2, ogr], w=[ymix])
                    for mc in range(DC):
                        bk = PSB[mc // 8]
                        TR(psbf(mc // 8)[:, (mc % 8) * 128:(mc % 8 + 1) * 128], ymix.a[:, mc * 128:(mc + 1) * 128],
                           identb.a[:], r=[ymix, identb], w=[bk])
                    for hb in range(2):
                        bk = PSB[hb]
                        CP("act", ymT.a[:, hb * 8:(hb + 1) * 8, tk], v3(psbf(hb), 128), r=[bk], w=[ymT])
                for cg in range(4):
                    wnext = None
                    if cg + 1 < 4:
                        wnext = load_w(wo_src[:, :, (cg + 1) * 512:(cg + 2) * 512], 512)
                    for j in range(4):
                        tile = t0 + j
                        s = (cg * 4 + j) % 2
                        DMA("sp", xp[s].a[:, :], xsrc.ap()[tile * 128:(tile + 1) * 128, cg * 512:(cg + 1) * 512],
                            r=[dbuf(xsrc.name, tile)], w=[xp[s]])
                        bk = PSB[2 + j % 2]
                        for mc in range(DC):
                            MM(bk.a[:, :], ymT.a[:, mc, j * 128:(j + 1) * 128], wo.a[:, mc, :],
                               start=(mc == 0), stop=(mc == DC - 1), r=[ymT, wo], w=[bk])
                        TT("dve", xo[s].a[:, :], bk.a[:, :], GTR.a[:, cg * 512:(cg + 1) * 512], ALU.mult,
                           r=[bk, GTR], w=[xo[s]])
                        TT("pool", xo[s].a[:, :], xo[s].a[:, :], xp[s].a[:, :], ALU.add, r=[xo[s], xp[s]], w=[xo[s]])
                        DMA("sp", xdst.ap()[tile * 128:(tile + 1) * 128, cg * 512:(cg + 1) * 512], xo[s].a[:, :],
                            r=[xo[s]], w=[dbuf(xdst.name, tile)])
                    wo = wnext

        def peer_layer(l, xsrc, xdst):
            new_phase()
            ub = [AR.take([128, D], BF16) for _ in range(2)]
            uo = [AR.take([128, D], BF16) for _ in range(2)]
            vb_ = [AR.take([128, D], BF16) for _ in range(2)]
            for i in range(128):
                s = i % 2
                DMA("pool", ub[s].a[:, :], pu_d.ap()[l, i * 128:(i + 1) * 128, :], w=[ub[s]])
                DMA("pool", vb_[s].a[:, :], pv_d.ap()[l, i * 128:(i + 1) * 128, :], w=[vb_[s]])
                DMA("sp", vb_d.ap()[i], vb_[s].a[:, :], r=[vb_[s]], w=[dbuf("vb", i)])
                for dc in range(DC):
                    bi = 4 + dc // 8 + 2 * s
                    TR(psbf(bi)[:, (dc % 8) * 128:(dc % 8 + 1) * 128],
                       ub[s].a[:, dc * 128:(dc + 1) * 128], identb.a[:], r=[ub[s], identb], w=[PSB[bi]])
                CP("act", uo[s].a[:, 0:1024], psbf(4 + 2 * s), r=[PSB[4 + 2 * s]], w=[uo[s]])
                CP("dve", uo[s].a[:, 1024:2048], psbf(5 + 2 * s), r=[PSB[5 + 2 * s]], w=[uo[s]])
                DMA("sp", ut_d.ap()[i], uo[s].a[:, :], r=[uo[s]], w=[dbuf("ut", i)])

            new_phase()
            k1T = AR.take([128, 8, 128])
            k2T = AR.take([128, 8, 128])
            h2T = AR.take([128, DC, 256], BF16)
            sc5T = AR.take([128, 5, 256])
            GA = AR.take([128, 128, 256], BF16)
            xblk = AR.take([128, 2, D])
            base = AR.off
            kin = X(xblk.a[:, 0, 0:1024].rearrange("p (a b) -> p a b", b=128))
            kin.b = xblk.b
            tr8(k1_d.ap()[l].rearrange("h k d -> k h d"), k1T, False, kin)
            tr8(k2_d.ap()[l].rearrange("h k d -> k h d"), k2T, False, kin)
            DMA("sp", GTR.a[:], modrow_d.ap()[l, 16:32, :].rearrange("a b -> (a b)").unsqueeze(0).to_broadcast([128, D]),
                r=[dbuf("modrow", l)], w=[GTR])
            wq_src = wq_d.ap()[l].rearrange("(dc p) c -> p dc c", p=128)

            for blk in range(NT // 2):
                tb0 = blk * 2
                P.fence()
                AR.off = base
                xnb = AR.take([128, D], BF16)
                junk = AR.take([128, D], BF16)
                wcq = AR.take([128, DC, 512], BF16)
                qT = AR.take([128, 16, 256])
                ssb = [AR.take([128, 1024]) for _ in range(2)]
                v12 = AR.take([128, 2, 8, 16])
                idx = AR.take([128, 8, 16], mybir.dt.uint32)
                tmpA = AR.take([128, 256])
                cand = AR.take([128, 8, 256])
                sc = AR.take([128, 8, 24])
                tm5 = AR.take([128, 4, 128])
                sm = AR.take([128, 64])
                ex16 = AR.take([128, 8, 16])
                for ts in range(2):
                    tile = tb0 + ts
                    DMA("sp", xblk.a[:, ts, :], xsrc.ap()[tile * 128:(tile + 1) * 128, :],
                        r=[dbuf(xsrc.name, tile)], w=[xblk])
                for ts in range(2):
                    norm_T(xblk.a[:, ts, :], xblk, 1, 48, h2T,
                           lambda dc, ts=ts: h2T.a[:, dc, ts * 128:(ts + 1) * 128], xnb, junk)
                for cg in range(4):
                    DMA("pool", wcq.a[:, :, :], wq_src[:, :, cg * 512:(cg + 1) * 512], w=[wcq])
                    for cc in range(4):
                        bk = PSB[2 + cc % 2]
                        half = (cc // 2 % 2) * 256
                        for dc in range(DC):
                            MM(bk.a[:, half:half + 256], wcq.a[:, dc, cc * 128:(cc + 1) * 128], h2T.a[:, dc, :],
                               start=(dc == 0), stop=(dc == DC - 1), r=[wcq, h2T], w=[bk])
                        CP("act", qT.a[:, cg * 4 + cc, :], bk.a[:, half:half + 256], r=[bk], w=[qT])
                for ts in range(2):
                    tile = tb0 + ts
                    tk = slice(ts * 128, (ts + 1) * 128)
                    for side in range(2):
                        kT = k1T if side == 0 else k2T
                        for h in range(8):
                            bk = PSB[4 + side * 2 + h // 4]
                            MM(bk.a[:, (h % 4) * 128:(h % 4 + 1) * 128], qT.a[:, 2 * h + side, tk], kT.a[:, h, :],
                               r=[qT, kT], w=[bk])
                        for hb in range(2):
                            bk = PSB[4 + side * 2 + hb]
                            CP("act", ssb[side].a[:, hb * 512:(hb + 1) * 512], bk.a[:, :], r=[bk], w=[ssb[side]])
                        if side == 1:
                            DMA("sp", s2_d.ap()[tile * 128:(tile + 1) * 128, :], ssb[1].a[:, :],
                                r=[ssb[1]], w=[dbuf("s2", tile)])
                        for h in range(8):
                            sv = ssb[side].a[:, h * 128:(h + 1) * 128]
                            MAX8(v12.a[:, side, h, 0:8], sv, r=[ssb[side]], w=[v12])
                            if side == 0:
                                MIDX(idx.a[:, h, 0:8], v12.a[:, 0, h, 0:8], sv, r=[ssb[0], v12], w=[idx])
                            MREP(tmpA.a[:, 0:128], v12.a[:, side, h, 0:8], sv, r=[ssb[side], v12], w=[tmpA])
                            MAX8(v12.a[:, side, h, 8:16], tmpA.a[:, 0:128], r=[tmpA], w=[v12])
                            if side == 0:
                                MIDX(idx.a[:, h, 8:16], v12.a[:, 0, h, 8:16], tmpA.a[:, 0:128], r=[tmpA, v12], w=[idx])
                    TT("dve", cand.a[:, :, :].rearrange("p h (a b) -> p h a b", b=16),
                       v12.a[:, 0, :, :].unsqueeze(3).to_broadcast([128, 8, 16, 16]),
                       v12.a[:, 1, :, :].unsqueeze(2).to_broadcast([128, 8, 16, 16]), ALU.add, r=[v12], w=[cand])
                    for h in range(8):
                        MAX8(sc.a[:, h, 0:8], cand.a[:, h, :], r=[cand], w=[sc])
                        MREP(tmpA.a[:, :], sc.a[:, h, 0:8], cand.a[:, h, :], r=[cand, sc], w=[tmpA])
                        MAX8(sc.a[:, h, 8:16], tmpA.a[:, :], r=[tmpA], w=[sc])
                        MREP(tmpA.a[:, :], sc.a[:, h, 8:16], tmpA.a[:, :], r=[tmpA, sc], w=[tmpA])
                        MAX8(sc.a[:, h, 16:24], tmpA.a[:, :], r=[tmpA], w=[sc])
                    TT("dve", sm.a[:, 0:8], sc.a[:, :, 15], sc.a[:, :, 16], ALU.add, r=[sc], w=[sm])
                    TS("dve", sm.a[:, 0:8], sm.a[:, 0:8], 0.5, None, ALU.mult, r=[sm], w=[sm])
                    TT("dve", ex16.a[:, :, :], sc.a[:, :, 0:16], sc.a[:, :, 0:1].to_broadcast([128, 8, 16]),
                       ALU.subtract, r=[sc], w=[ex16])
                    ACT(ex16.a[:, :, :], ex16.a[:, :, :], AF.Exp, r=[ex16], w=[ex16])
                    RED(sm.a[:, 8:16], ex16.a[:, :, :], ALU.add, r=[ex16], w=[sm])
                    RECIP(sm.a[:, 16:24], sm.a[:, 8:16], r=[sm], w=[sm])
                    v1v = v12.a[:, 0, :, :]
                    t4 = lambda k: tm5.a[:, k, :].rearrange("p (h a) -> p h a", a=16)
                    CP("dve", t4(0), idx.a[:, :, :], r=[idx], w=[tm5])
                    TT("dve", t4(1), v1v, v12.a[:, 0, :, 0:1].to_broadcast([128, 8, 16]), ALU.subtract,
                       r=[v12], w=[tm5])
                    ACT(t4(1), t4(1), AF.Exp, r=[tm5], w=[tm5])
                    TT("dve", t4(1), t4(1), sm.a[:, 16:24].unsqueeze(2).to_broadcast([128, 8, 16]), ALU.mult,
                       r=[tm5, sm], w=[tm5])
                    TT("dve", t4(2), sm.a[:, 0:8].unsqueeze(2).to_broadcast([128, 8, 16]), v1v, ALU.subtract,
                       r=[sm, v12], w=[tm5])
                    TS("dve", t4(3), v12.a[:, 1, :, 0:1].to_broadcast([128, 8, 16]), -1.0, None, ALU.mult,
                       r=[v12], w=[tm5])
                    for k in range(4):
                        TR(PSB[0].a[:, k * 128:(k + 1) * 128], tm5.a[:, k, :], ident, r=[tm5, cst], w=[PSB[0]])
                    CP("act", sc5T.a[:, 0:4, tk], v3(PSB[0].a[:, :], 128), r=[PSB[0]], w=[sc5T])
                P.fence()
                AR.off = base
                SR = [AR.take([128, 16, 128]) for _ in range(2)]
                Pt = [AR.take([128, 128], BF16) for _ in range(2)]
                Et = [AR.take([128, 128]) for _ in range(2)]
                Rt = [AR.take([128, 128], BF16) for _ in range(2)]

                def load_sr(bi):
                    sl = bi % 2
                    tok0 = tb0 * 128 + bi * 16
                    tile = tok0 // 128
                    for h in range(8):
                        src = bass.AP(s2_d, tok0 * 1024 + h * 128, [[0, 16], [1024, 16], [1, 128]])
                        DMA("sp", SR[sl].a[h * 16:(h + 1) * 16, :, :], src, r=[dbuf("s2", tile)], w=[SR[sl]])
                load_sr(0)
                for bi in range(16):
                    if bi + 1 < 16:
                        load_sr(bi + 1)
                    sl = bi % 2
                    for tt in range(16):
                        t = bi * 16 + tt
                        k2 = t % 2
                        col = lambda k, t=t: sc5T.a[:, k, t:t + 1]
                        TS("dve", Pt[k2].a[:, :], iota, col(0), col(1), ALU.is_equal, ALU.mult,
                           r=[cst, sc5T], w=[Pt[k2]])
                        ACT(Et[k2].a[:, :], SR[sl].a[:, tt, :], AF.Exp, bias=col(3), scale=1.0,
                            r=[SR[sl], sc5T], w=[Et[k2]])
                        STT(Rt[k2].a[:, :], SR[sl].a[:, tt, :], col(2), Et[k2].a[:, :], ALU.is_ge, ALU.mult,
                            r=[SR[sl], sc5T, Et[k2]], w=[Rt[k2]])
                        bk = PSB[6 + (t // 4) % 2]
                        MM(bk.a[:, (t % 4) * 128:(t % 4 + 1) * 128], Rt[k2].a[:, :], Pt[k2].a[:, :],
                           r=[Rt[k2], Pt[k2]], w=[bk])
                        if t % 4 == 3:
                            CP("act", GA.a[:, :, t - 3:t + 1], bk.a[:, :].rearrange("p (t i) -> p i t", i=128),
                               r=[bk], w=[GA])
                P.fence()
                AR.off = base
                gz = [AR.take([128, 256], BF16) for _ in range(2)]
                utb = [AR.take([128, 2, DC * 128], BF16) for _ in range(2)]
                vvb = [AR.take([128, 2, 1024], BF16) for _ in range(2)]
                yo = [AR.take([128, 1024]) for _ in range(2)]

                def load_ut(pi):
                    DMA("sp", utb[pi % 2].a[:, :, :], ut_d.ap()[2 * pi:2 * pi + 2].rearrange("i p f -> p i f"),
                        r=[dbuf("ut", 2 * pi), dbuf("ut", 2 * pi + 1)], w=[utb[pi % 2]])
                load_ut(0)
                for i in range(128):
                    if i % 2 == 0 and i // 2 + 1 < 64:
                        load_ut(i // 2 + 1)
                    ub_ = utb[(i // 2) % 2]
                    bk = PSB[4 + (i // 2) % 2]
                    half = (i % 2) * 256
                    for dc in range(DC):
                        MM(bk.a[:, half:half + 256], ub_.a[:, i % 2, dc * 128:(dc + 1) * 128], h2T.a[:, dc, :],
                           start=(dc == 0), stop=(dc == DC - 1), r=[ub_, h2T], w=[bk])
                    ACT(gz[i % 2].a[:, :], bk.a[:, half:half + 256], AF.Gelu_apprx_tanh, r=[bk], w=[gz[i % 2]])
                    TT("dve", GA.a[:, i, :], gz[i % 2].a[:, :], GA.a[:, i, :], ALU.mult, r=[gz[i % 2], GA], w=[GA])
                for dh in range(2):
                    def load_v(pi, dh=dh):
                        DMA("sp", vvb[pi % 2].a[:, :, :],
                            vb_d.ap()[2 * pi:2 * pi + 2, :, dh * 1024:(dh + 1) * 1024].rearrange("i p f -> p i f"),
                            r=[dbuf("vb", 2 * pi), dbuf("vb", 2 * pi + 1)], w=[vvb[pi % 2]])
                    load_v(0)
                    for i in range(128):
                        if i % 2 == 0 and i // 2 + 1 < 64:
                            load_v(i // 2 + 1)
                        vv_ = vvb[(i // 2) % 2]
                        for ts in range(2):
                            for cq in range(2):
                                bk = PSB[ts * 2 + cq]
                                MM(bk.a[:, :], GA.a[:, i, ts * 128:(ts + 1) * 128],
                                   vv_.a[:, i % 2, cq * 512:(cq + 1) * 512],
                                   start=(i == 0), stop=(i == 127), r=[GA, vv_], w=[bk])
                    for ts in range(2):
                        tile = tb0 + ts
                        y_ = yo[ts]
                        for cq in range(2):
                            bk = PSB[ts * 2 + cq]
                            c0 = dh * 1024 + cq * 512
                            TT("dve", y_.a[:, cq * 512:(cq + 1) * 512], bk.a[:, :], GTR.a[:, c0:c0 + 512], ALU.mult,
                               r=[bk, GTR], w=[y_])
                        TT("pool", y_.a[:, :], y_.a[:, :], xblk.a[:, ts, dh * 1024:(dh + 1) * 1024], ALU.add,
                           r=[y_, xblk], w=[y_])
                        DMA("sp", xdst.ap()[tile * 128:(tile + 1) * 128, dh * 1024:(dh + 1) * 1024], y_.a[:, :],
                            r=[y_], w=[dbuf(xdst.name, tile)])

        def tail(xsrc, norm):
            new_phase()
            xt = [AR.take([128, D]) for _ in range(2)]
            junk = AR.take([128, D], BF16)
            if norm:
                DMA("sp", GTR.a[:], fg_d.ap().to_broadcast([128, D]), w=[GTR])
            for tile in range(NT):
                s = tile % 2
                DMA("sp", xt[s].a[:, :], xsrc.ap()[tile * 128:(tile + 1) * 128, :], r=[dbuf(xsrc.name, tile)], w=[xt[s]])
                if norm:
                    ACT(junk.a[:, :], xt[s].a[:, :], AF.Square, accum=st8.a[:, 0:1], r=[xt[s]], w=[junk, st8])
                    rstd_of(st8.a[:, 0:1], 1.0 / D, st8.a[:, 1:2], r=[st8], w=[st8])
                    STT(xt[s].a[:, :], xt[s].a[:, :], st8.a[:, 1:2], GTR.a[:], ALU.mult, ALU.mult,
                        r=[xt[s], st8, GTR], w=[xt[s]])
                DMA("sp", out_d.ap()[tile * 128:(tile + 1) * 128, :], xt[s].a[:, :], r=[xt[s]], w=[dbuf("out", tile)])

        done = False
        cur = x_d
        for l in range(NL):
            setup_layer(l)
            mixer_layer(l, cur, xa_d)
            if stop_after == ("mixer", l):
                tail(xa_d, False)
                done = True
                break
            peer_layer(l, xa_d, xb_d)
            cur = xb_d
            if stop_after == ("peer", l):
                tail(xb_d, False)
                done = True
                break
        if not done:
            tail(cur, True)
        P.finish("sp")
        P.emit()
    return nc


_CONSTS = None


def _consts():
    global _CONSTS
    if _CONSTS is None:
        c = np.zeros((128, 5, 128), np.float32)
        c[:, 4, :] = np.arange(128, dtype=np.float32)[None, :]
        c[:, 0, :] = np.eye(128, dtype=np.float32)
        tri = np.triu(np.ones((128, 128), np.float32))
        c[:, 1, :] = tri
        c[:, 2, :] = -tri / 16.0
        c[:, 3, :] = -(1.0 - tri) / 16.0
        _CONSTS = c
    return _CONSTS


def make_in_maps(inputs, ncores, ntok):
    f = lambda a: np.ascontiguousarray(np.asarray(a, dtype=np.float32))
    shared = {
        "ada_w": f(inputs["ada_w"]),
        "ada_b": f(inputs["ada_b"]).reshape(DEPTH, 96, 128),
        "norm1_g": f(inputs["norm1_g"]).reshape(DEPTH, 16, 128),
        "w_in": f(inputs["w_in"]),
        "gla_w_a2": f(inputs["gla_w_a2"]),
        "gla_b_a": f(inputs["gla_b_a"]).reshape(DEPTH, 1, 512),
        "gla_norm_g": f(inputs["gla_norm_g"]).reshape(DEPTH, 1, 1024),
        "gmlp_vnorm_g": f(inputs["gmlp_vnorm_g"]).reshape(DEPTH, 1, 1024),
        "gmlp_ws": f(inputs["gmlp_ws"]),
        "gmlp_b": f(inputs["gmlp_b"]),
        "gmlp_out_g": f(inputs["gmlp_out_g"]).reshape(DEPTH, 1, 1024),
        "w_out": f(inputs["w_out"]),
        "norm2_g": f(inputs["norm2_g"]).reshape(DEPTH, 16, 128),
        "peer_wq": f(inputs["peer_wq"]),
        "peer_k1": f(inputs["peer_k1"]),
        "peer_k2": f(inputs["peer_k2"]),
        "peer_u": f(inputs["peer_u"]),
        "peer_v": f(inputs["peer_v"]),
        "final_g": f(inputs["final_g"]).reshape(1, D),
        "consts": _consts(),
    }
    x = f(inputs["x"])
    c = f(inputs["c"])
    maps = []
    for b in range(ncores):
        m = dict(shared)
        m["x"] = np.ascontiguousarray(x[b, :ntok, :])
        m["c"] = np.ascontiguousarray(c[b].reshape(16, 128))
        maps.append(m)
    return maps


_NC_CACHE = {}


def kernel(**inputs):
    key = "full"
    if key not in _NC_CACHE:
        _NC_CACHE[key] = build(NT=SEQ // 128, NL=DEPTH)
    nc = _NC_CACHE[key]
    in_maps = make_in_maps(inputs, BATCH, SEQ)
    res = run_bass_kernel_spmd(nc, in_maps, core_ids=list(range(BATCH)))
    out = np.stack([np.asarray(res.results[b]["out"], dtype=np.float32) for b in range(BATCH)], axis=0)
    return out
```

```python
import numpy as np
from contextlib import ExitStack
import concourse.bass as bass
import concourse.mybir as mybir
from concourse.bass_utils import run_bass_kernel_spmd

F32 = mybir.dt.float32
BF16 = mybir.dt.bfloat16
AF = mybir.ActivationFunctionType
ALU = mybir.AluOpType
AX = mybir.AxisListType

D = 2048
DC = 16
SEQ = 4096
BATCH = 4
DEPTH = 2
IN_COLS = 5136
NEXP = 16384
EPS = 1e-6
C_Q, C_K, C_V, C_R, C_AL, C_U, C_VS = 0, 512, 1024, 2048, 3072, 3088, 4112
NEG = -1.0e30


class Buf:
    __slots__ = ("lw", "rd")

    def __init__(self):
        self.lw = None
        self.rd = {}


class X:
    def __init__(self, t):
        self.a = t
        self.b = Buf()


def _b(x):
    return x.b if isinstance(x, X) else x


class Prog:
    ENGS = ("sp", "act", "dve", "pool", "pe")
    NDMA = 12

    def __init__(self, nc, self_wait=True):
        self.nc = nc
        self.h = {"sp": nc.sync, "act": nc.scalar, "dve": nc.vector,
                  "pool": nc.gpsimd, "pe": nc.tensor}
        self.ops = {e: [] for e in self.ENGS}
        self.cnt = {e: 0 for e in self.ENGS}
        self.waited = {e: {} for e in self.ENGS}
        self.dma_next = {e: 0 for e in self.ENGS}
        self.dma_val = {}
        self.self_wait = self_wait
        self.sems = {}

    def op(self, eng, fn, reads=(), writes=(), dma=False):
        waits = {}

        def need(kv):
            if kv is None:
                return
            k, v = kv
            if v > 0 and waits.get(k, 0) < v:
                waits[k] = v
        reads = [_b(x) for x in reads]
        writes = [_b(x) for x in writes]
        for b in reads:
            need(b.lw)
        for b in writes:
            need(b.lw)
            for k, v in b.rd.items():
                need((k, v))
        if dma:
            slot = self.dma_next[eng]
            self.dma_next[eng] = (slot + 1) % self.NDMA
            key = ("dma", eng, slot)
            prev = self.dma_val.get(key, 0)
            need((key, prev))
            myval = prev + 16
            self.dma_val[key] = myval
        else:
            key = ("eng", eng)
            self.cnt[eng] += 1
            myval = self.cnt[eng]
        fw = []
        for k, v in waits.items():
            if k == ("eng", eng) and (eng == "pe" or not self.self_wait):
                continue
            if self.waited[eng].get(k, 0) >= v:
                continue
            self.waited[eng][k] = v
            fw.append((k, v))
        self.ops[eng].append((fw, fn, key, 16 if dma else 1))
        for b in reads:
            if b.rd.get(key, 0) < myval:
                b.rd[key] = myval
        for b in writes:
            b.lw = (key, myval)
            b.rd = {}

    def fence(self):
        for e in self.ENGS:
            fw = []
            for key, v in self.dma_val.items():
                if self.waited[e].get(key, 0) < v:
                    self.waited[e][key] = v
                    fw.append((key, v))
            for e2 in self.ENGS:
                k = ("eng", e2)
                if self.cnt[e2] > 0 and self.waited[e].get(k, 0) < self.cnt[e2]:
                    self.waited[e][k] = self.cnt[e2]
                    fw.append((k, self.cnt[e2]))
            if fw:
                self.ops[e].append((fw, None, None, 0))

    def finish(self, eng="sp"):
        fw = []
        for key, v in self.dma_val.items():
            if self.waited[eng].get(key, 0) < v:
                fw.append((key, v))
        for e in self.ENGS:
            if e != eng and self.cnt[e] > 0:
                fw.append((("eng", e), self.cnt[e]))
        self.ops[eng].append((fw, None, None, 0))

    def emit(self):
        nc = self.nc
        keys = set()
        for e in self.ENGS:
            for fw, fn, key, inc in self.ops[e]:
                if key is not None:
                    keys.add(key)
                for k, v in fw:
                    keys.add(k)
        with ExitStack() as st:
            for k in sorted(keys):
                self.sems[k] = st.enter_context(
                    nc.semaphore("s_" + "_".join(str(x) for x in k)))
            block = st.enter_context(nc.Block())

            def run(e):
                h = self.h[e]
                for fw, fn, key, inc in self.ops[e]:
                    for k, v in fw:
                        h.wait_ge(self.sems[k], v)
                    if fn is not None:
                        fn().then_inc(self.sems[key], inc)

            @block.sync
            def _(x):
                run("sp")

            @block.scalar
            def _(x):
                run("act")

            @block.vector
            def _(x):
                run("dve")

            @block.gpsimd
            def _(x):
                run("pool")

            @block.tensor
            def _(x):
                run("pe")


class Arena:
    def __init__(self, t, nwords):
        self.t = t
        self.n = nwords
        self.off = 0

    def reset(self):
        self.off = 0

    def take(self, shape, dt=F32, parts=128):
        n = 1
        for s in shape[1:]:
            n *= s
        four = dt in (F32, mybir.dt.uint32, mybir.dt.int32)
        words = n if four else (n + 1) // 2
        words = (words + 15) // 16 * 16
        assert self.off + words <= self.n, ("arena overflow", self.off, words, self.n)
        ap = self.t[0:shape[0], self.off:self.off + words]
        self.off += words
        if dt != F32:
            ap = ap.bitcast(dt)
        ap = ap[:, 0:n]
        if len(shape) == 3:
            ap = ap.rearrange("p (a b) -> p a b", b=shape[2])
        elif len(shape) == 4:
            ap = ap.rearrange("p (a b c) -> p a b c", b=shape[2], c=shape[3])
        return X(ap)


def build(NT=32, NL=DEPTH, stop_after=None, opts=None):
    opts = opts or {}
    NTOK = NT * 128
    assert NT % 4 == 0
    nc = bass.Bass("TRN2", target_bir_lowering=False)
    P = Prog(nc, self_wait=bool(opts.get('self_wait', True)))

    def din(name, shape):
        return nc.dram_tensor(name, shape, F32, kind="ExternalInput")

    x_d = din("x", [NTOK, D])
    c_d = din("c", [16, 128])
    adaw_d = din("ada_w", [DEPTH, D, 6 * D])
    adab_d = din("ada_b", [DEPTH, 96, 128])
    n1g_d = din("norm1_g", [DEPTH, 16, 128])
    win_d = din("w_in", [DEPTH, D, IN_COLS])
    wa2_d = din("gla_w_a2", [DEPTH, 16, 512])
    ba_d = din("gla_b_a", [DEPTH, 1, 512])
    gng_d = din("gla_norm_g", [DEPTH, 1, 1024])
    vng_d = din("gmlp_vnorm_g", [DEPTH, 1, 1024])
    ws_d = din("gmlp_ws", [DEPTH, 8, 128, 128])
    gb_d = din("gmlp_b", [DEPTH, 8, 128])
    og_d = din("gmlp_out_g", [DEPTH, 1, 1024])
    wout_d = din("w_out", [DEPTH, D, D])
    n2g_d = din("norm2_g", [DEPTH, 16, 128])
    wq_d = din("peer_wq", [DEPTH, D, D])
    k1_d = din("peer_k1", [DEPTH, 8, 128, 128])
    k2_d = din("peer_k2", [DEPTH, 8, 128, 128])
    pu_d = din("peer_u", [DEPTH, NEXP, D])
    pv_d = din("peer_v", [DEPTH, NEXP, D])
    fg_d = din("final_g", [1, D])
    cst_d = din("consts", [128, 5, 128])
    out_d = nc.dram_tensor("out", [NTOK, D], F32, kind="ExternalOutput")

    def dscr(name, shape, dt=F32):
        return nc.dram_tensor(name, shape, dt, kind="Internal")

    xa_d = dscr("xa", [NTOK, D])
    xb_d = dscr("xb", [NTOK, D])
    modrow_d = dscr("modrow", [DEPTH, 32, 128])
    s2_d = dscr("s2rep", [NTOK, 16, 1024])
    wqb_d = dscr("wqb", [4, 128, DC * 512], BF16)
    winb_d = dscr("winb", [128, DC, IN_COLS], BF16)
    woutb_d = dscr("woutb", [128, DC, D], BF16)
    ut_d = dscr("uts", [128, 128, DC * 128], BF16)
    vb_d = dscr("vbs", [128, 128, D], BF16)

    db = {}

    def dbuf(*key):
        if key not in db:
            db[key] = Buf()
        return db[key]

    st = ExitStack()
    with st:
        def SB(name, shape, dt=F32):
            return X(st.enter_context(nc.sbuf_tensor(name, shape, dt)))

        def DMA(q, out, in_, r=(), w=()):
            h = P.h[q]
            P.op(q, lambda: h.dma_start(out=out, in_=in_), reads=r, writes=w, dma=True)

        def MM(out, lhsT, rhs, start=True, stop=True, r=(), w=()):
            P.op("pe", lambda: nc.tensor.matmul(out, lhsT=lhsT, rhs=rhs, start=start, stop=stop),
                 reads=r, writes=w)

        def TR(out, in_, ident, r=(), w=()):
            P.op("pe", lambda: nc.tensor.transpose(out=out, in_=in_, identity=ident),
                 reads=r, writes=w)

        def ACT(out, in_, func, bias=None, scale=None, accum=None, r=(), w=()):
            kw = {}
            if bias is not None:
                kw["bias"] = bias
            if scale is not None:
                kw["scale"] = scale
            if accum is not None:
                kw["accum_out"] = accum
            P.op("act", lambda: nc.scalar.activation(out=out, in_=in_, func=func, **kw),
                 reads=r, writes=w)

        def TS(e, out, in0, s1, s2, op0, op1=None, r=(), w=()):
            h = P.h[e]
            if op1 is None:
                P.op(e, lambda: h.tensor_scalar(out=out, in0=in0, scalar1=s1, scalar2=None, op0=op0),
                     reads=r, writes=w)
            else:
                P.op(e, lambda: h.tensor_scalar(out=out, in0=in0, scalar1=s1, scalar2=s2, op0=op0, op1=op1),
                     reads=r, writes=w)

        def TT(e, out, in0, in1, op, r=(), w=()):
            h = P.h[e]
            P.op(e, lambda: h.tensor_tensor(out=out, in0=in0, in1=in1, op=op), reads=r, writes=w)

        def STT(out, in0, scalar, in1, op0, op1, r=(), w=()):
            P.op("dve", lambda: nc.vector.scalar_tensor_tensor(out=out, in0=in0, scalar=scalar, in1=in1,
                                                               op0=op0, op1=op1), reads=r, writes=w)

        def CP(e, out, in_, r=(), w=()):
            h = P.h[e]
            if e == "act":
                P.op(e, lambda: nc.scalar.copy(out=out, in_=in_), reads=r, writes=w)
            else:
                P.op(e, lambda: h.tensor_copy(out=out, in_=in_), reads=r, writes=w)

        def MSET(e, out, val, w=()):
            h = P.h[e]
            P.op(e, lambda: h.memset(out, val), writes=w)

        def RED(out, in_, op, r=(), w=()):
            P.op("dve", lambda: nc.vector.tensor_reduce(out=out, in_=in_, axis=AX.X, op=op), reads=r, writes=w)

        def MAX8(out, in_, r=(), w=()):
            P.op("dve", lambda: nc.vector.max(out=out, in_=in_), reads=r, writes=w)

        def MIDX(out, mx, vals, r=(), w=()):
            P.op("dve", lambda: nc.vector.max_index(out=out, in_max=mx, in_values=vals), reads=r, writes=w)

        def MREP(out, rep, vals, r=(), w=()):
            P.op("dve", lambda: nc.vector.match_replace(out=out, in_to_replace=rep, in_values=vals,
                                                        imm_value=NEG), reads=r, writes=w)

        def RECIP(out, in_, r=(), w=()):
            P.op("dve", lambda: nc.vector.reciprocal(out=out, in_=in_), reads=r, writes=w)

        def v3(ap, b):
            return ap.rearrange("p (a b) -> p a b", b=b)

        PSB = [X(st.enter_context(nc.psum_tensor("psb%d" % i, [128, 512], F32))) for i in range(8)]

        def psbf(i):
            return PSB[i].a[:, :].bitcast(BF16)

        cst = SB("cst", [128, 5, 128])
        identb = SB("identb", [128, 128], BF16)
        epsc = SB("epsc", [128, 1])
        onec = SB("onec", [128, 1])
        smallT = SB("smallT", [128, 128])
        condT = SB("condT", [128, 16, 2])
        modT = SB("modT", [128, 96])
        AB = SB("AB", [128, 2, 16])
        GTR = SB("GTR", [128, D])
        st8 = SB("st8", [128, 64])
        st8b = SB("st8b", [128, 64])
        ARW = 48128
        AR = Arena(st.enter_context(nc.sbuf_tensor("arena", [128, ARW], F32)), ARW)

        def new_phase():
            P.fence()
            AR.reset()

        ident = cst.a[:, 0, :]
        M_G = cst.a[:, 2, :]
        M_D = cst.a[:, 3, :]
        iota = cst.a[:, 4, :]

        def rstd_of(ss, inv_n, tmp, r, w):
            ACT(tmp, ss, AF.Sqrt, bias=epsc.a[:, 0:1], scale=inv_n, r=r + [epsc], w=w)
            RECIP(tmp, tmp, r=w, w=w)

        stage = AR.take([128, 128])
        DMA("sp", cst.a[:], cst_d.ap(), w=[cst])
        CP("dve", identb.a[:], ident, r=[cst], w=[identb])
        MSET("dve", epsc.a[:], EPS, w=[epsc])
        MSET("dve", onec.a[:], 1.0, w=[onec])
        DMA("sp", stage.a[0:16, :], c_d.ap(), w=[stage])
        TR(PSB[0].a[:, 0:16], stage.a[0:16, :], cst.a[0:16, 0, 0:16], r=[stage, cst], w=[PSB[0]])
        ACT(condT.a[:, :, 0], PSB[0].a[:, 0:16], AF.Silu, r=[PSB[0]], w=[condT])
        ACT(condT.a[:, :, 1], PSB[0].a[:, 0:16], AF.Silu, r=[PSB[0]], w=[condT])

        def setup_layer(l):
            new_phase()
            stage = AR.take([128, 128])
            gtT = AR.take([128, 32])
            gtrow = AR.take([32, 128])
            aw = [AR.take([128, DC, 256]) for _ in range(2)]
            DMA("sp", stage.a[0:96, :], adab_d.ap()[l], w=[stage])
            DMA("sp", stage.a[96:112, :], n1g_d.ap()[l], w=[stage])
            DMA("sp", stage.a[112:128, :], n2g_d.ap()[l], w=[stage])
            TR(PSB[0].a[:, 0:128], stage.a[:, :], ident, r=[stage, cst], w=[PSB[0]])
            CP("dve", smallT.a[:], PSB[0].a[:, 0:128], r=[PSB[0]], w=[smallT])
            src = adaw_d.ap()[l].rearrange("(dc p) c -> p dc c", p=128)
            for cc in range(48):
                t = aw[cc % 2]
                DMA("sp" if cc % 2 == 0 else "act", t.a, src[:, :, cc * 256:(cc + 1) * 256], w=[t])
                for mm in range(2):
                    m = cc * 2 + mm
                    for dc in range(DC):
                        MM(PSB[1].a[:, 2 * m:2 * m + 2], t.a[:, dc, mm * 128:(mm + 1) * 128],
                           condT.a[:, dc, :], start=(dc == 0), stop=(dc == DC - 1),
                           r=[t, condT], w=[PSB[1]])
            TT("dve", modT.a[:], v3(PSB[1].a[:, 0:192], 2)[:, :, 0], smallT.a[:, 0:96], ALU.add,
               r=[PSB[1], smallT], w=[modT])
            STT(AB.a[:, 0, :], modT.a[:, 16:32], 1.0, smallT.a[:, 96:112], ALU.add, ALU.mult,
                r=[modT, smallT], w=[AB])
            STT(AB.a[:, 1, :], modT.a[:, 64:80], 1.0, smallT.a[:, 112:128], ALU.add, ALU.mult,
                r=[modT, smallT], w=[AB])
            CP("dve", gtT.a[:, 0:16], modT.a[:, 32:48], r=[modT], w=[gtT])
            CP("dve", gtT.a[:, 16:32], modT.a[:, 80:96], r=[modT], w=[gtT])
            TR(PSB[0].a[0:32, 0:128], gtT.a[:, :], ident, r=[gtT, cst], w=[PSB[0]])
            CP("dve", gtrow.a[:, :], PSB[0].a[0:32, 0:128], r=[PSB[0]], w=[gtrow])
            DMA("sp", modrow_d.ap()[l], gtrow.a[:, :], r=[gtrow], w=[dbuf("modrow", l)])
            wst = [AR.take([128, DC, 512], BF16) for _ in range(2)]
            win_v = win_d.ap()[l].rearrange("(dc p) c -> p dc c", p=128)
            wout_v = wout_d.ap()[l].rearrange("(mc p) c -> p mc c", p=128)
            jobs = [(win_v, winb_d, c0, min(512, IN_COLS - c0)) for c0 in range(0, IN_COLS, 512)]
            jobs += [(wout_v, woutb_d, c0, 512) for c0 in range(0, D, 512)]
            for k, (sv_, dd, c0, n) in enumerate(jobs):
                t = wst[k % 2]
                DMA("pool", t.a[:, :, 0:n], sv_[:, :, c0:c0 + n], w=[t])
                DMA("sp", dd.ap()[:, :, c0:c0 + n], t.a[:, :, 0:n], r=[t], w=[dbuf(dd.name)])

        def tr8(srcd_ap, dst, masked, kin):
            DMA("sp", kin.a, srcd_ap, w=[kin])
            for h in range(8):
                bk = PSB[2 + h // 4]
                TR(bk.a[:, (h % 4) * 128:(h % 4 + 1) * 128], kin.a[:, h, :], ident, r=[kin, cst], w=[bk])
            for hb in range(2):
                bk = PSB[2 + hb]
                if masked:
                    TT("dve", dst.a[:, hb * 4:(hb + 1) * 4, :], v3(bk.a[:, :], 128),
                       cst.a[:, 1:2, :].to_broadcast([128, 4, 128]), ALU.mult, r=[bk, cst], w=[dst])
                else:
                    CP("dve", dst.a[:, hb * 4:(hb + 1) * 4, :], v3(bk.a[:, :], 128), r=[bk], w=[dst])

        def norm_T(x_ap, xdep, which, bshift, dst, dst_fn, xnb, junk):
            ACT(junk.a[:, :], x_ap, AF.Square, accum=st8.a[:, 0:1], r=[xdep], w=[junk, st8])
            rstd_of(st8.a[:, 0:1], 1.0 / D, st8.a[:, 1:2], r=[st8], w=[st8])
            TS("dve", xnb.a[:, :], x_ap, st8.a[:, 1:2], None, ALU.mult, r=[xdep, st8], w=[xnb])
            for dc in range(DC):
                bk = PSB[dc // 8]
                TR(psbf(dc // 8)[:, (dc % 8) * 128:(dc % 8 + 1) * 128], xnb.a[:, dc * 128:(dc + 1) * 128],
                   identb.a[:], r=[xnb, identb], w=[bk])
            for dc in range(DC):
                bk = PSB[dc // 8]
                src = psbf(dc // 8)[:, (dc % 8) * 128:(dc % 8 + 1) * 128]
                if dc % 2 == 0:
                    TS("dve", dst_fn(dc), src, AB.a[:, which, dc:dc + 1],
                       modT.a[:, bshift + dc:bshift + dc + 1], ALU.mult, ALU.add,
                       r=[bk, AB, modT], w=[dst])
                else:
                    ACT(dst_fn(dc), src, AF.Identity, bias=modT.a[:, bshift + dc:bshift + dc + 1],
                        scale=AB.a[:, which, dc:dc + 1], r=[bk, AB, modT], w=[dst])

        def mixer_layer(l, xsrc, xdst):
            new_phase()
            ngr = AR.take([128, 1024])
            vgr = AR.take([128, 1024])
            ogr = AR.take([128, 1024])
            W2a = AR.take([17, 512])
            wsT = AR.take([128, 8, 128], BF16)
            bT = AR.take([128, 8])
            S32 = AR.take([128, 4, 256])
            Sbf = AR.take([128, 4, 256], BF16)
            xt = [AR.take([128, D]) for _ in range(2)]
            xnb = AR.take([128, D], BF16)
            junk = AR.take([128, D], BF16)
            wc = [AR.take([128, DC, 512], BF16) for _ in range(2)]
            hTg = AR.take([128, DC, 512], BF16)
            ymT = hTg
            alT = AR.take([17, 512])
            GTg = AR.take([128, 4, 4, 128])
            EGl = AR.take([128, 4, 4])
            EDg = AR.take([128, 4, 512], BF16)
            qtg = AR.take([128, 4, 512], BF16)
            ktg = AR.take([128, 4, 512], BF16)
            kdg = AR.take([128, 4, 512], BF16)
            vg_ = AR.take([128, 4, 1024], BF16)
            srg = AR.take([128, 4, 1024], BF16)
            gug = AR.take([128, 4, 1024], BF16)
            gvg = AR.take([128, 4, 1024], BF16)
            lt = AR.take([128, 512])
            et = AR.take([128, 512])
            attm = AR.take([128, 4, 128], BF16)
            ymix = AR.take([128, D], BF16)
            f1 = AR.take([128, 1024])
            f2 = AR.take([128, 1024])
            vnb = AR.take([128, 1024], BF16)
            xp = [AR.take([128, 512]) for _ in range(2)]
            xo = [AR.take([128, 512]) for _ in range(2)]
            stage = AR.take([8, 128])
            kin = X(f1.a[:, :].rearrange("p (a b) -> p a b", b=128))
            kin.b = f1.b

            DMA("sp", GTR.a[:], modrow_d.ap()[l, 0:16, :].rearrange("a b -> (a b)").unsqueeze(0).to_broadcast([128, D]),
                r=[dbuf("modrow", l)], w=[GTR])
            DMA("sp", W2a.a[0:16, :], wa2_d.ap()[l], w=[W2a])
            DMA("sp", W2a.a[16:17, :], ba_d.ap()[l], w=[W2a])
            DMA("sp", ngr.a[:, :], gng_d.ap()[l].to_broadcast([128, 1024]), w=[ngr])
            DMA("sp", vgr.a[:, :], vng_d.ap()[l].to_broadcast([128, 1024]), w=[vgr])
            DMA("sp", ogr.a[:, :], og_d.ap()[l].to_broadcast([128, 1024]), w=[ogr])
            tr8(ws_d.ap()[l].rearrange("h t s -> t h s"), wsT, True, kin)
            DMA("sp", stage.a[0:8, :], gb_d.ap()[l], w=[stage])
            TR(PSB[0].a[:, 0:8], stage.a[0:8, :], cst.a[0:8, 0, 0:8], r=[stage, cst], w=[PSB[0]])
            CP("dve", bT.a[:, :], PSB[0].a[:, 0:8], r=[PSB[0]], w=[bT])
            MSET("dve", S32.a[:], 0.0, w=[S32])
            MSET("pool", Sbf.a[:], 0.0, w=[Sbf])
            MSET("dve", alT.a[:, :], 1.0, w=[alT])

            wcnt = [0]

            def load_w(src_ap3, ncols):
                t = wc[wcnt[0] % 2]
                wcnt[0] += 1
                DMA("sp", t.a[:, :, 0:ncols], src_ap3, r=[dbuf("winb"), dbuf("woutb")], w=[t])
                return t

            win_v = winb_d.ap()
            wo_src = woutb_d.ap()

            def win_cols(c0, n):
                return win_v[:, :, c0:c0 + n]

            for g in range(NT // 4):
                t0 = g * 4
                DMA("sp", xt[0].a[:, :], xsrc.ap()[t0 * 128:(t0 + 1) * 128, :], r=[dbuf(xsrc.name, t0)], w=[xt[0]])
                for j in range(4):
                    if j + 1 < 4:
                        s = (j + 1) % 2
                        DMA("sp", xt[s].a[:, :], xsrc.ap()[(t0 + j + 1) * 128:(t0 + j + 2) * 128, :],
                            r=[dbuf(xsrc.name, t0 + j + 1)], w=[xt[s]])
                    norm_T(xt[j % 2].a[:, :], xt[j % 2], 0, 0, hTg,
                           lambda dc, j=j: hTg.a[:, dc, j * 128:(j + 1) * 128], xnb, junk)
                wal = load_w(win_cols(C_AL, 16), 16)
                wq_ = load_w(win_cols(C_Q, 512), 512)
                for dc in range(DC):
                    MM(PSB[2].a[0:16, :], wal.a[:, dc, 0:16], hTg.a[:, dc, :], start=(dc == 0), stop=(dc == DC - 1),
                       r=[wal, hTg], w=[PSB[2]])
                CP("dve", alT.a[0:16, :], PSB[2].a[0:16, :], r=[PSB[2]], w=[alT])
                for j in range(4):
                    tk = slice(j * 128, (j + 1) * 128)
                    MM(PSB[4].a[:, :], alT.a[:, tk], W2a.a[:, :], r=[alT, W2a], w=[PSB[4]])
                    ACT(et.a[:, :], PSB[4].a[:, :], AF.Exp, scale=-1.0, r=[PSB[4]], w=[et])
                    ACT(lt.a[:, :], et.a[:, :], AF.Ln, bias=onec.a[:, 0:1], scale=1.0, r=[et, onec], w=[lt])
                    MM(PSB[4].a[:, :], M_D, lt.a[:, :], r=[cst, lt], w=[PSB[4]])
                    ACT(EDg.a[:, j, :], PSB[4].a[:, :], AF.Exp, r=[PSB[4]], w=[EDg])
                    for h in range(4):
                        MM(PSB[5].a[:, h * 128:(h + 1) * 128], lt.a[:, h * 128:(h + 1) * 128], M_G,
                           r=[lt, cst], w=[PSB[5]])
                    CP("dve", GTg.a[:, j, :, :], v3(PSB[5].a[:, :], 128), r=[PSB[5]], w=[GTg])
                ACT(EGl.a[:, :, :], GTg.a[:, :, :, 127], AF.Exp, r=[GTg], w=[EGl])
                wk_ = load_w(win_cols(C_K, 512), 512)
                for h in range(4):
                    bk = PSB[2 + h % 2]
                    for dc in range(DC):
                        MM(bk.a[:, :], wq_.a[:, dc, h * 128:(h + 1) * 128], hTg.a[:, dc, :],
                           start=(dc == 0), stop=(dc == DC - 1), r=[wq_, hTg], w=[bk])
                    ACT(v3(et.a[:, :], 128), GTg.a[:, :, h, :], AF.Exp, r=[GTg], w=[et])
                    STT(qtg.a[:, h, :], bk.a[:, :], 128.0 ** -0.5, et.a[:, :], ALU.mult, ALU.mult,
                        r=[bk, et], w=[qtg])
                wv0 = load_w(win_cols(C_V, 512), 512)
                for h in range(4):
                    bk = PSB[2 + h % 2]
                    for dc in range(DC):
                        MM(bk.a[:, :], wk_.a[:, dc, h * 128:(h + 1) * 128], hTg.a[:, dc, :],
                           start=(dc == 0), stop=(dc == DC - 1), r=[wk_, hTg], w=[bk])
                    ACT(v3(et.a[:, :], 128), GTg.a[:, :, h, :], AF.Exp, scale=-1.0, r=[GTg], w=[et])
                    TT("dve", ktg.a[:, h, :], bk.a[:, :], et.a[:, :], ALU.mult, r=[bk, et], w=[ktg])
                for j in range(4):
                    bk = PSB[2 + j % 2]
                    for dc in range(DC):
                        MM(bk.a[:, :], hTg.a[:, dc, j * 128:(j + 1) * 128], wk_.a[:, dc, :],
                           start=(dc == 0), stop=(dc == DC - 1), r=[wk_, hTg], w=[bk])
                    TT("dve", kdg.a[:, j, :], bk.a[:, :], EDg.a[:, j, :], ALU.mult, r=[bk, EDg], w=[kdg])
                chunks = [(C_V, vg_, 0, None), (C_V + 512, vg_, 512, None),
                          (C_R, srg, 0, AF.Silu), (C_R + 512, srg, 512, AF.Silu),
                          (C_U, gug, 0, AF.Gelu_apprx_tanh), (C_U + 512, gug, 512, AF.Gelu_apprx_tanh),
                          (C_VS, gvg, 0, AF.Gelu_apprx_tanh), (C_VS + 512, gvg, 512, AF.Gelu_apprx_tanh)]
                wcur = wv0
                for ci, (c0, dstt, off, fn) in enumerate(chunks):
                    if ci + 1 < len(chunks):
                        nxt = load_w(win_cols(chunks[ci + 1][0], 512), 512)
                    else:
                        nxt = load_w(wo_src[:, :, 0:512], 512)
                    for j in range(4):
                        bk = PSB[2 + j % 2]
                        for dc in range(DC):
                            MM(bk.a[:, :], hTg.a[:, dc, j * 128:(j + 1) * 128], wcur.a[:, dc, :],
                               start=(dc == 0), stop=(dc == DC - 1), r=[wcur, hTg], w=[bk])
                        if fn is None:
                            CP("act", dstt.a[:, j, off:off + 512], bk.a[:, :], r=[bk], w=[dstt])
                        else:
                            ACT(dstt.a[:, j, off:off + 512], bk.a[:, :], fn, r=[bk], w=[dstt])
                    wcur = nxt
                wo = wcur
                for j in range(4):
                    tk = slice(j * 128, (j + 1) * 128)
                    for h in range(4):
                        MM(PSB[5].a[:, h * 128:(h + 1) * 128], ktg.a[:, h, tk], qtg.a[:, h, tk],
                           r=[ktg, qtg], w=[PSB[5]])
                    TT("dve", attm.a[:, :, :], v3(PSB[5].a[:, :], 128), cst.a[:, 1:2, :].to_broadcast([128, 4, 128]),
                       ALU.mult, r=[PSB[5], cst], w=[attm])
                    for h in range(4):
                        bk = PSB[6 + h // 2]
                        oc = slice((h % 2) * 256, (h % 2 + 1) * 256)
                        MM(bk.a[:, oc], attm.a[:, h, :], vg_.a[:, j, h * 256:(h + 1) * 256], start=True, stop=False,
                           r=[attm, vg_], w=[bk])
                        MM(bk.a[:, oc], qtg.a[:, h, tk], Sbf.a[:, h, :], start=False, stop=True,
                           r=[qtg, Sbf], w=[bk])
                    for h in range(4):
                        bk = PSB[h // 2]
                        oc = slice((h % 2) * 256, (h % 2 + 1) * 256)
                        MM(bk.a[:, oc], kdg.a[:, j, h * 128:(h + 1) * 128], vg_.a[:, j, h * 256:(h + 1) * 256],
                           r=[kdg, vg_], w=[bk])
                    for h in range(4):
                        bk = PSB[h // 2]
                        oc = slice((h % 2) * 256, (h % 2 + 1) * 256)
                        STT(S32.a[:, h, :], S32.a[:, h, :], EGl.a[:, j, h:h + 1], bk.a[:, oc], ALU.mult, ALU.add,
                            r=[S32, EGl, bk], w=[S32])
                    CP("pool", Sbf.a[:, :, :], S32.a[:, :, :], r=[S32], w=[Sbf])
                    for h in range(4):
                        bk = PSB[6 + h // 2]
                        oc = slice((h % 2) * 256, (h % 2 + 1) * 256)
                        ACT(junk.a[:, 0:256], bk.a[:, oc], AF.Square, accum=st8b.a[:, h:h + 1], r=[bk], w=[junk, st8b])
                    rstd_of(st8b.a[:, 0:4], 1.0 / 256, st8b.a[:, 4:8], r=[st8b], w=[st8b])
                    for h in range(4):
                        bk = PSB[6 + h // 2]
                        oc = slice((h % 2) * 256, (h % 2 + 1) * 256)
                        STT(f1.a[:, h * 256:(h + 1) * 256], bk.a[:, oc], st8b.a[:, 4 + h:5 + h],
                            ngr.a[:, h * 256:(h + 1) * 256], ALU.mult, ALU.mult, r=[bk, st8b, ngr], w=[f1])
                    TT("dve", ymix.a[:, 0:1024], f1.a[:, :], srg.a[:, j, :], ALU.mult, r=[f1, srg], w=[ymix])
                    TT("dve", f1.a[:, :], gvg.a[:, j, :], gvg.a[:, j, :], ALU.mult, r=[gvg], w=[f1])
                    RED(st8b.a[:, 8:16], v3(f1.a[:, :], 128), ALU.add, r=[f1], w=[st8b])
                    rstd_of(st8b.a[:, 8:16], 1.0 / 128, st8b.a[:, 16:24], r=[st8b], w=[st8b])
                    TT("dve", v3(f1.a[:, :], 128), v3(gvg.a[:, j, :], 128),
                       st8b.a[:, 16:24].unsqueeze(2).to_broadcast([128, 8, 128]), ALU.mult, r=[gvg, st8b], w=[f1])
                    TT("dve", vnb.a[:, :], f1.a[:, :], vgr.a[:, :], ALU.mult, r=[f1, vgr], w=[vnb])
                    for h in range(8):
                        bk = PSB[6 + h // 4]
                        MM(bk.a[:, (h % 4) * 128:(h % 4 + 1) * 128], wsT.a[:, h, :], vnb.a[:, h * 128:(h + 1) * 128],
                           r=[wsT, vnb], w=[bk])
                    for hb in range(2):
                        bk = PSB[6 + hb]
                        TT("dve", v3(f2.a[:, hb * 512:(hb + 1) * 512], 128), v3(bk.a[:, :], 128),
                           bT.a[:, hb * 4:(hb + 1) * 4].unsqueeze(2).to_broadcast([128, 4, 128]), ALU.add,
                           r=[bk, bT], w=[f2])
                    TT("dve", f2.a[:, :], f2.a[:, :], gug.a[:, j, :], ALU.mult, r=[f2, gug], w=[f2])
                    TT("dve", f1.a[:, :], f2.a[:, :], f2.a[:, :], ALU.mult, r=[f2], w=[f1])
                    RED(st8b.a[:, 24:32], v3(f1.a[:, :], 128), ALU.add, r=[f1], w=[st8b])
                    rstd_of(st8b.a[:, 24:32], 1.0 / 128, st8b.a[:, 32:40], r=[st8b], w=[st8b])
                    TT("dve", v3(f2.a[:, :], 128), v3(f2.a[:, :], 128),
                       st8b.a[:, 32:40].unsqueeze(2).to_broadcast([128, 8, 128]), ALU.mult, r=[f2, st8b], w=[f2])
                    TT("dve", ymix.a[:, 1024:2048], f2.a[:, :], ogr.a[:, :], ALU.mult, r=[f2, ogr], w=[ymix])
                    for mc in range(DC):
                        bk = PSB[mc // 8]
                        TR(psbf(mc // 8)[:, (mc % 8) * 128:(mc % 8 + 1) * 128], ymix.a[:, mc * 128:(mc + 1) * 128],
                           identb.a[:], r=[ymix, identb], w=[bk])
                    for hb in range(2):
                        bk = PSB[hb]
                        CP("act", ymT.a[:, hb * 8:(hb + 1) * 8, tk], v3(psbf(hb), 128), r=[bk], w=[ymT])
                for cg in range(4):
                    wnext = None
                    if cg + 1 < 4:
                        wnext = load_w(wo_src[:, :, (cg + 1) * 512:(cg + 2) * 512], 512)
                    for j in range(4):
                        tile = t0 + j
                        s = (cg * 4 + j) % 2
                        DMA("sp", xp[s].a[:, :], xsrc.ap()[tile * 128:(tile + 1) * 128, cg * 512:(cg + 1) * 512],
                            r=[dbuf(xsrc.name, tile)], w=[xp[s]])
                        bk = PSB[2 + j % 2]
                        for mc in range(DC):
                            MM(bk.a[:, :], ymT.a[:, mc, j * 128:(j + 1) * 128], wo.a[:, mc, :],
                               start=(mc == 0), stop=(mc == DC - 1), r=[ymT, wo], w=[bk])
                        TT("dve", xo[s].a[:, :], bk.a[:, :], GTR.a[:, cg * 512:(cg + 1) * 512], ALU.mult,
                           r=[bk, GTR], w=[xo[s]])
                        TT("pool", xo[s].a[:, :], xo[s].a[:, :], xp[s].a[:, :], ALU.add, r=[xo[s], xp[s]], w=[xo[s]])
                        DMA("sp", xdst.ap()[tile * 128:(tile + 1) * 128, cg * 512:(cg + 1) * 512], xo[s].a[:, :],
                            r=[xo[s]], w=[dbuf(xdst.name, tile)])
                    wo = wnext

        def peer_layer(l, xsrc, xdst):
            new_phase()
            ub = [AR.take([128, D], BF16) for _ in range(2)]
            uo = [AR.take([128, D], BF16) for _ in range(2)]
            vb_ = [AR.take([128, D], BF16) for _ in range(2)]
            wq_src = wq_d.ap()[l].rearrange("(dc p) c -> p dc c", p=128)
            wst = [AR.take([128, DC, 512], BF16) for _ in range(2)]
            for cg in range(4):
                DMA("pool", wst[cg % 2].a[:, :, :], wq_src[:, :, cg * 512:(cg + 1) * 512], w=[wst[cg % 2]])
                DMA("sp", wqb_d.ap()[cg], wst[cg % 2].a[:, :, :].rearrange("p a b -> p (a b)"),
                    r=[wst[cg % 2]], w=[dbuf("wqb", cg)])
            for i in range(128):
                s = i % 2
                DMA("pool", ub[s].a[:, :], pu_d.ap()[l, i * 128:(i + 1) * 128, :], w=[ub[s]])
                DMA("pool", vb_[s].a[:, :], pv_d.ap()[l, i * 128:(i + 1) * 128, :], w=[vb_[s]])
                DMA("sp", vb_d.ap()[i], vb_[s].a[:, :], r=[vb_[s]], w=[dbuf("vb", i)])
                for dc in range(DC):
                    bi = 4 + dc // 8 + 2 * s
                    TR(psbf(bi)[:, (dc % 8) * 128:(dc % 8 + 1) * 128],
                       ub[s].a[:, dc * 128:(dc + 1) * 128], identb.a[:], r=[ub[s], identb], w=[PSB[bi]])
                CP("act", uo[s].a[:, 0:1024], psbf(4 + 2 * s), r=[PSB[4 + 2 * s]], w=[uo[s]])
                CP("dve", uo[s].a[:, 1024:2048], psbf(5 + 2 * s), r=[PSB[5 + 2 * s]], w=[uo[s]])
                DMA("sp", ut_d.ap()[i], uo[s].a[:, :], r=[uo[s]], w=[dbuf("ut", i)])

            new_phase()
            k1T = AR.take([128, 8, 128])
            k2T = AR.take([128, 8, 128])
            h2T = AR.take([128, DC, 256], BF16)
            sc5T = AR.take([128, 5, 256])
            GA = AR.take([128, 128, 256], BF16)
            xblk = AR.take([128, 2, D])
            base = AR.off
            kin = X(xblk.a[:, 0, 0:1024].rearrange("p (a b) -> p a b", b=128))
            kin.b = xblk.b
            tr8(k1_d.ap()[l].rearrange("h k d -> k h d"), k1T, False, kin)
            tr8(k2_d.ap()[l].rearrange("h k d -> k h d"), k2T, False, kin)
            DMA("sp", GTR.a[:], modrow_d.ap()[l, 16:32, :].rearrange("a b -> (a b)").unsqueeze(0).to_broadcast([128, D]),
                r=[dbuf("modrow", l)], w=[GTR])

            for blk in range(NT // 2):
                tb0 = blk * 2
                P.fence()
                AR.off = base
                xnb = AR.take([128, D], BF16)
                junk = AR.take([128, D], BF16)
                wcq = [AR.take([128, DC, 512], BF16) for _ in range(2)]
                qT = AR.take([128, 16, 256])
                ssb = [AR.take([128, 1024]) for _ in range(2)]
                v12 = AR.take([128, 2, 8, 16])
                idx = AR.take([128, 8, 16], mybir.dt.uint32)
                tmpA = AR.take([128, 256])
                cand = AR.take([128, 8, 256])
                sc = AR.take([128, 8, 24])
                tm5 = AR.take([128, 4, 128])
                sm = AR.take([128, 64])
                ex16 = AR.take([128, 8, 16])
                for ts in range(2):
                    tile = tb0 + ts
                    DMA("sp", xblk.a[:, ts, :], xsrc.ap()[tile * 128:(tile + 1) * 128, :],
                        r=[dbuf(xsrc.name, tile)], w=[xblk])
                for ts in range(2):
                    norm_T(xblk.a[:, ts, :], xblk, 1, 48, h2T,
                           lambda dc, ts=ts: h2T.a[:, dc, ts * 128:(ts + 1) * 128], xnb, junk)
                def load_wq(cg):
                    DMA("sp", wcq[cg % 2].a[:, :, :].rearrange("p a b -> p (a b)"), wqb_d.ap()[cg],
                        r=[dbuf("wqb", cg)], w=[wcq[cg % 2]])
                load_wq(0)
                for cg in range(4):
                    if cg + 1 < 4:
                        load_wq(cg + 1)
                    wq_ = wcq[cg % 2]
                    for cc in range(4):
                        bk = PSB[2 + cc % 2]
                        half = (cc // 2 % 2) * 256
                        for dc in range(DC):
                            MM(bk.a[:, half:half + 256], wq_.a[:, dc, cc * 128:(cc + 1) * 128], h2T.a[:, dc, :],
                               start=(dc == 0), stop=(dc == DC - 1), r=[wq_, h2T], w=[bk])
                        CP("act", qT.a[:, cg * 4 + cc, :], bk.a[:, half:half + 256], r=[bk], w=[qT])
                for ts in range(2):
                    tile = tb0 + ts
                    tk = slice(ts * 128, (ts + 1) * 128)
                    for side in range(2):
                        kT = k1T if side == 0 else k2T
                        for h in range(8):
                            bk = PSB[4 + side * 2 + h // 4]
                            MM(bk.a[:, (h % 4) * 128:(h % 4 + 1) * 128], qT.a[:, 2 * h + side, tk], kT.a[:, h, :],
                               r=[qT, kT], w=[bk])
                        for hb in range(2):
                            bk = PSB[4 + side * 2 + hb]
                            CP("act", ssb[side].a[:, hb * 512:(hb + 1) * 512], bk.a[:, :], r=[bk], w=[ssb[side]])
                        if side == 1:
                            DMA("sp", s2_d.ap()[tile * 128:(tile + 1) * 128],
                                ssb[1].a[:, :].unsqueeze(1).to_broadcast([128, 16, 1024]),
                                r=[ssb[1]], w=[dbuf("s2", tile)])
                        for h in range(8):
                            sv = ssb[side].a[:, h * 128:(h + 1) * 128]
                            MAX8(v12.a[:, side, h, 0:8], sv, r=[ssb[side]], w=[v12])
                            if side == 0:
                                MIDX(idx.a[:, h, 0:8], v12.a[:, 0, h, 0:8], sv, r=[ssb[0], v12], w=[idx])
                            MREP(tmpA.a[:, 0:128], v12.a[:, side, h, 0:8], sv, r=[ssb[side], v12], w=[tmpA])
                            MAX8(v12.a[:, side, h, 8:16], tmpA.a[:, 0:128], r=[tmpA], w=[v12])
                            if side == 0:
                                MIDX(idx.a[:, h, 8:16], v12.a[:, 0, h, 8:16], tmpA.a[:, 0:128], r=[tmpA, v12], w=[idx])
                    TT("dve", cand.a[:, :, :].rearrange("p h (a b) -> p h a b", b=16),
                       v12.a[:, 0, :, :].unsqueeze(3).to_broadcast([128, 8, 16, 16]),
                       v12.a[:, 1, :, :].unsqueeze(2).to_broadcast([128, 8, 16, 16]), ALU.add, r=[v12], w=[cand])
                    for h in range(8):
                        MAX8(sc.a[:, h, 0:8], cand.a[:, h, :], r=[cand], w=[sc])
                        MREP(tmpA.a[:, :], sc.a[:, h, 0:8], cand.a[:, h, :], r=[cand, sc], w=[tmpA])
                        MAX8(sc.a[:, h, 8:16], tmpA.a[:, :], r=[tmpA], w=[sc])
                        MREP(tmpA.a[:, :], sc.a[:, h, 8:16], tmpA.a[:, :], r=[tmpA, sc], w=[tmpA])
                        MAX8(sc.a[:, h, 16:24], tmpA.a[:, :], r=[tmpA], w=[sc])
                    TT("dve", sm.a[:, 0:8], sc.a[:, :, 15], sc.a[:, :, 16], ALU.add, r=[sc], w=[sm])
                    TS("dve", sm.a[:, 0:8], sm.a[:, 0:8], 0.5, None, ALU.mult, r=[sm], w=[sm])
                    TT("dve", ex16.a[:, :, :], sc.a[:, :, 0:16], sc.a[:, :, 0:1].to_broadcast([128, 8, 16]),
                       ALU.subtract, r=[sc], w=[ex16])
                    ACT(ex16.a[:, :, :], ex16.a[:, :, :], AF.Exp, r=[ex16], w=[ex16])
                    RED(sm.a[:, 8:16], ex16.a[:, :, :], ALU.add, r=[ex16], w=[sm])
                    RECIP(sm.a[:, 16:24], sm.a[:, 8:16], r=[sm], w=[sm])
                    v1v = v12.a[:, 0, :, :]
                    t4 = lambda k: tm5.a[:, k, :].rearrange("p (a h) -> p h a", h=8)
                    CP("dve", t4(0), idx.a[:, :, :], r=[idx], w=[tm5])
                    TT("dve", t4(1), v1v, v12.a[:, 0, :, 0:1].to_broadcast([128, 8, 16]), ALU.subtract,
                       r=[v12], w=[tm5])
                    ACT(t4(1), t4(1), AF.Exp, r=[tm5], w=[tm5])
                    TT("dve", t4(1), t4(1), sm.a[:, 16:24].unsqueeze(2).to_broadcast([128, 8, 16]), ALU.mult,
                       r=[tm5, sm], w=[tm5])
                    TT("dve", t4(2), sm.a[:, 0:8].unsqueeze(2).to_broadcast([128, 8, 16]), v1v, ALU.subtract,
                       r=[sm, v12], w=[tm5])
                    TS("dve", t4(3), v12.a[:, 1, :, 0:1].to_broadcast([128, 8, 16]), -1.0, None, ALU.mult,
                       r=[v12], w=[tm5])
                    for k in range(4):
                        TR(PSB[0].a[:, k * 128:(k + 1) * 128], tm5.a[:, k, :], ident, r=[tm5, cst], w=[PSB[0]])
                    CP("act", sc5T.a[:, 0:4, tk], v3(PSB[0].a[:, :], 128), r=[PSB[0]], w=[sc5T])
                P.fence()
                AR.off = base
                NSR = 3
                SR = [AR.take([128, 16, 128]) for _ in range(NSR)]
                Pt = [AR.take([128, 128], BF16) for _ in range(4)]
                Et = [AR.take([128, 128]) for _ in range(4)]
                Rt = [AR.take([128, 128], BF16) for _ in range(4)]

                def load_sr(bi):
                    sl = bi % NSR
                    tok0 = tb0 * 128 + bi * 16
                    tile = tok0 // 128
                    src = bass.AP(s2_d, tok0 * 16384, [[128, 128], [16384, 16], [1, 128]])
                    DMA("sp", SR[sl].a[:, :, :], src, r=[dbuf("s2", tile)], w=[SR[sl]])
                load_sr(0)
                load_sr(1)
                for bi in range(0 if opts.get('skipB') else 16):
                    if bi + 2 < 16:
                        load_sr(bi + 2)
                    sl = bi % NSR
                    for tt in range(16):
                        t = bi * 16 + tt
                        k2 = t % 4
                        col = lambda k, t=t: sc5T.a[:, k, t:t + 1]
                        TS("dve", Pt[k2].a[:, :], iota, col(0), col(1), ALU.is_equal, ALU.mult,
                           r=[cst, sc5T], w=[Pt[k2]])
                        ACT(Et[k2].a[:, :], SR[sl].a[:, tt, :], AF.Exp, bias=col(3), scale=1.0,
                            r=[SR[sl], sc5T], w=[Et[k2]])
                        STT(Rt[k2].a[:, :], SR[sl].a[:, tt, :], col(2), Et[k2].a[:, :], ALU.is_ge, ALU.mult,
                            r=[SR[sl], sc5T, Et[k2]], w=[Rt[k2]])
                        bk = PSB[4 + (t // 4) % 4]
                        MM(bk.a[:, (t % 4) * 128:(t % 4 + 1) * 128], Rt[k2].a[:, :], Pt[k2].a[:, :],
                           r=[Rt[k2], Pt[k2]], w=[bk])
                        if t % 4 == 3 and t >= 11:
                            te = t - 8
                            bke = PSB[4 + (te // 4) % 4]
                            CP("act", GA.a[:, :, te - 3:te + 1], bke.a[:, :].rearrange("p (t i) -> p i t", i=128),
                               r=[bke], w=[GA])
                if not opts.get('skipB'):
                    for te in (247, 251, 255):
                        bke = PSB[4 + (te // 4) % 4]
                        CP("act", GA.a[:, :, te - 3:te + 1], bke.a[:, :].rearrange("p (t i) -> p i t", i=128),
                           r=[bke], w=[GA])
                P.fence()
                AR.off = base
                NUT, NVV = 3, 4
                gz = [AR.take([128, 256], BF16) for _ in range(4)]
                utb = [AR.take([128, 2, DC * 128], BF16) for _ in range(NUT)]
                vvb = [AR.take([128, 2, 1024], BF16) for _ in range(NVV)]
                yo = [AR.take([128, 1024]) for _ in range(2)]
                ZS = [X(PSB[4 + k].a[:, 0:256]) for k in range(4)]

                def load_ut(pi):
                    DMA("sp", utb[pi % NUT].a[:, :, :],
                        ut_d.ap()[2 * pi:2 * pi + 2].rearrange("i p f -> p i f"),
                        r=[dbuf("ut", 2 * pi), dbuf("ut", 2 * pi + 1)], w=[utb[pi % NUT]])
                if not opts.get('skipC'):
                    load_ut(0)
                    load_ut(1)
                for i in range(0 if opts.get('skipC') else 128):
                    if i % 2 == 0 and i // 2 + 2 < 64:
                        load_ut(i // 2 + 2)
                    ub_ = utb[(i // 2) % NUT]
                    zs = ZS[i % 4]
                    for dc in range(DC):
                        MM(zs.a, ub_.a[:, i % 2, dc * 128:(dc + 1) * 128], h2T.a[:, dc, :],
                           start=(dc == 0), stop=(dc == DC - 1), r=[ub_, h2T], w=[zs])
                    ACT(gz[i % 4].a[:, :], zs.a, AF.Gelu_apprx_tanh, r=[zs], w=[gz[i % 4]])
                    TT("dve", GA.a[:, i, :], gz[i % 4].a[:, :], GA.a[:, i, :], ALU.mult, r=[gz[i % 4], GA], w=[GA])
                for dh in range(0 if opts.get('skipC') else 2):
                    def load_v(pi, dh=dh):
                        DMA("sp", vvb[pi % NVV].a[:, :, :],
                            vb_d.ap()[2 * pi:2 * pi + 2, :, dh * 1024:(dh + 1) * 1024].rearrange("i p f -> p i f"),
                            r=[dbuf("vb", 2 * pi), dbuf("vb", 2 * pi + 1)], w=[vvb[pi % NVV]])
                    load_v(0)
                    load_v(1)
                    load_v(2)
                    for i in range(128):
                        if i % 2 == 0 and i // 2 + 3 < 64:
                            load_v(i // 2 + 3)
                        vv_ = vvb[(i // 2) % NVV]
                        for ts in range(2):
                            for cq in range(2):
                                bk = PSB[ts * 2 + cq]
                                MM(bk.a[:, :], GA.a[:, i, ts * 128:(ts + 1) * 128],
                                   vv_.a[:, i % 2, cq * 512:(cq + 1) * 512],
                                   start=(i == 0), stop=(i == 127), r=[GA, vv_], w=[bk])
                    for ts in range(2):
                        tile = tb0 + ts
                        y_ = yo[ts]
                        for cq in range(2):
                            bk = PSB[ts * 2 + cq]
                            c0 = dh * 1024 + cq * 512
                            TT("dve", y_.a[:, cq * 512:(cq + 1) * 512], bk.a[:, :], GTR.a[:, c0:c0 + 512], ALU.mult,
                               r=[bk, GTR], w=[y_])
                        TT("pool", y_.a[:, :], y_.a[:, :], xblk.a[:, ts, dh * 1024:(dh + 1) * 1024], ALU.add,
                           r=[y_, xblk], w=[y_])
                        DMA("sp", xdst.ap()[tile * 128:(tile + 1) * 128, dh * 1024:(dh + 1) * 1024], y_.a[:, :],
                            r=[y_], w=[dbuf(xdst.name, tile)])

        def tail(xsrc, norm):
            new_phase()
            xt = [AR.take([128, D]) for _ in range(2)]
            junk = AR.take([128, D], BF16)
            if norm:
                DMA("sp", GTR.a[:], fg_d.ap().to_broadcast([128, D]), w=[GTR])
            for tile in range(NT):
                s = tile % 2
                DMA("sp", xt[s].a[:, :], xsrc.ap()[tile * 128:(tile + 1) * 128, :], r=[dbuf(xsrc.name, tile)], w=[xt[s]])
                if norm:
                    ACT(junk.a[:, :], xt[s].a[:, :], AF.Square, accum=st8.a[:, 0:1], r=[xt[s]], w=[junk, st8])
                    rstd_of(st8.a[:, 0:1], 1.0 / D, st8.a[:, 1:2], r=[st8], w=[st8])
                    STT(xt[s].a[:, :], xt[s].a[:, :], st8.a[:, 1:2], GTR.a[:], ALU.mult, ALU.mult,
                        r=[xt[s], st8, GTR], w=[xt[s]])
                DMA("sp", out_d.ap()[tile * 128:(tile + 1) * 128, :], xt[s].a[:, :], r=[xt[s]], w=[dbuf("out", tile)])

        done = False
        cur = x_d
        for l in range(NL):
            setup_layer(l)
            mixer_layer(l, cur, xa_d)
            if stop_after == ("mixer", l):
                tail(xa_d, False)
                done = True
                break
            peer_layer(l, xa_d, xb_d)
            cur = xb_d
            if stop_after == ("peer", l):
                tail(xb_d, False)
                done = True
                break
        if not done:
            tail(cur, True)
        P.finish("sp")
        P.emit()
    return nc


_CONSTS = None


def _consts():
    global _CONSTS
    if _CONSTS is None:
        c = np.zeros((128, 5, 128), np.float32)
        c[:, 4, :] = np.arange(128, dtype=np.float32)[None, :]
        c[:, 0, :] = np.eye(128, dtype=np.float32)
        tri = np.triu(np.ones((128, 128), np.float32))
        c[:, 1, :] = tri
        c[:, 2, :] = -tri / 16.0
        c[:, 3, :] = -(1.0 - tri) / 16.0
        _CONSTS = c
    return _CONSTS


def make_in_maps(inputs, ncores, ntok):
    f = lambda a: np.ascontiguousarray(np.asarray(a, dtype=np.float32))
    shared = {
        "ada_w": f(inputs["ada_w"]),
        "ada_b": f(inputs["ada_b"]).reshape(DEPTH, 96, 128),
        "norm1_g": f(inputs["norm1_g"]).reshape(DEPTH, 16, 128),
        "w_in": f(inputs["w_in"]),
        "gla_w_a2": f(inputs["gla_w_a2"]),
        "gla_b_a": f(inputs["gla_b_a"]).reshape(DEPTH, 1, 512),
        "gla_norm_g": f(inputs["gla_norm_g"]).reshape(DEPTH, 1, 1024),
        "gmlp_vnorm_g": f(inputs["gmlp_vnorm_g"]).reshape(DEPTH, 1, 1024),
        "gmlp_ws": f(inputs["gmlp_ws"]),
        "gmlp_b": f(inputs["gmlp_b"]),
        "gmlp_out_g": f(inputs["gmlp_out_g"]).reshape(DEPTH, 1, 1024),
        "w_out": f(inputs["w_out"]),
        "norm2_g": f(inputs["norm2_g"]).reshape(DEPTH, 16, 128),
        "peer_wq": f(inputs["peer_wq"]),
        "peer_k1": f(inputs["peer_k1"]),
        "peer_k2": f(inputs["peer_k2"]),
        "peer_u": f(inputs["peer_u"]),
        "peer_v": f(inputs["peer_v"]),
        "final_g": f(inputs["final_g"]).reshape(1, D),
        "consts": _consts(),
    }
    x = f(inputs["x"])
    c = f(inputs["c"])
    maps = []
    for b in range(ncores):
        m = dict(shared)
        m["x"] = np.ascontiguousarray(x[b, :ntok, :])
        m["c"] = np.ascontiguousarray(c[b].reshape(16, 128))
        maps.append(m)
    return maps


_NC_CACHE = {}


def kernel(**inputs):
    key = "full"
    if key not in _NC_CACHE:
        _NC_CACHE[key] = build(NT=SEQ // 128, NL=DEPTH)
    nc = _NC_CACHE[key]
    in_maps = make_in_maps(inputs, BATCH, SEQ)
    res = run_bass_kernel_spmd(nc, in_maps, core_ids=list(range(BATCH)))
    out = np.stack([np.asarray(res.results[b]["out"], dtype=np.float32) for b in range(BATCH)], axis=0)
    return out
```

```python
import numpy as np
from contextlib import ExitStack
import concourse.bass as bass
import concourse.mybir as mybir
from concourse.bass_utils import run_bass_kernel_spmd

F32 = mybir.dt.float32
BF16 = mybir.dt.bfloat16
AF = mybir.ActivationFunctionType
ALU = mybir.AluOpType
AX = mybir.AxisListType

D = 2048
DC = 16
SEQ = 4096
BATCH = 4
DEPTH = 2
IN_COLS = 5136
NEXP = 16384
EPS = 1e-6
C_Q, C_K, C_V, C_R, C_AL, C_U, C_VS = 0, 512, 1024, 2048, 3072, 3088, 4112
NEG = -1.0e30


class Buf:
    __slots__ = ("lw", "rd")

    def __init__(self):
        self.lw = None
        self.rd = {}


class X:
    def __init__(self, t):
        self.a = t
        self.b = Buf()


def _b(x):
    return x.b if isinstance(x, X) else x


class Prog:
    ENGS = ("sp", "act", "dve", "pool", "pe")
    NDMA = 12

    def __init__(self, nc, self_wait=True):
        self.nc = nc
        self.h = {"sp": nc.sync, "act": nc.scalar, "dve": nc.vector,
                  "pool": nc.gpsimd, "pe": nc.tensor}
        self.ops = {e: [] for e in self.ENGS}
        self.cnt = {e: 0 for e in self.ENGS}
        self.waited = {e: {} for e in self.ENGS}
        self.dma_next = {e: 0 for e in self.ENGS}
        self.dma_val = {}
        self.self_wait = self_wait
        self.sems = {}

    def op(self, eng, fn, reads=(), writes=(), dma=False):
        waits = {}

        def need(kv):
            if kv is None:
                return
            k, v = kv
            if v > 0 and waits.get(k, 0) < v:
                waits[k] = v
        reads = [_b(x) for x in reads]
        writes = [_b(x) for x in writes]
        for b in reads:
            need(b.lw)
        for b in writes:
            need(b.lw)
            for k, v in b.rd.items():
                need((k, v))
        if dma:
            slot = self.dma_next[eng]
            self.dma_next[eng] = (slot + 1) % self.NDMA
            key = ("dma", eng, slot)
            prev = self.dma_val.get(key, 0)
            need((key, prev))
            myval = prev + 16
            self.dma_val[key] = myval
        else:
            key = ("eng", eng)
            self.cnt[eng] += 1
            myval = self.cnt[eng]
        fw = []
        for k, v in waits.items():
            if k == ("eng", eng) and (eng == "pe" or not self.self_wait):
                continue
            if self.waited[eng].get(k, 0) >= v:
                continue
            self.waited[eng][k] = v
            fw.append((k, v))
        self.ops[eng].append((fw, fn, key, 16 if dma else 1))
        for b in reads:
            if b.rd.get(key, 0) < myval:
                b.rd[key] = myval
        for b in writes:
            b.lw = (key, myval)
            b.rd = {}

    def fence(self):
        for e in self.ENGS:
            fw = []
            for key, v in self.dma_val.items():
                if self.waited[e].get(key, 0) < v:
                    self.waited[e][key] = v
                    fw.append((key, v))
            for e2 in self.ENGS:
                k = ("eng", e2)
                if self.cnt[e2] > 0 and self.waited[e].get(k, 0) < self.cnt[e2]:
                    self.waited[e][k] = self.cnt[e2]
                    fw.append((k, self.cnt[e2]))
            if fw:
                self.ops[e].append((fw, None, None, 0))

    def finish(self, eng="sp"):
        fw = []
        for key, v in self.dma_val.items():
            if self.waited[eng].get(key, 0) < v:
                fw.append((key, v))
        for e in self.ENGS:
            if e != eng and self.cnt[e] > 0:
                fw.append((("eng", e), self.cnt[e]))
        self.ops[eng].append((fw, None, None, 0))

    def emit(self):
        nc = self.nc
        keys = set()
        for e in self.ENGS:
            for fw, fn, key, inc in self.ops[e]:
                if key is not None:
                    keys.add(key)
                for k, v in fw:
                    keys.add(k)
        with ExitStack() as st:
            for k in sorted(keys):
                self.sems[k] = st.enter_context(
                    nc.semaphore("s_" + "_".join(str(x) for x in k)))
            block = st.enter_context(nc.Block())

            def run(e):
                h = self.h[e]
                for fw, fn, key, inc in self.ops[e]:
                    for k, v in fw:
                        h.wait_ge(self.sems[k], v)
                    if fn is not None:
                        fn().then_inc(self.sems[key], inc)

            @block.sync
            def _(x):
                run("sp")

            @block.scalar
            def _(x):
                run("act")

            @block.vector
            def _(x):
                run("dve")

            @block.gpsimd
            def _(x):
                run("pool")

            @block.tensor
            def _(x):
                run("pe")


class Arena:
    def __init__(self, t, nwords):
        self.t = t
        self.n = nwords
        self.off = 0

    def reset(self):
        self.off = 0

    def take(self, shape, dt=F32, parts=128):
        n = 1
        for s in shape[1:]:
            n *= s
        four = dt in (F32, mybir.dt.uint32, mybir.dt.int32)
        words = n if four else (n + 1) // 2
        words = (words + 15) // 16 * 16
        assert self.off + words <= self.n, ("arena overflow", self.off, words, self.n)
        ap = self.t[0:shape[0], self.off:self.off + words]
        self.off += words
        if dt != F32:
            ap = ap.bitcast(dt)
        ap = ap[:, 0:n]
        if len(shape) == 3:
            ap = ap.rearrange("p (a b) -> p a b", b=shape[2])
        elif len(shape) == 4:
            ap = ap.rearrange("p (a b c) -> p a b c", b=shape[2], c=shape[3])
        return X(ap)


def build(NT=32, NL=DEPTH, stop_after=None, opts=None, split=False):
    opts = opts or {}
    HALF = NT // 2 if split else 0
    NTOK = NT * 128
    assert NT % 4 == 0 and HALF % 4 == 0
    nc = bass.Bass("TRN2", target_bir_lowering=False)
    P = Prog(nc, self_wait=bool(opts.get('self_wait', True)))

    def din(name, shape):
        return nc.dram_tensor(name, shape, F32, kind="ExternalInput")

    x_d = din("x", [NTOK, D])
    c_d = din("c", [16, 128])
    adaw_d = din("ada_w", [DEPTH, D, 6 * D])
    adab_d = din("ada_b", [DEPTH, 96, 128])
    n1g_d = din("norm1_g", [DEPTH, 16, 128])
    win_d = din("w_in", [DEPTH, D, IN_COLS])
    wa2_d = din("gla_w_a2", [DEPTH, 16, 512])
    ba_d = din("gla_b_a", [DEPTH, 1, 512])
    gng_d = din("gla_norm_g", [DEPTH, 1, 1024])
    vng_d = din("gmlp_vnorm_g", [DEPTH, 1, 1024])
    ws_d = din("gmlp_ws", [DEPTH, 8, 128, 128])
    gb_d = din("gmlp_b", [DEPTH, 8, 128])
    og_d = din("gmlp_out_g", [DEPTH, 1, 1024])
    wout_d = din("w_out", [DEPTH, D, D])
    n2g_d = din("norm2_g", [DEPTH, 16, 128])
    wq_d = din("peer_wq", [DEPTH, D, D])
    k1_d = din("peer_k1", [DEPTH, 8, 128, 128])
    k2_d = din("peer_k2", [DEPTH, 8, 128, 128])
    pu_d = din("peer_u", [DEPTH, NEXP, D])
    pv_d = din("peer_v", [DEPTH, NEXP, D])
    fg_d = din("final_g", [1, D])
    cst_d = din("consts", [128, 5, 128])
    flag_d = din("flag", [128, 16])
    out_d = nc.dram_tensor("out", [NTOK - HALF * 128, D], F32, kind="ExternalOutput")

    def dscr(name, shape, dt=F32):
        return nc.dram_tensor(name, shape, dt, kind="Internal")

    xa_d = dscr("xa", [NTOK, D])
    xb_d = dscr("xb", [NTOK, D])
    modrow_d = dscr("modrow", [DEPTH, 32, 128])
    s2_d = dscr("s2rep", [NTOK, 16, 1024])
    wqb_d = dscr("wqb", [4, 128, DC * 512], BF16)
    winb_d = dscr("winb", [128, DC, IN_COLS], BF16)
    woutb_d = dscr("woutb", [128, DC, D], BF16)
    ut_d = dscr("uts", [128, 128, DC * 128], BF16)
    vb_d = dscr("vbs", [128, 128, D], BF16)

    db = {}

    def dbuf(*key):
        if key not in db:
            db[key] = Buf()
        return db[key]

    st = ExitStack()
    with st:
        def SB(name, shape, dt=F32):
            return X(st.enter_context(nc.sbuf_tensor(name, shape, dt)))

        def DMA(q, out, in_, r=(), w=()):
            h = P.h[q]
            P.op(q, lambda: h.dma_start(out=out, in_=in_), reads=r, writes=w, dma=True)

        def MM(out, lhsT, rhs, start=True, stop=True, r=(), w=()):
            P.op("pe", lambda: nc.tensor.matmul(out, lhsT=lhsT, rhs=rhs, start=start, stop=stop),
                 reads=r, writes=w)

        def TR(out, in_, ident, r=(), w=()):
            P.op("pe", lambda: nc.tensor.transpose(out=out, in_=in_, identity=ident),
                 reads=r, writes=w)

        def ACT(out, in_, func, bias=None, scale=None, accum=None, r=(), w=()):
            kw = {}
            if bias is not None:
                kw["bias"] = bias
            if scale is not None:
                kw["scale"] = scale
            if accum is not None:
                kw["accum_out"] = accum
            P.op("act", lambda: nc.scalar.activation(out=out, in_=in_, func=func, **kw),
                 reads=r, writes=w)

        def TS(e, out, in0, s1, s2, op0, op1=None, r=(), w=()):
            h = P.h[e]
            if op1 is None:
                P.op(e, lambda: h.tensor_scalar(out=out, in0=in0, scalar1=s1, scalar2=None, op0=op0),
                     reads=r, writes=w)
            else:
                P.op(e, lambda: h.tensor_scalar(out=out, in0=in0, scalar1=s1, scalar2=s2, op0=op0, op1=op1),
                     reads=r, writes=w)

        def TT(e, out, in0, in1, op, r=(), w=()):
            h = P.h[e]
            P.op(e, lambda: h.tensor_tensor(out=out, in0=in0, in1=in1, op=op), reads=r, writes=w)

        def STT(out, in0, scalar, in1, op0, op1, r=(), w=()):
            P.op("dve", lambda: nc.vector.scalar_tensor_tensor(out=out, in0=in0, scalar=scalar, in1=in1,
                                                               op0=op0, op1=op1), reads=r, writes=w)

        def CP(e, out, in_, r=(), w=()):
            h = P.h[e]
            if e == "act":
                P.op(e, lambda: nc.scalar.copy(out=out, in_=in_), reads=r, writes=w)
            else:
                P.op(e, lambda: h.tensor_copy(out=out, in_=in_), reads=r, writes=w)

        def MSET(e, out, val, w=()):
            h = P.h[e]
            P.op(e, lambda: h.memset(out, val), writes=w)

        def RED(out, in_, op, r=(), w=()):
            P.op("dve", lambda: nc.vector.tensor_reduce(out=out, in_=in_, axis=AX.X, op=op), reads=r, writes=w)

        def MAX8(out, in_, r=(), w=()):
            P.op("dve", lambda: nc.vector.max(out=out, in_=in_), reads=r, writes=w)

        def MIDX(out, mx, vals, r=(), w=()):
            P.op("dve", lambda: nc.vector.max_index(out=out, in_max=mx, in_values=vals), reads=r, writes=w)

        def MREP(out, rep, vals, r=(), w=()):
            P.op("dve", lambda: nc.vector.match_replace(out=out, in_to_replace=rep, in_values=vals,
                                                        imm_value=NEG), reads=r, writes=w)

        def RECIP(out, in_, r=(), w=()):
            P.op("dve", lambda: nc.vector.reciprocal(out=out, in_=in_), reads=r, writes=w)

        def v3(ap, b):
            return ap.rearrange("p (a b) -> p a b", b=b)

        PSB = [X(st.enter_context(nc.psum_tensor("psb%d" % i, [128, 512], F32))) for i in range(8)]

        def psbf(i):
            return PSB[i].a[:, :].bitcast(BF16)

        cst = SB("cst", [128, 5, 128])
        identb = SB("identb", [128, 128], BF16)
        epsc = SB("epsc", [128, 1])
        onec = SB("onec", [128, 1])
        smallT = SB("smallT", [128, 128])
        condT = SB("condT", [128, 16, 2])
        modT = SB("modT", [128, 96])
        AB = SB("AB", [128, 2, 16])
        GTR = SB("GTR", [128, D])
        st8 = SB("st8", [128, 64])
        st8b = SB("st8b", [128, 64])
        flag = SB("flag_sb", [128, 16])
        ARW = 48128
        AR = Arena(st.enter_context(nc.sbuf_tensor("arena", [128, ARW], F32)), ARW)

        def new_phase():
            P.fence()
            AR.reset()

        ident = cst.a[:, 0, :]
        M_G = cst.a[:, 2, :]
        M_D = cst.a[:, 3, :]
        iota = cst.a[:, 4, :]

        def rstd_of(ss, inv_n, tmp, r, w):
            ACT(tmp, ss, AF.Sqrt, bias=epsc.a[:, 0:1], scale=inv_n, r=r + [epsc], w=w)
            RECIP(tmp, tmp, r=w, w=w)

        stage = AR.take([128, 128])
        DMA("sp", cst.a[:], cst_d.ap(), w=[cst])
        DMA("sp", flag.a[:], flag_d.ap(), w=[flag])
        CP("dve", identb.a[:], ident, r=[cst], w=[identb])
        MSET("dve", epsc.a[:], EPS, w=[epsc])
        MSET("dve", onec.a[:], 1.0, w=[onec])
        DMA("sp", stage.a[0:16, :], c_d.ap(), w=[stage])
        TR(PSB[0].a[:, 0:16], stage.a[0:16, :], cst.a[0:16, 0, 0:16], r=[stage, cst], w=[PSB[0]])
        ACT(condT.a[:, :, 0], PSB[0].a[:, 0:16], AF.Silu, r=[PSB[0]], w=[condT])
        ACT(condT.a[:, :, 1], PSB[0].a[:, 0:16], AF.Silu, r=[PSB[0]], w=[condT])

        def setup_layer(l):
            new_phase()
            stage = AR.take([128, 128])
            gtT = AR.take([128, 32])
            gtrow = AR.take([32, 128])
            aw = [AR.take([128, DC, 256]) for _ in range(2)]
            DMA("sp", stage.a[0:96, :], adab_d.ap()[l], w=[stage])
            DMA("sp", stage.a[96:112, :], n1g_d.ap()[l], w=[stage])
            DMA("sp", stage.a[112:128, :], n2g_d.ap()[l], w=[stage])
            TR(PSB[0].a[:, 0:128], stage.a[:, :], ident, r=[stage, cst], w=[PSB[0]])
            CP("dve", smallT.a[:], PSB[0].a[:, 0:128], r=[PSB[0]], w=[smallT])
            src = adaw_d.ap()[l].rearrange("(dc p) c -> p dc c", p=128)
            for cc in range(48):
                t = aw[cc % 2]
                DMA("sp" if cc % 2 == 0 else "act", t.a, src[:, :, cc * 256:(cc + 1) * 256], w=[t])
                for mm in range(2):
                    m = cc * 2 + mm
                    for dc in range(DC):
                        MM(PSB[1].a[:, 2 * m:2 * m + 2], t.a[:, dc, mm * 128:(mm + 1) * 128],
                           condT.a[:, dc, :], start=(dc == 0), stop=(dc == DC - 1),
                           r=[t, condT], w=[PSB[1]])
            TT("dve", modT.a[:], v3(PSB[1].a[:, 0:192], 2)[:, :, 0], smallT.a[:, 0:96], ALU.add,
               r=[PSB[1], smallT], w=[modT])
            STT(AB.a[:, 0, :], modT.a[:, 16:32], 1.0, smallT.a[:, 96:112], ALU.add, ALU.mult,
                r=[modT, smallT], w=[AB])
            STT(AB.a[:, 1, :], modT.a[:, 64:80], 1.0, smallT.a[:, 112:128], ALU.add, ALU.mult,
                r=[modT, smallT], w=[AB])
            CP("dve", gtT.a[:, 0:16], modT.a[:, 32:48], r=[modT], w=[gtT])
            CP("dve", gtT.a[:, 16:32], modT.a[:, 80:96], r=[modT], w=[gtT])
            TR(PSB[0].a[0:32, 0:128], gtT.a[:, :], ident, r=[gtT, cst], w=[PSB[0]])
            CP("dve", gtrow.a[:, :], PSB[0].a[0:32, 0:128], r=[PSB[0]], w=[gtrow])
            DMA("sp", modrow_d.ap()[l], gtrow.a[:, :], r=[gtrow], w=[dbuf("modrow", l)])
            wst = [AR.take([128, DC, 512], BF16) for _ in range(2)]
            win_v = win_d.ap()[l].rearrange("(dc p) c -> p dc c", p=128)
            wout_v = wout_d.ap()[l].rearrange("(mc p) c -> p mc c", p=128)
            jobs = [(win_v, winb_d, c0, min(512, IN_COLS - c0)) for c0 in range(0, IN_COLS, 512)]
            jobs += [(wout_v, woutb_d, c0, 512) for c0 in range(0, D, 512)]
            for k, (sv_, dd, c0, n) in enumerate(jobs):
                t = wst[k % 2]
                DMA("pool", t.a[:, :, 0:n], sv_[:, :, c0:c0 + n], w=[t])
                DMA("sp", dd.ap()[:, :, c0:c0 + n], t.a[:, :, 0:n], r=[t], w=[dbuf(dd.name)])

        def tr8(srcd_ap, dst, masked, kin):
            DMA("sp", kin.a, srcd_ap, w=[kin])
            for h in range(8):
                bk = PSB[2 + h // 4]
                TR(bk.a[:, (h % 4) * 128:(h % 4 + 1) * 128], kin.a[:, h, :], ident, r=[kin, cst], w=[bk])
            for hb in range(2):
                bk = PSB[2 + hb]
                if masked:
                    TT("dve", dst.a[:, hb * 4:(hb + 1) * 4, :], v3(bk.a[:, :], 128),
                       cst.a[:, 1:2, :].to_broadcast([128, 4, 128]), ALU.mult, r=[bk, cst], w=[dst])
                else:
                    CP("dve", dst.a[:, hb * 4:(hb + 1) * 4, :], v3(bk.a[:, :], 128), r=[bk], w=[dst])

        def norm_T(x_ap, xdep, which, bshift, dst, dst_fn, xnb, junk):
            ACT(junk.a[:, :], x_ap, AF.Square, accum=st8.a[:, 0:1], r=[xdep], w=[junk, st8])
            rstd_of(st8.a[:, 0:1], 1.0 / D, st8.a[:, 1:2], r=[st8], w=[st8])
            TS("dve", xnb.a[:, :], x_ap, st8.a[:, 1:2], None, ALU.mult, r=[xdep, st8], w=[xnb])
            for dc in range(DC):
                bk = PSB[dc // 8]
                TR(psbf(dc // 8)[:, (dc % 8) * 128:(dc % 8 + 1) * 128], xnb.a[:, dc * 128:(dc + 1) * 128],
                   identb.a[:], r=[xnb, identb], w=[bk])
            for dc in range(DC):
                bk = PSB[dc // 8]
                src = psbf(dc // 8)[:, (dc % 8) * 128:(dc % 8 + 1) * 128]
                if dc // 8 == 0:
                    TS("dve", dst_fn(dc), src, AB.a[:, which, dc:dc + 1],
                       modT.a[:, bshift + dc:bshift + dc + 1], ALU.mult, ALU.add,
                       r=[bk, AB, modT], w=[dst])
                else:
                    ACT(dst_fn(dc), src, AF.Identity, bias=modT.a[:, bshift + dc:bshift + dc + 1],
                        scale=AB.a[:, which, dc:dc + 1], r=[bk, AB, modT], w=[dst])

        def mixer_layer(l, xsrc, xdst):
            new_phase()
            ngr = AR.take([128, 1024])
            vgr = AR.take([128, 1024])
            ogr = AR.take([128, 1024])
            W2a = AR.take([17, 512])
            wsT = AR.take([128, 8, 128], BF16)
            bT = AR.take([128, 8])
            S32 = AR.take([128, 4, 256])
            Sbf = AR.take([128, 4, 256], BF16)
            xt = [AR.take([128, D]) for _ in range(2)]
            xnb = AR.take([128, D], BF16)
            junk = AR.take([128, D], BF16)
            wc = [AR.take([128, DC, 512], BF16) for _ in range(2)]
            hTg = AR.take([128, DC, 512], BF16)
            ymT = hTg
            alT = AR.take([17, 512])
            GTg = AR.take([128, 4, 4, 128])
            EGl = AR.take([128, 4, 4])
            EDg = AR.take([128, 4, 512], BF16)
            qtg = AR.take([128, 4, 512], BF16)
            ktg = AR.take([128, 4, 512], BF16)
            kdg = AR.take([128, 4, 512], BF16)
            vg_ = AR.take([128, 4, 1024], BF16)
            srg = AR.take([128, 4, 1024], BF16)
            gug = AR.take([128, 4, 1024], BF16)
            gvg = AR.take([128, 4, 1024], BF16)
            lt = AR.take([128, 512])
            et = AR.take([128, 512])
            attm = AR.take([128, 4, 128], BF16)
            ymix = AR.take([128, D], BF16)
            f1 = AR.take([128, 1024])
            f2 = AR.take([128, 1024])
            vnb = AR.take([128, 1024], BF16)
            xp = [AR.take([128, 512]) for _ in range(2)]
            xo = [AR.take([128, 512]) for _ in range(2)]
            stage = AR.take([8, 128])
            kin = X(f1.a[:, :].rearrange("p (a b) -> p a b", b=128))
            kin.b = f1.b

            DMA("sp", GTR.a[:], modrow_d.ap()[l, 0:16, :].rearrange("a b -> (a b)").unsqueeze(0).to_broadcast([128, D]),
                r=[dbuf("modrow", l)], w=[GTR])
            DMA("sp", W2a.a[0:16, :], wa2_d.ap()[l], w=[W2a])
            DMA("sp", W2a.a[16:17, :], ba_d.ap()[l], w=[W2a])
            DMA("sp", ngr.a[:, :], gng_d.ap()[l].to_broadcast([128, 1024]), w=[ngr])
            DMA("sp", vgr.a[:, :], vng_d.ap()[l].to_broadcast([128, 1024]), w=[vgr])
            DMA("sp", ogr.a[:, :], og_d.ap()[l].to_broadcast([128, 1024]), w=[ogr])
            tr8(ws_d.ap()[l].rearrange("h t s -> t h s"), wsT, True, kin)
            DMA("sp", stage.a[0:8, :], gb_d.ap()[l], w=[stage])
            TR(PSB[0].a[:, 0:8], stage.a[0:8, :], cst.a[0:8, 0, 0:8], r=[stage, cst], w=[PSB[0]])
            CP("dve", bT.a[:, :], PSB[0].a[:, 0:8], r=[PSB[0]], w=[bT])
            MSET("dve", S32.a[:], 0.0, w=[S32])
            MSET("pool", Sbf.a[:], 0.0, w=[Sbf])
            MSET("dve", alT.a[:, :], 1.0, w=[alT])

            wcnt = [0]

            def load_w(src_ap3, ncols):
                t = wc[wcnt[0] % 2]
                wcnt[0] += 1
                DMA("sp", t.a[:, :, 0:ncols], src_ap3, r=[dbuf("winb"), dbuf("woutb")], w=[t])
                return t

            win_v = winb_d.ap()
            wo_src = woutb_d.ap()

            def win_cols(c0, n):
                return win_v[:, :, c0:c0 + n]

            last = (l == NL - 1)
            for g in range(NT // 4):
                t0 = g * 4
                so = split and last and g < HALF // 4
                if split and g == HALF // 4:
                    TS("dve", S32.a[:, :, :], S32.a[:, :, :], flag.a[:, 0:1], None, ALU.mult, r=[S32, flag], w=[S32])
                    CP("pool", Sbf.a[:, :, :], S32.a[:, :, :], r=[S32], w=[Sbf])
                DMA("sp", xt[0].a[:, :], xsrc.ap()[t0 * 128:(t0 + 1) * 128, :], r=[dbuf(xsrc.name, t0)], w=[xt[0]])
                for j in range(4):
                    if j + 1 < 4:
                        s = (j + 1) % 2
                        DMA("sp", xt[s].a[:, :], xsrc.ap()[(t0 + j + 1) * 128:(t0 + j + 2) * 128, :],
                            r=[dbuf(xsrc.name, t0 + j + 1)], w=[xt[s]])
                    norm_T(xt[j % 2].a[:, :], xt[j % 2], 0, 0, hTg,
                           lambda dc, j=j: hTg.a[:, dc, j * 128:(j + 1) * 128], xnb, junk)
                wal = load_w(win_cols(C_AL, 16), 16)
                wq_ = load_w(win_cols(C_K if so else C_Q, 512), 512)
                for dc in range(DC):
                    MM(PSB[2].a[0:16, :], wal.a[:, dc, 0:16], hTg.a[:, dc, :], start=(dc == 0), stop=(dc == DC - 1),
                       r=[wal, hTg], w=[PSB[2]])
                CP("dve", alT.a[0:16, :], PSB[2].a[0:16, :], r=[PSB[2]], w=[alT])
                for j in range(4):
                    tk = slice(j * 128, (j + 1) * 128)
                    MM(PSB[4].a[:, :], alT.a[:, tk], W2a.a[:, :], r=[alT, W2a], w=[PSB[4]])
                    ACT(et.a[:, :], PSB[4].a[:, :], AF.Exp, scale=-1.0, r=[PSB[4]], w=[et])
                    ACT(lt.a[:, :], et.a[:, :], AF.Ln, bias=onec.a[:, 0:1], scale=1.0, r=[et, onec], w=[lt])
                    MM(PSB[4].a[:, :], M_D, lt.a[:, :], r=[cst, lt], w=[PSB[4]])
                    ACT(EDg.a[:, j, :], PSB[4].a[:, :], AF.Exp, r=[PSB[4]], w=[EDg])
                    for h in range(4):
                        MM(PSB[5].a[:, h * 128:(h + 1) * 128], lt.a[:, h * 128:(h + 1) * 128], M_G,
                           r=[lt, cst], w=[PSB[5]])
                    CP("dve", GTg.a[:, j, :, :], v3(PSB[5].a[:, :], 128), r=[PSB[5]], w=[GTg])
                ACT(EGl.a[:, :, :], GTg.a[:, :, :, 127], AF.Exp, r=[GTg], w=[EGl])
                if so:
                    wk_ = wq_
                    wv0 = load_w(win_cols(C_V, 512), 512)
                    for j in range(4):
                        bk = PSB[2 + j % 2]
                        for dc in range(DC):
                            MM(bk.a[:, :], hTg.a[:, dc, j * 128:(j + 1) * 128], wk_.a[:, dc, :],
                               start=(dc == 0), stop=(dc == DC - 1), r=[wk_, hTg], w=[bk])
                        TT("dve", kdg.a[:, j, :], bk.a[:, :], EDg.a[:, j, :], ALU.mult, r=[bk, EDg], w=[kdg])
                    wv1 = load_w(win_cols(C_V + 512, 512), 512)
                    for (wv, off) in ((wv0, 0), (wv1, 512)):
                        for j in range(4):
                            bk = PSB[2 + j % 2]
                            for dc in range(DC):
                                MM(bk.a[:, :], hTg.a[:, dc, j * 128:(j + 1) * 128], wv.a[:, dc, :],
                                   start=(dc == 0), stop=(dc == DC - 1), r=[wv, hTg], w=[bk])
                            CP("act", vg_.a[:, j, off:off + 512], bk.a[:, :], r=[bk], w=[vg_])
                    for j in range(4):
                        for h in range(4):
                            bk = PSB[h // 2]
                            oc = slice((h % 2) * 256, (h % 2 + 1) * 256)
                            MM(bk.a[:, oc], kdg.a[:, j, h * 128:(h + 1) * 128], vg_.a[:, j, h * 256:(h + 1) * 256],
                               r=[kdg, vg_], w=[bk])
                        for h in range(4):
                            bk = PSB[h // 2]
                            oc = slice((h % 2) * 256, (h % 2 + 1) * 256)
                            STT(S32.a[:, h, :], S32.a[:, h, :], EGl.a[:, j, h:h + 1], bk.a[:, oc], ALU.mult, ALU.add,
                                r=[S32, EGl, bk], w=[S32])
                    CP("pool", Sbf.a[:, :, :], S32.a[:, :, :], r=[S32], w=[Sbf])
                    continue
                wk_ = load_w(win_cols(C_K, 512), 512)
                for h in range(4):
                    bk = PSB[2 + h % 2]
                    for dc in range(DC):
                        MM(bk.a[:, :], wq_.a[:, dc, h * 128:(h + 1) * 128], hTg.a[:, dc, :],
                           start=(dc == 0), stop=(dc == DC - 1), r=[wq_, hTg], w=[bk])
                    ACT(v3(et.a[:, :], 128), GTg.a[:, :, h, :], AF.Exp, r=[GTg], w=[et])
                    STT(qtg.a[:, h, :], bk.a[:, :], 128.0 ** -0.5, et.a[:, :], ALU.mult, ALU.mult,
                        r=[bk, et], w=[qtg])
                wv0 = load_w(win_cols(C_V, 512), 512)
                for h in range(4):
                    bk = PSB[2 + h % 2]
                    for dc in range(DC):
                        MM(bk.a[:, :], wk_.a[:, dc, h * 128:(h + 1) * 128], hTg.a[:, dc, :],
                           start=(dc == 0), stop=(dc == DC - 1), r=[wk_, hTg], w=[bk])
                    ACT(v3(et.a[:, :], 128), GTg.a[:, :, h, :], AF.Exp, scale=-1.0, r=[GTg], w=[et])
                    TT("dve", ktg.a[:, h, :], bk.a[:, :], et.a[:, :], ALU.mult, r=[bk, et], w=[ktg])
                for j in range(4):
                    bk = PSB[2 + j % 2]
                    for dc in range(DC):
                        MM(bk.a[:, :], hTg.a[:, dc, j * 128:(j + 1) * 128], wk_.a[:, dc, :],
                           start=(dc == 0), stop=(dc == DC - 1), r=[wk_, hTg], w=[bk])
                    TT("dve", kdg.a[:, j, :], bk.a[:, :], EDg.a[:, j, :], ALU.mult, r=[bk, EDg], w=[kdg])
                chunks = [(C_V, vg_, 0, None), (C_V + 512, vg_, 512, None),
                          (C_R, srg, 0, AF.Silu), (C_R + 512, srg, 512, AF.Silu),
                          (C_U, gug, 0, AF.Gelu_apprx_tanh), (C_U + 512, gug, 512, AF.Gelu_apprx_tanh),
                          (C_VS, gvg, 0, AF.Gelu_apprx_tanh), (C_VS + 512, gvg, 512, AF.Gelu_apprx_tanh)]
                wcur = wv0
                for ci, (c0, dstt, off, fn) in enumerate(chunks):
                    if ci + 1 < len(chunks):
                        nxt = load_w(win_cols(chunks[ci + 1][0], 512), 512)
                    else:
                        nxt = load_w(wo_src[:, :, 0:512], 512)
                    for j in range(4):
                        bk = PSB[2 + j % 2]
                        for dc in range(DC):
                            MM(bk.a[:, :], hTg.a[:, dc, j * 128:(j + 1) * 128], wcur.a[:, dc, :],
                               start=(dc == 0), stop=(dc == DC - 1), r=[wcur, hTg], w=[bk])
                        if fn is None:
                            CP("act", dstt.a[:, j, off:off + 512], bk.a[:, :], r=[bk], w=[dstt])
                        else:
                            ACT(dstt.a[:, j, off:off + 512], bk.a[:, :], fn, r=[bk], w=[dstt])
                    wcur = nxt
                wo = wcur
                for j in range(4):
                    tk = slice(j * 128, (j + 1) * 128)
                    for h in range(4):
                        MM(PSB[5].a[:, h * 128:(h + 1) * 128], ktg.a[:, h, tk], qtg.a[:, h, tk],
                           r=[ktg, qtg], w=[PSB[5]])
                    TT("dve", attm.a[:, :, :], v3(PSB[5].a[:, :], 128), cst.a[:, 1:2, :].to_broadcast([128, 4, 128]),
                       ALU.mult, r=[PSB[5], cst], w=[attm])
                    for h in range(4):
                        bk = PSB[6 + h // 2]
                        oc = slice((h % 2) * 256, (h % 2 + 1) * 256)
                        MM(bk.a[:, oc], attm.a[:, h, :], vg_.a[:, j, h * 256:(h + 1) * 256], start=True, stop=False,
                           r=[attm, vg_], w=[bk])
                        MM(bk.a[:, oc], qtg.a[:, h, tk], Sbf.a[:, h, :], start=False, stop=True,
                           r=[qtg, Sbf], w=[bk])
                    for h in range(4):
                        bk = PSB[h // 2]
                        oc = slice((h % 2) * 256, (h % 2 + 1) * 256)
                        MM(bk.a[:, oc], kdg.a[:, j, h * 128:(h + 1) * 128], vg_.a[:, j, h * 256:(h + 1) * 256],
                           r=[kdg, vg_], w=[bk])
                    for h in range(4):
                        bk = PSB[h // 2]
                        oc = slice((h % 2) * 256, (h % 2 + 1) * 256)
                        STT(S32.a[:, h, :], S32.a[:, h, :], EGl.a[:, j, h:h + 1], bk.a[:, oc], ALU.mult, ALU.add,
                            r=[S32, EGl, bk], w=[S32])
                    CP("pool", Sbf.a[:, :, :], S32.a[:, :, :], r=[S32], w=[Sbf])
                    for h in range(4):
                        bk = PSB[6 + h // 2]
                        oc = slice((h % 2) * 256, (h % 2 + 1) * 256)
                        ACT(junk.a[:, 0:256], bk.a[:, oc], AF.Square, accum=st8b.a[:, h:h + 1], r=[bk], w=[junk, st8b])
                    rstd_of(st8b.a[:, 0:4], 1.0 / 256, st8b.a[:, 4:8], r=[st8b], w=[st8b])
                    for h in range(4):
                        bk = PSB[6 + h // 2]
                        oc = slice((h % 2) * 256, (h % 2 + 1) * 256)
                        STT(f1.a[:, h * 256:(h + 1) * 256], bk.a[:, oc], st8b.a[:, 4 + h:5 + h],
                            ngr.a[:, h * 256:(h + 1) * 256], ALU.mult, ALU.mult, r=[bk, st8b, ngr], w=[f1])
                    TT("dve", ymix.a[:, 0:1024], f1.a[:, :], srg.a[:, j, :], ALU.mult, r=[f1, srg], w=[ymix])
                    TT("dve", f1.a[:, :], gvg.a[:, j, :], gvg.a[:, j, :], ALU.mult, r=[gvg], w=[f1])
                    RED(st8b.a[:, 8:16], v3(f1.a[:, :], 128), ALU.add, r=[f1], w=[st8b])
                    rstd_of(st8b.a[:, 8:16], 1.0 / 128, st8b.a[:, 16:24], r=[st8b], w=[st8b])
                    TT("dve", v3(f1.a[:, :], 128), v3(gvg.a[:, j, :], 128),
                       st8b.a[:, 16:24].unsqueeze(2).to_broadcast([128, 8, 128]), ALU.mult, r=[gvg, st8b], w=[f1])
                    TT("dve", vnb.a[:, :], f1.a[:, :], vgr.a[:, :], ALU.mult, r=[f1, vgr], w=[vnb])
                    for h in range(8):
                        bk = PSB[6 + h // 4]
                        MM(bk.a[:, (h % 4) * 128:(h % 4 + 1) * 128], wsT.a[:, h, :], vnb.a[:, h * 128:(h + 1) * 128],
                           r=[wsT, vnb], w=[bk])
                    for hb in range(2):
                        bk = PSB[6 + hb]
                        TT("dve", v3(f2.a[:, hb * 512:(hb + 1) * 512], 128), v3(bk.a[:, :], 128),
                           bT.a[:, hb * 4:(hb + 1) * 4].unsqueeze(2).to_broadcast([128, 4, 128]), ALU.add,
                           r=[bk, bT], w=[f2])
                    TT("dve", f2.a[:, :], f2.a[:, :], gug.a[:, j, :], ALU.mult, r=[f2, gug], w=[f2])
                    TT("dve", f1.a[:, :], f2.a[:, :], f2.a[:, :], ALU.mult, r=[f2], w=[f1])
                    RED(st8b.a[:, 24:32], v3(f1.a[:, :], 128), ALU.add, r=[f1], w=[st8b])
                    rstd_of(st8b.a[:, 24:32], 1.0 / 128, st8b.a[:, 32:40], r=[st8b], w=[st8b])
                    TT("dve", v3(f2.a[:, :], 128), v3(f2.a[:, :], 128),
                       st8b.a[:, 32:40].unsqueeze(2).to_broadcast([128, 8, 128]), ALU.mult, r=[f2, st8b], w=[f2])
                    TT("dve", ymix.a[:, 1024:2048], f2.a[:, :], ogr.a[:, :], ALU.mult, r=[f2, ogr], w=[ymix])
                    for mc in range(DC):
                        bk = PSB[mc // 8]
                        TR(psbf(mc // 8)[:, (mc % 8) * 128:(mc % 8 + 1) * 128], ymix.a[:, mc * 128:(mc + 1) * 128],
                           identb.a[:], r=[ymix, identb], w=[bk])
                    for hb in range(2):
                        bk = PSB[hb]
                        CP("act", ymT.a[:, hb * 8:(hb + 1) * 8, tk], v3(psbf(hb), 128), r=[bk], w=[ymT])
                for cg in range(4):
                    wnext = None
                    if cg + 1 < 4:
                        wnext = load_w(wo_src[:, :, (cg + 1) * 512:(cg + 2) * 512], 512)
                    for j in range(4):
                        tile = t0 + j
                        s = (cg * 4 + j) % 2
                        DMA("sp", xp[s].a[:, :], xsrc.ap()[tile * 128:(tile + 1) * 128, cg * 512:(cg + 1) * 512],
                            r=[dbuf(xsrc.name, tile)], w=[xp[s]])
                        bk = PSB[2 + j % 2]
                        for mc in range(DC):
                            MM(bk.a[:, :], ymT.a[:, mc, j * 128:(j + 1) * 128], wo.a[:, mc, :],
                               start=(mc == 0), stop=(mc == DC - 1), r=[ymT, wo], w=[bk])
                        TT("dve", xo[s].a[:, :], bk.a[:, :], GTR.a[:, cg * 512:(cg + 1) * 512], ALU.mult,
                           r=[bk, GTR], w=[xo[s]])
                        TT("pool", xo[s].a[:, :], xo[s].a[:, :], xp[s].a[:, :], ALU.add, r=[xo[s], xp[s]], w=[xo[s]])
                        DMA("sp", xdst.ap()[tile * 128:(tile + 1) * 128, cg * 512:(cg + 1) * 512], xo[s].a[:, :],
                            r=[xo[s]], w=[dbuf(xdst.name, tile)])
                    wo = wnext

        def peer_layer(l, xsrc, xdst):
            new_phase()
            ub = [AR.take([128, D], BF16) for _ in range(2)]
            uo = [AR.take([128, D], BF16) for _ in range(2)]
            vb_ = [AR.take([128, D], BF16) for _ in range(2)]
            wq_src = wq_d.ap()[l].rearrange("(dc p) c -> p dc c", p=128)
            wst = [AR.take([128, DC, 512], BF16) for _ in range(2)]
            for cg in range(4):
                DMA("pool", wst[cg % 2].a[:, :, :], wq_src[:, :, cg * 512:(cg + 1) * 512], w=[wst[cg % 2]])
                DMA("sp", wqb_d.ap()[cg], wst[cg % 2].a[:, :, :].rearrange("p a b -> p (a b)"),
                    r=[wst[cg % 2]], w=[dbuf("wqb", cg)])
            for i in range(128):
                s = i % 2
                DMA("pool", ub[s].a[:, :], pu_d.ap()[l, i * 128:(i + 1) * 128, :], w=[ub[s]])
                DMA("pool", vb_[s].a[:, :], pv_d.ap()[l, i * 128:(i + 1) * 128, :], w=[vb_[s]])
                DMA("sp", vb_d.ap()[i], vb_[s].a[:, :], r=[vb_[s]], w=[dbuf("vb", i)])
                for dc in range(DC):
                    bi = 4 + dc // 8 + 2 * s
                    TR(psbf(bi)[:, (dc % 8) * 128:(dc % 8 + 1) * 128],
                       ub[s].a[:, dc * 128:(dc + 1) * 128], identb.a[:], r=[ub[s], identb], w=[PSB[bi]])
                CP("act", uo[s].a[:, 0:1024], psbf(4 + 2 * s), r=[PSB[4 + 2 * s]], w=[uo[s]])
                CP("dve", uo[s].a[:, 1024:2048], psbf(5 + 2 * s), r=[PSB[5 + 2 * s]], w=[uo[s]])
                DMA("sp", ut_d.ap()[i], uo[s].a[:, :], r=[uo[s]], w=[dbuf("ut", i)])

            new_phase()
            k1T = AR.take([128, 8, 128])
            k2T = AR.take([128, 8, 128])
            h2T = AR.take([128, DC, 256], BF16)
            sc5T = AR.take([128, 5, 256])
            GA = AR.take([128, 128, 256], BF16)
            xblk = AR.take([128, 2, D])
            base = AR.off
            kin = X(xblk.a[:, 0, 0:1024].rearrange("p (a b) -> p a b", b=128))
            kin.b = xblk.b
            tr8(k1_d.ap()[l].rearrange("h k d -> k h d"), k1T, False, kin)
            tr8(k2_d.ap()[l].rearrange("h k d -> k h d"), k2T, False, kin)
            DMA("sp", GTR.a[:], modrow_d.ap()[l, 16:32, :].rearrange("a b -> (a b)").unsqueeze(0).to_broadcast([128, D]),
                r=[dbuf("modrow", l)], w=[GTR])

            for blk in range(HALF // 2 if (split and l == NL - 1) else 0, NT // 2):
                tb0 = blk * 2
                P.fence()
                AR.off = base
                xnb = AR.take([128, D], BF16)
                junk = AR.take([128, D], BF16)
                wcq = [AR.take([128, DC, 512], BF16) for _ in range(2)]
                qT = AR.take([128, 16, 256])
                ssb = [AR.take([128, 1024]) for _ in range(2)]
                v12 = AR.take([128, 2, 8, 16])
                idx = AR.take([128, 8, 16], mybir.dt.uint32)
                tmpA = AR.take([128, 256])
                cand = AR.take([128, 8, 256])
                sc = AR.take([128, 8, 24])
                tm5 = AR.take([128, 4, 128])
                sm = AR.take([128, 64])
                ex16 = AR.take([128, 8, 16])
                for ts in range(2):
                    tile = tb0 + ts
                    DMA("sp", xblk.a[:, ts, :], xsrc.ap()[tile * 128:(tile + 1) * 128, :],
                        r=[dbuf(xsrc.name, tile)], w=[xblk])
                for ts in range(2):
                    norm_T(xblk.a[:, ts, :], xblk, 1, 48, h2T,
                           lambda dc, ts=ts: h2T.a[:, dc, ts * 128:(ts + 1) * 128], xnb, junk)
                def load_wq(cg):
                    DMA("sp", wcq[cg % 2].a[:, :, :].rearrange("p a b -> p (a b)"), wqb_d.ap()[cg],
                        r=[dbuf("wqb", cg)], w=[wcq[cg % 2]])
                load_wq(0)
                for cg in range(4):
                    if cg + 1 < 4:
                        load_wq(cg + 1)
                    wq_ = wcq[cg % 2]
                    for cc in range(4):
                        bk = PSB[2 + cc % 2]
                        half = (cc // 2 % 2) * 256
                        for dc in range(DC):
                            MM(bk.a[:, half:half + 256], wq_.a[:, dc, cc * 128:(cc + 1) * 128], h2T.a[:, dc, :],
                               start=(dc == 0), stop=(dc == DC - 1), r=[wq_, h2T], w=[bk])
                        CP("act", qT.a[:, cg * 4 + cc, :], bk.a[:, half:half + 256], r=[bk], w=[qT])
                for ts in range(2):
                    tile = tb0 + ts
                    tk = slice(ts * 128, (ts + 1) * 128)
                    for side in range(2):
                        kT = k1T if side == 0 else k2T
                        for h in range(8):
                            bk = PSB[4 + side * 2 + h // 4]
                            MM(bk.a[:, (h % 4) * 128:(h % 4 + 1) * 128], qT.a[:, 2 * h + side, tk], kT.a[:, h, :],
                               r=[qT, kT], w=[bk])
                        for hb in range(2):
                            bk = PSB[4 + side * 2 + hb]
                            CP("act", ssb[side].a[:, hb * 512:(hb + 1) * 512], bk.a[:, :], r=[bk], w=[ssb[side]])
                        if side == 1:
                            DMA("sp", s2_d.ap()[tile * 128:(tile + 1) * 128],
                                ssb[1].a[:, :].unsqueeze(1).to_broadcast([128, 16, 1024]),
                                r=[ssb[1]], w=[dbuf("s2", tile)])
                        for h in range(8):
                            sv = ssb[side].a[:, h * 128:(h + 1) * 128]
                            MAX8(v12.a[:, side, h, 0:8], sv, r=[ssb[side]], w=[v12])
                            if side == 0:
                                MIDX(idx.a[:, h, 0:8], v12.a[:, 0, h, 0:8], sv, r=[ssb[0], v12], w=[idx])
                            MREP(tmpA.a[:, 0:128], v12.a[:, side, h, 0:8], sv, r=[ssb[side], v12], w=[tmpA])
                            MAX8(v12.a[:, side, h, 8:16], tmpA.a[:, 0:128], r=[tmpA], w=[v12])
                            if side == 0:
                                MIDX(idx.a[:, h, 8:16], v12.a[:, 0, h, 8:16], tmpA.a[:, 0:128], r=[tmpA, v12], w=[idx])
                    TT("dve", cand.a[:, :, :].rearrange("p h (a b) -> p h a b", b=16),
                       v12.a[:, 0, :, :].unsqueeze(3).to_broadcast([128, 8, 16, 16]),
                       v12.a[:, 1, :, :].unsqueeze(2).to_broadcast([128, 8, 16, 16]), ALU.add, r=[v12], w=[cand])
                    for h in range(8):
                        MAX8(sc.a[:, h, 0:8], cand.a[:, h, :], r=[cand], w=[sc])
                        MREP(tmpA.a[:, :], sc.a[:, h, 0:8], cand.a[:, h, :], r=[cand, sc], w=[tmpA])
                        MAX8(sc.a[:, h, 8:16], tmpA.a[:, :], r=[tmpA], w=[sc])
                        MREP(tmpA.a[:, :], sc.a[:, h, 8:16], tmpA.a[:, :], r=[tmpA, sc], w=[tmpA])
                        MAX8(sc.a[:, h, 16:24], tmpA.a[:, :], r=[tmpA], w=[sc])
                    TT("dve", sm.a[:, 0:8], sc.a[:, :, 15], sc.a[:, :, 16], ALU.add, r=[sc], w=[sm])
                    TS("dve", sm.a[:, 0:8], sm.a[:, 0:8], 0.5, None, ALU.mult, r=[sm], w=[sm])
                    TT("dve", ex16.a[:, :, :], sc.a[:, :, 0:16], sc.a[:, :, 0:1].to_broadcast([128, 8, 16]),
                       ALU.subtract, r=[sc], w=[ex16])
                    ACT(ex16.a[:, :, :], ex16.a[:, :, :], AF.Exp, r=[ex16], w=[ex16])
                    RED(sm.a[:, 8:16], ex16.a[:, :, :], ALU.add, r=[ex16], w=[sm])
                    RECIP(sm.a[:, 16:24], sm.a[:, 8:16], r=[sm], w=[sm])
                    v1v = v12.a[:, 0, :, :]
                    t4 = lambda k: tm5.a[:, k, :].rearrange("p (a h) -> p h a", h=8)
                    CP("dve", t4(0), idx.a[:, :, :], r=[idx], w=[tm5])
                    TT("dve", t4(1), v1v, v12.a[:, 0, :, 0:1].to_broadcast([128, 8, 16]), ALU.subtract,
                       r=[v12], w=[tm5])
                    ACT(t4(1), t4(1), AF.Exp, r=[tm5], w=[tm5])
                    TT("dve", t4(1), t4(1), sm.a[:, 16:24].unsqueeze(2).to_broadcast([128, 8, 16]), ALU.mult,
                       r=[tm5, sm], w=[tm5])
                    TT("dve", t4(2), sm.a[:, 0:8].unsqueeze(2).to_broadcast([128, 8, 16]), v1v, ALU.subtract,
                       r=[sm, v12], w=[tm5])
                    TS("dve", t4(3), v12.a[:, 1, :, 0:1].to_broadcast([128, 8, 16]), -1.0, None, ALU.mult,
                       r=[v12], w=[tm5])
                    for k in range(4):
                        TR(PSB[0].a[:, k * 128:(k + 1) * 128], tm5.a[:, k, :], ident, r=[tm5, cst], w=[PSB[0]])
                    CP("act", sc5T.a[:, 0:4, tk], v3(PSB[0].a[:, :], 128), r=[PSB[0]], w=[sc5T])
                P.fence()
                AR.off = base
                NSR = 3
                SR = [AR.take([128, 16, 128]) for _ in range(NSR)]
                Pt = [AR.take([128, 128], BF16) for _ in range(4)]
                Et = [AR.take([128, 128]) for _ in range(4)]
                Rt = [AR.take([128, 128], BF16) for _ in range(4)]

                def load_sr(bi):
                    sl = bi % NSR
                    tok0 = tb0 * 128 + bi * 16
                    tile = tok0 // 128
                    src = bass.AP(s2_d, tok0 * 16384, [[128, 128], [16384, 16], [1, 128]])
                    DMA("sp", SR[sl].a[:, :, :], src, r=[dbuf("s2", tile)], w=[SR[sl]])
                load_sr(0)
                load_sr(1)
                for bi in range(0 if opts.get('skipB') else 16):
                    if bi + 2 < 16:
                        load_sr(bi + 2)
                    sl = bi % NSR
                    for tt in range(16):
                        t = bi * 16 + tt
                        k2 = t % 4
                        col = lambda k, t=t: sc5T.a[:, k, t:t + 1]
                        TS("dve", Pt[k2].a[:, :], iota, col(0), col(1), ALU.is_equal, ALU.mult,
                           r=[cst, sc5T], w=[Pt[k2]])
                        ACT(Et[k2].a[:, :], SR[sl].a[:, tt, :], AF.Exp, bias=col(3), scale=1.0,
                            r=[SR[sl], sc5T], w=[Et[k2]])
                        STT(Rt[k2].a[:, :], SR[sl].a[:, tt, :], col(2), Et[k2].a[:, :], ALU.is_ge, ALU.mult,
                            r=[SR[sl], sc5T, Et[k2]], w=[Rt[k2]])
                        bk = PSB[4 + (t // 4) % 4]
                        MM(bk.a[:, (t % 4) * 128:(t % 4 + 1) * 128], Rt[k2].a[:, :], Pt[k2].a[:, :],
                           r=[Rt[k2], Pt[k2]], w=[bk])
                        if t % 4 == 3 and t >= 11:
                            te = t - 8
                            bke = PSB[4 + (te // 4) % 4]
                            CP("act", GA.a[:, :, te - 3:te + 1], bke.a[:, :].rearrange("p (t i) -> p i t", i=128),
                               r=[bke], w=[GA])
                if not opts.get('skipB'):
                    for te in (251, 255):
                        bke = PSB[4 + (te // 4) % 4]
                        CP("act", GA.a[:, :, te - 3:te + 1], bke.a[:, :].rearrange("p (t i) -> p i t", i=128),
                           r=[bke], w=[GA])
                P.fence()
                AR.off = base
                NUT, NVV = 3, 4
                gz = [AR.take([128, 256], BF16) for _ in range(4)]
                utb = [AR.take([128, 2, DC * 128], BF16) for _ in range(NUT)]
                vvb = [AR.take([128, 2, 1024], BF16) for _ in range(NVV)]
                yo = [AR.take([128, 1024]) for _ in range(2)]
                ZS = [X(PSB[4 + k].a[:, 0:256]) for k in range(4)]

                def load_ut(pi):
                    DMA("sp", utb[pi % NUT].a[:, :, :],
                        ut_d.ap()[2 * pi:2 * pi + 2].rearrange("i p f -> p i f"),
                        r=[dbuf("ut", 2 * pi), dbuf("ut", 2 * pi + 1)], w=[utb[pi % NUT]])
                if not opts.get('skipC'):
                    load_ut(0)
                    load_ut(1)
                for i in range(0 if opts.get('skipC') else 128):
                    if i % 2 == 0 and i // 2 + 2 < 64:
                        load_ut(i // 2 + 2)
                    ub_ = utb[(i // 2) % NUT]
                    zs = ZS[i % 4]
                    for dc in range(DC):
                        MM(zs.a, ub_.a[:, i % 2, dc * 128:(dc + 1) * 128], h2T.a[:, dc, :],
                           start=(dc == 0), stop=(dc == DC - 1), r=[ub_, h2T], w=[zs])
                    ACT(gz[i % 4].a[:, :], zs.a, AF.Gelu_apprx_tanh, r=[zs], w=[gz[i % 4]])
                    TT("dve", GA.a[:, i, :], gz[i % 4].a[:, :], GA.a[:, i, :], ALU.mult, r=[gz[i % 4], GA], w=[GA])
                for dh in range(0 if opts.get('skipC') else 2):
                    def load_v(pi, dh=dh):
                        DMA("sp", vvb[pi % NVV].a[:, :, :],
                            vb_d.ap()[2 * pi:2 * pi + 2, :, dh * 1024:(dh + 1) * 1024].rearrange("i p f -> p i f"),
                            r=[dbuf("vb", 2 * pi), dbuf("vb", 2 * pi + 1)], w=[vvb[pi % NVV]])
                    load_v(0)
                    load_v(1)
                    load_v(2)
                    for i in range(128):
                        if i % 2 == 0 and i // 2 + 3 < 64:
                            load_v(i // 2 + 3)
                        vv_ = vvb[(i // 2) % NVV]
                        for ts in range(2):
                            for cq in range(2):
                                bk = PSB[ts * 2 + cq]
                                MM(bk.a[:, :], GA.a[:, i, ts * 128:(ts + 1) * 128],
                                   vv_.a[:, i % 2, cq * 512:(cq + 1) * 512],
                                   start=(i == 0), stop=(i == 127), r=[GA, vv_], w=[bk])
                    for ts in range(2):
                        tile = tb0 + ts
                        y_ = yo[ts]
                        for cq in range(2):
                            bk = PSB[ts * 2 + cq]
                            c0 = dh * 1024 + cq * 512
                            TT("dve", y_.a[:, cq * 512:(cq + 1) * 512], bk.a[:, :], GTR.a[:, c0:c0 + 512], ALU.mult,
                               r=[bk, GTR], w=[y_])
                        TT("pool", y_.a[:, :], y_.a[:, :], xblk.a[:, ts, dh * 1024:(dh + 1) * 1024], ALU.add,
                           r=[y_, xblk], w=[y_])
                        DMA("sp", xdst.ap()[tile * 128:(tile + 1) * 128, dh * 1024:(dh + 1) * 1024], y_.a[:, :],
                            r=[y_], w=[dbuf(xdst.name, tile)])

        def tail(xsrc, norm):
            new_phase()
            xt = [AR.take([128, D]) for _ in range(2)]
            junk = AR.take([128, D], BF16)
            if norm:
                DMA("sp", GTR.a[:], fg_d.ap().to_broadcast([128, D]), w=[GTR])
            for tile in range(HALF, NT):
                s = tile % 2
                DMA("sp", xt[s].a[:, :], xsrc.ap()[tile * 128:(tile + 1) * 128, :], r=[dbuf(xsrc.name, tile)], w=[xt[s]])
                if norm:
                    ACT(junk.a[:, :], xt[s].a[:, :], AF.Square, accum=st8.a[:, 0:1], r=[xt[s]], w=[junk, st8])
                    rstd_of(st8.a[:, 0:1], 1.0 / D, st8.a[:, 1:2], r=[st8], w=[st8])
                    STT(xt[s].a[:, :], xt[s].a[:, :], st8.a[:, 1:2], GTR.a[:], ALU.mult, ALU.mult,
                        r=[xt[s], st8, GTR], w=[xt[s]])
                DMA("sp", out_d.ap()[(tile - HALF) * 128:(tile - HALF + 1) * 128, :], xt[s].a[:, :], r=[xt[s]],
                    w=[dbuf("out", tile)])

        done = False
        cur = x_d
        for l in range(NL):
            setup_layer(l)
            mixer_layer(l, cur, xa_d)
            if stop_after == ("mixer", l):
                tail(xa_d, False)
                done = True
                break
            peer_layer(l, xa_d, xb_d)
            cur = xb_d
            if stop_after == ("peer", l):
                tail(xb_d, False)
                done = True
                break
        if not done:
            tail(cur, True)
        P.finish("sp")
        P.emit()
    return nc


_CONSTS = None


def _consts():
    global _CONSTS
    if _CONSTS is None:
        c = np.zeros((128, 5, 128), np.float32)
        c[:, 4, :] = np.arange(128, dtype=np.float32)[None, :]
        c[:, 0, :] = np.eye(128, dtype=np.float32)
        tri = np.triu(np.ones((128, 128), np.float32))
        c[:, 1, :] = tri
        c[:, 2, :] = -tri / 16.0
        c[:, 3, :] = -(1.0 - tri) / 16.0
        _CONSTS = c
    return _CONSTS


def make_in_maps(inputs, ncores, ntok, split=False):
    f = lambda a: np.ascontiguousarray(np.asarray(a, dtype=np.float32))
    shared = {
        "ada_w": f(inputs["ada_w"]),
        "ada_b": f(inputs["ada_b"]).reshape(DEPTH, 96, 128),
        "norm1_g": f(inputs["norm1_g"]).reshape(DEPTH, 16, 128),
        "w_in": f(inputs["w_in"]),
        "gla_w_a2": f(inputs["gla_w_a2"]),
        "gla_b_a": f(inputs["gla_b_a"]).reshape(DEPTH, 1, 512),
        "gla_norm_g": f(inputs["gla_norm_g"]).reshape(DEPTH, 1, 1024),
        "gmlp_vnorm_g": f(inputs["gmlp_vnorm_g"]).reshape(DEPTH, 1, 1024),
        "gmlp_ws": f(inputs["gmlp_ws"]),
        "gmlp_b": f(inputs["gmlp_b"]),
        "gmlp_out_g": f(inputs["gmlp_out_g"]).reshape(DEPTH, 1, 1024),
        "w_out": f(inputs["w_out"]),
        "norm2_g": f(inputs["norm2_g"]).reshape(DEPTH, 16, 128),
        "peer_wq": f(inputs["peer_wq"]),
        "peer_k1": f(inputs["peer_k1"]),
        "peer_k2": f(inputs["peer_k2"]),
        "peer_u": f(inputs["peer_u"]),
        "peer_v": f(inputs["peer_v"]),
        "final_g": f(inputs["final_g"]).reshape(1, D),
        "consts": _consts(),
    }
    x = f(inputs["x"])
    c = f(inputs["c"])
    maps = []
    if not split:
        for b in range(ncores):
            m = dict(shared)
            m["x"] = np.ascontiguousarray(x[b, :ntok, :])
            m["c"] = np.ascontiguousarray(c[b].reshape(16, 128))
            m["flag"] = np.ones((128, 16), np.float32)
            maps.append(m)
        return maps
    hn = ntok // 2
    for cid in range(ncores):
        b, second = cid // 2, cid % 2
        m = dict(shared)
        if second:
            m["x"] = np.ascontiguousarray(x[b, :ntok, :])
        else:
            m["x"] = np.ascontiguousarray(np.concatenate([x[b, hn:ntok, :], x[b, :hn, :]], axis=0))
        m["c"] = np.ascontiguousarray(c[b].reshape(16, 128))
        m["flag"] = np.full((128, 16), float(second), np.float32)
        maps.append(m)
    return maps


_NC_CACHE = {}


def kernel(**inputs):
    key = "full"
    if key not in _NC_CACHE:
        _NC_CACHE[key] = build(NT=SEQ // 128, NL=DEPTH, split=True)
    nc = _NC_CACHE[key]
    ncores = 2 * BATCH
    in_maps = make_in_maps(inputs, ncores, SEQ, split=True)
    res = run_bass_kernel_spmd(nc, in_maps, core_ids=list(range(ncores)))
    out = np.empty((BATCH, SEQ, D), np.float32)
    hn = SEQ // 2
    for cid in range(ncores):
        b, second = cid // 2, cid % 2
        out[b, second * hn:(second + 1) * hn, :] = np.asarray(res.results[cid]["out"], dtype=np.float32)
    return out
```

```python
import numpy as np
from contextlib import ExitStack
import concourse.bass as bass
import concourse.mybir as mybir
from concourse.bass_utils import run_bass_kernel_spmd

F32 = mybir.dt.float32
BF16 = mybir.dt.bfloat16
AF = mybir.ActivationFunctionType
ALU = mybir.AluOpType
AX = mybir.AxisListType

D = 2048
DC = 16
SEQ = 4096
BATCH = 4
DEPTH = 2
IN_COLS = 5136
NEXP = 16384
EPS = 1e-6
C_Q, C_K, C_V, C_R, C_AL, C_U, C_VS = 0, 512, 1024, 2048, 3072, 3088, 4112
NEG = -1.0e30


class Buf:
    __slots__ = ("lw", "rd")

    def __init__(self):
        self.lw = None
        self.rd = {}


class X:
    def __init__(self, t):
        self.a = t
        self.b = Buf()


def _b(x):
    return x.b if isinstance(x, X) else x


class Prog:
    ENGS = ("sp", "act", "dve", "pool", "pe")
    NDMA = 12

    def __init__(self, nc, self_wait=True):
        self.nc = nc
        self.h = {"sp": nc.sync, "act": nc.scalar, "dve": nc.vector,
                  "pool": nc.gpsimd, "pe": nc.tensor}
        self.ops = {e: [] for e in self.ENGS}
        self.cnt = {e: 0 for e in self.ENGS}
        self.waited = {e: {} for e in self.ENGS}
        self.dma_next = {e: 0 for e in self.ENGS}
        self.dma_val = {}
        self.self_wait = self_wait
        self.sems = {}

    def op(self, eng, fn, reads=(), writes=(), dma=False):
        waits = {}

        def need(kv):
            if kv is None:
                return
            k, v = kv
            if v > 0 and waits.get(k, 0) < v:
                waits[k] = v
        reads = [_b(x) for x in reads]
        writes = [_b(x) for x in writes]
        for b in reads:
            need(b.lw)
        for b in writes:
            need(b.lw)
            for k, v in b.rd.items():
                need((k, v))
        if dma:
            slot = self.dma_next[eng]
            self.dma_next[eng] = (slot + 1) % self.NDMA
            key = ("dma", eng, slot)
            prev = self.dma_val.get(key, 0)
            need((key, prev))
            myval = prev + 16
            self.dma_val[key] = myval
        else:
            key = ("eng", eng)
            self.cnt[eng] += 1
            myval = self.cnt[eng]
        fw = []
        for k, v in waits.items():
            if k == ("eng", eng) and (eng == "pe" or not self.self_wait):
                continue
            if self.waited[eng].get(k, 0) >= v:
                continue
            self.waited[eng][k] = v
            fw.append((k, v))
        self.ops[eng].append((fw, fn, key, 16 if dma else 1))
        for b in reads:
            if b.rd.get(key, 0) < myval:
                b.rd[key] = myval
        for b in writes:
            b.lw = (key, myval)
            b.rd = {}

    def fence(self):
        for e in self.ENGS:
            fw = []
            for key, v in self.dma_val.items():
                if self.waited[e].get(key, 0) < v:
                    self.waited[e][key] = v
                    fw.append((key, v))
            for e2 in self.ENGS:
                k = ("eng", e2)
                if self.cnt[e2] > 0 and self.waited[e].get(k, 0) < self.cnt[e2]:
                    self.waited[e][k] = self.cnt[e2]
                    fw.append((k, self.cnt[e2]))
            if fw:
                self.ops[e].append((fw, None, None, 0))

    def finish(self, eng="sp"):
        fw = []
        for key, v in self.dma_val.items():
            if self.waited[eng].get(key, 0) < v:
                fw.append((key, v))
        for e in self.ENGS:
            if e != eng and self.cnt[e] > 0:
                fw.append((("eng", e), self.cnt[e]))
        self.ops[eng].append((fw, None, None, 0))

    def emit(self):
        nc = self.nc
        keys = set()
        for e in self.ENGS:
            for fw, fn, key, inc in self.ops[e]:
                if key is not None:
                    keys.add(key)
                for k, v in fw:
                    keys.add(k)
        with ExitStack() as st:
            for k in sorted(keys):
                self.sems[k] = st.enter_context(
                    nc.semaphore("s_" + "_".join(str(x) for x in k)))
            block = st.enter_context(nc.Block())

            def run(e):
                h = self.h[e]
                for fw, fn, key, inc in self.ops[e]:
                    for k, v in fw:
                        h.wait_ge(self.sems[k], v)
                    if fn is not None:
                        fn().then_inc(self.sems[key], inc)

            @block.sync
            def _(x):
                run("sp")

            @block.scalar
            def _(x):
                run("act")

            @block.vector
            def _(x):
                run("dve")

            @block.gpsimd
            def _(x):
                run("pool")

            @block.tensor
            def _(x):
                run("pe")


class Arena:
    def __init__(self, t, nwords):
        self.t = t
        self.n = nwords
        self.off = 0

    def reset(self):
        self.off = 0

    def take(self, shape, dt=F32, parts=128):
        n = 1
        for s in shape[1:]:
            n *= s
        four = dt in (F32, mybir.dt.uint32, mybir.dt.int32)
        words = n if four else (n + 1) // 2
        words = (words + 15) // 16 * 16
        assert self.off + words <= self.n, ("arena overflow", self.off, words, self.n)
        ap = self.t[0:shape[0], self.off:self.off + words]
        self.off += words
        if dt != F32:
            ap = ap.bitcast(dt)
        ap = ap[:, 0:n]
        if len(shape) == 3:
            ap = ap.rearrange("p (a b) -> p a b", b=shape[2])
        elif len(shape) == 4:
            ap = ap.rearrange("p (a b c) -> p a b c", b=shape[2], c=shape[3])
        return X(ap)


def build(NT=32, NL=DEPTH, stop_after=None, opts=None, split=False):
    opts = opts or {}
    HALF = NT // 2 if split else 0
    NTOK = NT * 128
    assert NT % 4 == 0 and HALF % 4 == 0
    nc = bass.Bass("TRN2", target_bir_lowering=False)
    P = Prog(nc, self_wait=bool(opts.get('self_wait', True)))

    def din(name, shape):
        return nc.dram_tensor(name, shape, F32, kind="ExternalInput")

    x_d = din("x", [NTOK, D])
    c_d = din("c", [16, 128])
    adaw_d = din("ada_w", [DEPTH, D, 6 * D])
    adab_d = din("ada_b", [DEPTH, 96, 128])
    n1g_d = din("norm1_g", [DEPTH, 16, 128])
    win_d = din("w_in", [DEPTH, D, IN_COLS])
    wa2_d = din("gla_w_a2", [DEPTH, 16, 512])
    ba_d = din("gla_b_a", [DEPTH, 1, 512])
    gng_d = din("gla_norm_g", [DEPTH, 1, 1024])
    vng_d = din("gmlp_vnorm_g", [DEPTH, 1, 1024])
    ws_d = din("gmlp_ws", [DEPTH, 8, 128, 128])
    gb_d = din("gmlp_b", [DEPTH, 8, 128])
    og_d = din("gmlp_out_g", [DEPTH, 1, 1024])
    wout_d = din("w_out", [DEPTH, D, D])
    n2g_d = din("norm2_g", [DEPTH, 16, 128])
    wq_d = din("peer_wq", [DEPTH, D, D])
    k1_d = din("peer_k1", [DEPTH, 8, 128, 128])
    k2_d = din("peer_k2", [DEPTH, 8, 128, 128])
    pu_d = din("peer_u", [DEPTH, NEXP, D])
    pv_d = din("peer_v", [DEPTH, NEXP, D])
    fg_d = din("final_g", [1, D])
    cst_d = din("consts", [128, 5, 128])
    flag_d = din("flag", [128, 16])
    out_d = nc.dram_tensor("out", [NTOK - HALF * 128, D], F32, kind="ExternalOutput")

    def dscr(name, shape, dt=F32):
        return nc.dram_tensor(name, shape, dt, kind="Internal")

    xa_d = dscr("xa", [NTOK, D])
    xb_d = dscr("xb", [NTOK, D])
    modrow_d = dscr("modrow", [DEPTH, 32, 128])
    s2_d = dscr("s2rep", [NTOK, 16, 1024])
    wqb_d = dscr("wqb", [4, 128, DC * 512], BF16)
    WCH = [(C_Q, 512), (C_K, 512), (C_V, 512), (C_V + 512, 512), (C_R, 512), (C_R + 512, 512), (C_AL, 16),
           (C_U, 512), (C_U + 512, 512), (C_VS, 512), (C_VS + 512, 512)]
    WIDX = {c0: k for k, (c0, n) in enumerate(WCH)}
    winb_d = dscr("winb", [len(WCH), 128, DC * 512], BF16)
    woutb_d = dscr("woutb", [4, 128, DC * 512], BF16)
    ut_d = dscr("uts", [128, 128, DC * 128], BF16)
    vb_d = dscr("vbs", [128, 128, D], BF16)

    db = {}

    def dbuf(*key):
        if key not in db:
            db[key] = Buf()
        return db[key]

    st = ExitStack()
    with st:
        def SB(name, shape, dt=F32):
            return X(st.enter_context(nc.sbuf_tensor(name, shape, dt)))

        def DMA(q, out, in_, r=(), w=()):
            h = P.h[q]
            P.op(q, lambda: h.dma_start(out=out, in_=in_), reads=r, writes=w, dma=True)

        def MM(out, lhsT, rhs, start=True, stop=True, r=(), w=()):
            P.op("pe", lambda: nc.tensor.matmul(out, lhsT=lhsT, rhs=rhs, start=start, stop=stop),
                 reads=r, writes=w)

        def TR(out, in_, ident, r=(), w=()):
            P.op("pe", lambda: nc.tensor.transpose(out=out, in_=in_, identity=ident),
                 reads=r, writes=w)

        def ACT(out, in_, func, bias=None, scale=None, accum=None, r=(), w=()):
            kw = {}
            if bias is not None:
                kw["bias"] = bias
            if scale is not None:
                kw["scale"] = scale
            if accum is not None:
                kw["accum_out"] = accum
            P.op("act", lambda: nc.scalar.activation(out=out, in_=in_, func=func, **kw),
                 reads=r, writes=w)

        def TS(e, out, in0, s1, s2, op0, op1=None, r=(), w=()):
            h = P.h[e]
            if op1 is None:
                P.op(e, lambda: h.tensor_scalar(out=out, in0=in0, scalar1=s1, scalar2=None, op0=op0),
                     reads=r, writes=w)
            else:
                P.op(e, lambda: h.tensor_scalar(out=out, in0=in0, scalar1=s1, scalar2=s2, op0=op0, op1=op1),
                     reads=r, writes=w)

        def TT(e, out, in0, in1, op, r=(), w=()):
            h = P.h[e]
            P.op(e, lambda: h.tensor_tensor(out=out, in0=in0, in1=in1, op=op), reads=r, writes=w)

        def STT(out, in0, scalar, in1, op0, op1, r=(), w=()):
            P.op("dve", lambda: nc.vector.scalar_tensor_tensor(out=out, in0=in0, scalar=scalar, in1=in1,
                                                               op0=op0, op1=op1), reads=r, writes=w)

        def CP(e, out, in_, r=(), w=()):
            h = P.h[e]
            if e == "act":
                P.op(e, lambda: nc.scalar.copy(out=out, in_=in_), reads=r, writes=w)
            else:
                P.op(e, lambda: h.tensor_copy(out=out, in_=in_), reads=r, writes=w)

        def MSET(e, out, val, w=()):
            h = P.h[e]
            P.op(e, lambda: h.memset(out, val), writes=w)

        def RED(out, in_, op, r=(), w=()):
            P.op("dve", lambda: nc.vector.tensor_reduce(out=out, in_=in_, axis=AX.X, op=op), reads=r, writes=w)

        def MAX8(out, in_, r=(), w=()):
            P.op("dve", lambda: nc.vector.max(out=out, in_=in_), reads=r, writes=w)

        def MIDX(out, mx, vals, r=(), w=()):
            P.op("dve", lambda: nc.vector.max_index(out=out, in_max=mx, in_values=vals), reads=r, writes=w)

        def MREP(out, rep, vals, r=(), w=()):
            P.op("dve", lambda: nc.vector.match_replace(out=out, in_to_replace=rep, in_values=vals,
                                                        imm_value=NEG), reads=r, writes=w)

        def RECIP(out, in_, r=(), w=()):
            P.op("dve", lambda: nc.vector.reciprocal(out=out, in_=in_), reads=r, writes=w)

        def v3(ap, b):
            return ap.rearrange("p (a b) -> p a b", b=b)

        PSB = [X(st.enter_context(nc.psum_tensor("psb%d" % i, [128, 512], F32))) for i in range(8)]

        def psbf(i):
            return PSB[i].a[:, :].bitcast(BF16)

        cst = SB("cst", [128, 5, 128])
        identb = SB("identb", [128, 128], BF16)
        epsc = SB("epsc", [128, 1])
        onec = SB("onec", [128, 1])
        smallT = SB("smallT", [128, 128])
        condT = SB("condT", [128, 16, 2])
        modT = SB("modT", [128, 96])
        AB = SB("AB", [128, 2, 16])
        GTR = SB("GTR", [128, D])
        st8 = SB("st8", [128, 64])
        st8b = SB("st8b", [128, 64])
        flag = SB("flag_sb", [128, 16])
        ARW = 48128
        AR = Arena(st.enter_context(nc.sbuf_tensor("arena", [128, ARW], F32)), ARW)

        def new_phase():
            P.fence()
            AR.reset()

        ident = cst.a[:, 0, :]
        M_G = cst.a[:, 2, :]
        M_D = cst.a[:, 3, :]
        iota = cst.a[:, 4, :]

        def rstd_of(ss, inv_n, tmp, r, w):
            ACT(tmp, ss, AF.Sqrt, bias=epsc.a[:, 0:1], scale=inv_n, r=r + [epsc], w=w)
            RECIP(tmp, tmp, r=w, w=w)

        stage = AR.take([128, 128])
        DMA("sp", cst.a[:], cst_d.ap(), w=[cst])
        DMA("sp", flag.a[:], flag_d.ap(), w=[flag])
        CP("dve", identb.a[:], ident, r=[cst], w=[identb])
        MSET("dve", epsc.a[:], EPS, w=[epsc])
        MSET("dve", onec.a[:], 1.0, w=[onec])
        DMA("sp", stage.a[0:16, :], c_d.ap(), w=[stage])
        TR(PSB[0].a[:, 0:16], stage.a[0:16, :], cst.a[0:16, 0, 0:16], r=[stage, cst], w=[PSB[0]])
        ACT(condT.a[:, :, 0], PSB[0].a[:, 0:16], AF.Silu, r=[PSB[0]], w=[condT])
        ACT(condT.a[:, :, 1], PSB[0].a[:, 0:16], AF.Silu, r=[PSB[0]], w=[condT])

        def setup_layer(l):
            new_phase()
            stage = AR.take([128, 128])
            gtT = AR.take([128, 32])
            gtrow = AR.take([32, 128])
            aw = [AR.take([128, DC, 256]) for _ in range(2)]
            DMA("sp", stage.a[0:96, :], adab_d.ap()[l], w=[stage])
            DMA("sp", stage.a[96:112, :], n1g_d.ap()[l], w=[stage])
            DMA("sp", stage.a[112:128, :], n2g_d.ap()[l], w=[stage])
            TR(PSB[0].a[:, 0:128], stage.a[:, :], ident, r=[stage, cst], w=[PSB[0]])
            CP("dve", smallT.a[:], PSB[0].a[:, 0:128], r=[PSB[0]], w=[smallT])
            src = adaw_d.ap()[l].rearrange("(dc p) c -> p dc c", p=128)
            for cc in range(48):
                t = aw[cc % 2]
                DMA("sp" if cc % 2 == 0 else "act", t.a, src[:, :, cc * 256:(cc + 1) * 256], w=[t])
                for mm in range(2):
                    m = cc * 2 + mm
                    for dc in range(DC):
                        MM(PSB[1].a[:, 2 * m:2 * m + 2], t.a[:, dc, mm * 128:(mm + 1) * 128],
                           condT.a[:, dc, :], start=(dc == 0), stop=(dc == DC - 1),
                           r=[t, condT], w=[PSB[1]])
            TT("dve", modT.a[:], v3(PSB[1].a[:, 0:192], 2)[:, :, 0], smallT.a[:, 0:96], ALU.add,
               r=[PSB[1], smallT], w=[modT])
            STT(AB.a[:, 0, :], modT.a[:, 16:32], 1.0, smallT.a[:, 96:112], ALU.add, ALU.mult,
                r=[modT, smallT], w=[AB])
            STT(AB.a[:, 1, :], modT.a[:, 64:80], 1.0, smallT.a[:, 112:128], ALU.add, ALU.mult,
                r=[modT, smallT], w=[AB])
            CP("dve", gtT.a[:, 0:16], modT.a[:, 32:48], r=[modT], w=[gtT])
            CP("dve", gtT.a[:, 16:32], modT.a[:, 80:96], r=[modT], w=[gtT])
            TR(PSB[0].a[0:32, 0:128], gtT.a[:, :], ident, r=[gtT, cst], w=[PSB[0]])
            CP("dve", gtrow.a[:, :], PSB[0].a[0:32, 0:128], r=[PSB[0]], w=[gtrow])
            DMA("sp", modrow_d.ap()[l], gtrow.a[:, :], r=[gtrow], w=[dbuf("modrow", l)])
            wst = [AR.take([128, DC, 512], BF16) for _ in range(2)]
            win_v = win_d.ap()[l].rearrange("(dc p) c -> p dc c", p=128)
            wout_v = wout_d.ap()[l].rearrange("(mc p) c -> p mc c", p=128)
            jobs = [(win_v, winb_d, k, c0, n) for k, (c0, n) in enumerate(WCH)]
            jobs += [(wout_v, woutb_d, k, k * 512, 512) for k in range(4)]
            for q, (sv_, dd, k, c0, n) in enumerate(jobs):
                t = wst[q % 2]
                DMA("pool", t.a[:, :, 0:n], sv_[:, :, c0:c0 + n], w=[t])
                DMA("sp", dd.ap()[k, :, 0:DC * n].rearrange("p (a b) -> p a b", b=n), t.a[:, :, 0:n],
                    r=[t], w=[dbuf(dd.name)])

        def tr8(srcd_ap, dst, masked, kin):
            DMA("sp", kin.a, srcd_ap, w=[kin])
            for h in range(8):
                bk = PSB[2 + h // 4]
                TR(bk.a[:, (h % 4) * 128:(h % 4 + 1) * 128], kin.a[:, h, :], ident, r=[kin, cst], w=[bk])
            for hb in range(2):
                bk = PSB[2 + hb]
                if masked:
                    TT("dve", dst.a[:, hb * 4:(hb + 1) * 4, :], v3(bk.a[:, :], 128),
                       cst.a[:, 1:2, :].to_broadcast([128, 4, 128]), ALU.mult, r=[bk, cst], w=[dst])
                else:
                    CP("dve", dst.a[:, hb * 4:(hb + 1) * 4, :], v3(bk.a[:, :], 128), r=[bk], w=[dst])

        def norm_T(x_ap, xdep, which, bshift, dst, dst_fn, xnb, junk):
            ACT(junk.a[:, :], x_ap, AF.Square, accum=st8.a[:, 0:1], r=[xdep], w=[junk, st8])
            rstd_of(st8.a[:, 0:1], 1.0 / D, st8.a[:, 1:2], r=[st8], w=[st8])
            TS("dve", xnb.a[:, :], x_ap, st8.a[:, 1:2], None, ALU.mult, r=[xdep, st8], w=[xnb])
            for dc in range(DC):
                bk = PSB[dc // 8]
                TR(psbf(dc // 8)[:, (dc % 8) * 128:(dc % 8 + 1) * 128], xnb.a[:, dc * 128:(dc + 1) * 128],
                   identb.a[:], r=[xnb, identb], w=[bk])
            for dc in range(DC):
                bk = PSB[dc // 8]
                src = psbf(dc // 8)[:, (dc % 8) * 128:(dc % 8 + 1) * 128]
                if dc // 8 == 0:
                    TS("dve", dst_fn(dc), src, AB.a[:, which, dc:dc + 1],
                       modT.a[:, bshift + dc:bshift + dc + 1], ALU.mult, ALU.add,
                       r=[bk, AB, modT], w=[dst])
                else:
                    ACT(dst_fn(dc), src, AF.Identity, bias=modT.a[:, bshift + dc:bshift + dc + 1],
                        scale=AB.a[:, which, dc:dc + 1], r=[bk, AB, modT], w=[dst])

        def mixer_layer(l, xsrc, xdst):
            new_phase()
            ngr = AR.take([128, 1024])
            vgr = AR.take([128, 1024])
            ogr = AR.take([128, 1024])
            W2a = AR.take([17, 512])
            wsT = AR.take([128, 8, 128], BF16)
            bT = AR.take([128, 8])
            S32 = AR.take([128, 4, 256])
            Sbf = AR.take([128, 4, 256], BF16)
            xt = [AR.take([128, D]) for _ in range(2)]
            xnb = AR.take([128, D], BF16)
            junk = AR.take([128, D], BF16)
            wc = [AR.take([128, DC, 512], BF16) for _ in range(2)]
            hTg = AR.take([128, DC, 512], BF16)
            ymT = hTg
            alT = AR.take([17, 512])
            GTg = AR.take([128, 4, 4, 128])
            EGl = AR.take([128, 4, 4])
            EDg = AR.take([128, 4, 512], BF16)
            qtg = AR.take([128, 4, 512], BF16)
            ktg = AR.take([128, 4, 512], BF16)
            kdg = AR.take([128, 4, 512], BF16)
            vg_ = AR.take([128, 4, 1024], BF16)
            srg = AR.take([128, 4, 1024], BF16)
            gug = AR.take([128, 4, 1024], BF16)
            gvg = AR.take([128, 4, 1024], BF16)
            lt = AR.take([128, 512])
            et = AR.take([128, 512])
            attm = AR.take([128, 4, 128], BF16)
            ymix = AR.take([128, 1024], BF16)
            yg = AR.take([128, 4, 1024], BF16)
            f1 = AR.take([128, 1024])
            f2 = AR.take([128, 1024])
            vnb = AR.take([128, 1024], BF16)
            xp = [AR.take([128, 512]) for _ in range(2)]
            xo = [AR.take([128, 512]) for _ in range(2)]
            stage = AR.take([8, 128])
            kin = X(f1.a[:, :].rearrange("p (a b) -> p a b", b=128))
            kin.b = f1.b

            DMA("sp", GTR.a[:], modrow_d.ap()[l, 0:16, :].rearrange("a b -> (a b)").unsqueeze(0).to_broadcast([128, D]),
                r=[dbuf("modrow", l)], w=[GTR])
            DMA("sp", W2a.a[0:16, :], wa2_d.ap()[l], w=[W2a])
            DMA("sp", W2a.a[16:17, :], ba_d.ap()[l], w=[W2a])
            DMA("sp", ngr.a[:, :], gng_d.ap()[l].to_broadcast([128, 1024]), w=[ngr])
            DMA("sp", vgr.a[:, :], vng_d.ap()[l].to_broadcast([128, 1024]), w=[vgr])
            DMA("sp", ogr.a[:, :], og_d.ap()[l].to_broadcast([128, 1024]), w=[ogr])
            tr8(ws_d.ap()[l].rearrange("h t s -> t h s"), wsT, True, kin)
            DMA("sp", stage.a[0:8, :], gb_d.ap()[l], w=[stage])
            TR(PSB[0].a[:, 0:8], stage.a[0:8, :], cst.a[0:8, 0, 0:8], r=[stage, cst], w=[PSB[0]])
            CP("dve", bT.a[:, :], PSB[0].a[:, 0:8], r=[PSB[0]], w=[bT])
            MSET("dve", S32.a[:], 0.0, w=[S32])
            MSET("pool", Sbf.a[:], 0.0, w=[Sbf])
            MSET("dve", alT.a[:, :], 1.0, w=[alT])

            wcnt = [0]

            def load_w(src_ap3, ncols):
                t = wc[wcnt[0] % 2]
                wcnt[0] += 1
                DMA("sp", t.a[:, :, 0:ncols], src_ap3, r=[dbuf("winb"), dbuf("woutb")], w=[t])
                return t

            class _WO:
                def __getitem__(self, idx):
                    c0 = idx[2].start
                    return woutb_d.ap()[c0 // 512].rearrange("p (a b) -> p a b", b=512)
            wo_src = _WO()

            def win_cols(c0, n):
                return winb_d.ap()[WIDX[c0], :, 0:DC * n].rearrange("p (a b) -> p a b", b=n)

            last = (l == NL - 1)
            for g in range(NT // 4):
                t0 = g * 4
                so = split and last and g < HALF // 4
                if split and g == HALF // 4:
                    TS("dve", S32.a[:, :, :], S32.a[:, :, :], flag.a[:, 0:1], None, ALU.mult, r=[S32, flag], w=[S32])
                    CP("pool", Sbf.a[:, :, :], S32.a[:, :, :], r=[S32], w=[Sbf])
                DMA("sp", xt[0].a[:, :], xsrc.ap()[t0 * 128:(t0 + 1) * 128, :], r=[dbuf(xsrc.name, t0)], w=[xt[0]])
                for j in range(4):
                    if j + 1 < 4:
                        s = (j + 1) % 2
                        DMA("sp", xt[s].a[:, :], xsrc.ap()[(t0 + j + 1) * 128:(t0 + j + 2) * 128, :],
                            r=[dbuf(xsrc.name, t0 + j + 1)], w=[xt[s]])
                    norm_T(xt[j % 2].a[:, :], xt[j % 2], 0, 0, hTg,
                           lambda dc, j=j: hTg.a[:, dc, j * 128:(j + 1) * 128], xnb, junk)
                wal = load_w(win_cols(C_AL, 16), 16)
                wq_ = load_w(win_cols(C_K if so else C_Q, 512), 512)
                for dc in range(DC):
                    MM(PSB[2].a[0:16, :], wal.a[:, dc, 0:16], hTg.a[:, dc, :], start=(dc == 0), stop=(dc == DC - 1),
                       r=[wal, hTg], w=[PSB[2]])
                CP("dve", alT.a[0:16, :], PSB[2].a[0:16, :], r=[PSB[2]], w=[alT])
                for j in range(4):
                    tk = slice(j * 128, (j + 1) * 128)
                    MM(PSB[4].a[:, :], alT.a[:, tk], W2a.a[:, :], r=[alT, W2a], w=[PSB[4]])
                    ACT(et.a[:, :], PSB[4].a[:, :], AF.Exp, scale=-1.0, r=[PSB[4]], w=[et])
                    ACT(lt.a[:, :], et.a[:, :], AF.Ln, bias=onec.a[:, 0:1], scale=1.0, r=[et, onec], w=[lt])
                    MM(PSB[4].a[:, :], M_D, lt.a[:, :], r=[cst, lt], w=[PSB[4]])
                    ACT(EDg.a[:, j, :], PSB[4].a[:, :], AF.Exp, r=[PSB[4]], w=[EDg])
                    for h in range(4):
                        MM(PSB[5].a[:, h * 128:(h + 1) * 128], lt.a[:, h * 128:(h + 1) * 128], M_G,
                           r=[lt, cst], w=[PSB[5]])
                    CP("dve", GTg.a[:, j, :, :], v3(PSB[5].a[:, :], 128), r=[PSB[5]], w=[GTg])
                ACT(EGl.a[:, :, :], GTg.a[:, :, :, 127], AF.Exp, r=[GTg], w=[EGl])
                if so:
                    wk_ = wq_
                    wv0 = load_w(win_cols(C_V, 512), 512)
                    for j in range(4):
                        bk = PSB[2 + j % 2]
                        for dc in range(DC):
                            MM(bk.a[:, :], hTg.a[:, dc, j * 128:(j + 1) * 128], wk_.a[:, dc, :],
                               start=(dc == 0), stop=(dc == DC - 1), r=[wk_, hTg], w=[bk])
                        TT("dve", kdg.a[:, j, :], bk.a[:, :], EDg.a[:, j, :], ALU.mult, r=[bk, EDg], w=[kdg])
                    wv1 = load_w(win_cols(C_V + 512, 512), 512)
                    for (wv, off) in ((wv0, 0), (wv1, 512)):
                        for j in range(4):
                            bk = PSB[2 + j % 2]
                            for dc in range(DC):
                                MM(bk.a[:, :], hTg.a[:, dc, j * 128:(j + 1) * 128], wv.a[:, dc, :],
                                   start=(dc == 0), stop=(dc == DC - 1), r=[wv, hTg], w=[bk])
                            CP("act", vg_.a[:, j, off:off + 512], bk.a[:, :], r=[bk], w=[vg_])
                    for j in range(4):
                        for h in range(4):
                            bk = PSB[h // 2]
                            oc = slice((h % 2) * 256, (h % 2 + 1) * 256)
                            MM(bk.a[:, oc], kdg.a[:, j, h * 128:(h + 1) * 128], vg_.a[:, j, h * 256:(h + 1) * 256],
                               r=[kdg, vg_], w=[bk])
                        for h in range(4):
                            bk = PSB[h // 2]
                            oc = slice((h % 2) * 256, (h % 2 + 1) * 256)
                            STT(S32.a[:, h, :], S32.a[:, h, :], EGl.a[:, j, h:h + 1], bk.a[:, oc], ALU.mult, ALU.add,
                                r=[S32, EGl, bk], w=[S32])
                    CP("pool", Sbf.a[:, :, :], S32.a[:, :, :], r=[S32], w=[Sbf])
                    continue
                wk_ = load_w(win_cols(C_K, 512), 512)
                for h in range(4):
                    bk = PSB[2 + h % 2]
                    for dc in range(DC):
                        MM(bk.a[:, :], wq_.a[:, dc, h * 128:(h + 1) * 128], hTg.a[:, dc, :],
                           start=(dc == 0), stop=(dc == DC - 1), r=[wq_, hTg], w=[bk])
                    ACT(v3(et.a[:, :], 128), GTg.a[:, :, h, :], AF.Exp, r=[GTg], w=[et])
                    STT(qtg.a[:, h, :], bk.a[:, :], 128.0 ** -0.5, et.a[:, :], ALU.mult, ALU.mult,
                        r=[bk, et], w=[qtg])
                wv0 = load_w(win_cols(C_V, 512), 512)
                for h in range(4):
                    bk = PSB[2 + h % 2]
                    for dc in range(DC):
                        MM(bk.a[:, :], wk_.a[:, dc, h * 128:(h + 1) * 128], hTg.a[:, dc, :],
                           start=(dc == 0), stop=(dc == DC - 1), r=[wk_, hTg], w=[bk])
                    ACT(v3(et.a[:, :], 128), GTg.a[:, :, h, :], AF.Exp, scale=-1.0, r=[GTg], w=[et])
                    TT("dve", ktg.a[:, h, :], bk.a[:, :], et.a[:, :], ALU.mult, r=[bk, et], w=[ktg])
                for j in range(4):
                    bk = PSB[2 + j % 2]
                    for dc in range(DC):
                        MM(bk.a[:, :], hTg.a[:, dc, j * 128:(j + 1) * 128], wk_.a[:, dc, :],
                           start=(dc == 0), stop=(dc == DC - 1), r=[wk_, hTg], w=[bk])
                    TT("dve", kdg.a[:, j, :], bk.a[:, :], EDg.a[:, j, :], ALU.mult, r=[bk, EDg], w=[kdg])
                def gla_tile(j):
                    tk = slice(j * 128, (j + 1) * 128)
                    for h in range(4):
                        MM(PSB[5].a[:, h * 128:(h + 1) * 128], ktg.a[:, h, tk], qtg.a[:, h, tk],
                           r=[ktg, qtg], w=[PSB[5]])
                    TT("dve", attm.a[:, :, :], v3(PSB[5].a[:, :], 128), cst.a[:, 1:2, :].to_broadcast([128, 4, 128]),
                       ALU.mult, r=[PSB[5], cst], w=[attm])
                    for h in range(4):
                        bk = PSB[6 + h // 2]
                        oc = slice((h % 2) * 256, (h % 2 + 1) * 256)
                        MM(bk.a[:, oc], attm.a[:, h, :], vg_.a[:, j, h * 256:(h + 1) * 256], start=True, stop=False,
                           r=[attm, vg_], w=[bk])
                        MM(bk.a[:, oc], qtg.a[:, h, tk], Sbf.a[:, h, :], start=False, stop=True,
                           r=[qtg, Sbf], w=[bk])
                    for h in range(4):
                        bk = PSB[h // 2]
                        oc = slice((h % 2) * 256, (h % 2 + 1) * 256)
                        MM(bk.a[:, oc], kdg.a[:, j, h * 128:(h + 1) * 128], vg_.a[:, j, h * 256:(h + 1) * 256],
                           r=[kdg, vg_], w=[bk])
                    for h in range(4):
                        bk = PSB[h // 2]
                        oc = slice((h % 2) * 256, (h % 2 + 1) * 256)
                        STT(S32.a[:, h, :], S32.a[:, h, :], EGl.a[:, j, h:h + 1], bk.a[:, oc], ALU.mult, ALU.add,
                            r=[S32, EGl, bk], w=[S32])
                    CP("pool", Sbf.a[:, :, :], S32.a[:, :, :], r=[S32], w=[Sbf])
                    for h in range(4):
                        bk = PSB[6 + h // 2]
                        oc = slice((h % 2) * 256, (h % 2 + 1) * 256)
                        ACT(junk.a[:, 0:256], bk.a[:, oc], AF.Square, accum=st8b.a[:, h:h + 1], r=[bk], w=[junk, st8b])
                    rstd_of(st8b.a[:, 0:4], 1.0 / 256, st8b.a[:, 4:8], r=[st8b], w=[st8b])
                    for h in range(4):
                        bk = PSB[6 + h // 2]
                        oc = slice((h % 2) * 256, (h % 2 + 1) * 256)
                        STT(f1.a[:, h * 256:(h + 1) * 256], bk.a[:, oc], st8b.a[:, 4 + h:5 + h],
                            ngr.a[:, h * 256:(h + 1) * 256], ALU.mult, ALU.mult, r=[bk, st8b, ngr], w=[f1])
                    TT("dve", yg.a[:, j, :], f1.a[:, :], srg.a[:, j, :], ALU.mult, r=[f1, srg], w=[yg])
                def gmlp_tile(j):
                    tk = slice(j * 128, (j + 1) * 128)
                    TT("dve", f1.a[:, :], gvg.a[:, j, :], gvg.a[:, j, :], ALU.mult, r=[gvg], w=[f1])
                    RED(st8b.a[:, 8:16], v3(f1.a[:, :], 128), ALU.add, r=[f1], w=[st8b])
                    rstd_of(st8b.a[:, 8:16], 1.0 / 128, st8b.a[:, 16:24], r=[st8b], w=[st8b])
                    TT("dve", v3(f1.a[:, :], 128), v3(gvg.a[:, j, :], 128),
                       st8b.a[:, 16:24].unsqueeze(2).to_broadcast([128, 8, 128]), ALU.mult, r=[gvg, st8b], w=[f1])
                    TT("dve", vnb.a[:, :], f1.a[:, :], vgr.a[:, :], ALU.mult, r=[f1, vgr], w=[vnb])
                    for h in range(8):
                        bk = PSB[6 + h // 4]
                        MM(bk.a[:, (h % 4) * 128:(h % 4 + 1) * 128], wsT.a[:, h, :], vnb.a[:, h * 128:(h + 1) * 128],
                           r=[wsT, vnb], w=[bk])
                    for hb in range(2):
                        bk = PSB[6 + hb]
                        TT("dve", v3(f2.a[:, hb * 512:(hb + 1) * 512], 128), v3(bk.a[:, :], 128),
                           bT.a[:, hb * 4:(hb + 1) * 4].unsqueeze(2).to_broadcast([128, 4, 128]), ALU.add,
                           r=[bk, bT], w=[f2])
                    TT("dve", f2.a[:, :], f2.a[:, :], gug.a[:, j, :], ALU.mult, r=[f2, gug], w=[f2])
                    TT("dve", f1.a[:, :], f2.a[:, :], f2.a[:, :], ALU.mult, r=[f2], w=[f1])
                    RED(st8b.a[:, 24:32], v3(f1.a[:, :], 128), ALU.add, r=[f1], w=[st8b])
                    rstd_of(st8b.a[:, 24:32], 1.0 / 128, st8b.a[:, 32:40], r=[st8b], w=[st8b])
                    TT("dve", v3(f2.a[:, :], 128), v3(f2.a[:, :], 128),
                       st8b.a[:, 32:40].unsqueeze(2).to_broadcast([128, 8, 128]), ALU.mult, r=[f2, st8b], w=[f2])
                    TT("dve", ymix.a[:, :], f2.a[:, :], ogr.a[:, :], ALU.mult, r=[f2, ogr], w=[ymix])
                    for mc in range(DC):
                        bk = PSB[mc // 8]
                        TR(psbf(mc // 8)[:, (mc % 8) * 128:(mc % 8 + 1) * 128], (yg.a[:, j, mc * 128:(mc + 1) * 128] if mc < 8 else ymix.a[:, (mc - 8) * 128:(mc - 7) * 128]),
                           identb.a[:], r=[yg, ymix, identb], w=[bk])
                    for hb in range(2):
                        bk = PSB[hb]
                        CP("act", ymT.a[:, hb * 8:(hb + 1) * 8, tk], v3(psbf(hb), 128), r=[bk], w=[ymT])
                chunks = [(C_V, vg_, 0, None), (C_V + 512, vg_, 512, None),
                          (C_R, srg, 0, AF.Silu), (C_R + 512, srg, 512, AF.Silu),
                          (C_U, gug, 0, AF.Gelu_apprx_tanh), (C_U + 512, gug, 512, AF.Gelu_apprx_tanh),
                          (C_VS, gvg, 0, AF.Gelu_apprx_tanh), (C_VS + 512, gvg, 512, AF.Gelu_apprx_tanh)]
                wcur = wv0
                for ci, (c0, dstt, off, fn) in enumerate(chunks):
                    if ci + 1 < len(chunks):
                        nxt = load_w(win_cols(chunks[ci + 1][0], 512), 512)
                    else:
                        nxt = load_w(wo_src[:, :, 0:512], 512)
                    for j in range(4):
                        bk = PSB[2 + j % 2]
                        for dc in range(DC):
                            MM(bk.a[:, :], hTg.a[:, dc, j * 128:(j + 1) * 128], wcur.a[:, dc, :],
                               start=(dc == 0), stop=(dc == DC - 1), r=[wcur, hTg], w=[bk])
                        if fn is None:
                            CP("act", dstt.a[:, j, off:off + 512], bk.a[:, :], r=[bk], w=[dstt])
                        else:
                            ACT(dstt.a[:, j, off:off + 512], bk.a[:, :], fn, r=[bk], w=[dstt])
                    wcur = nxt
                    if ci >= 4:
                        if not opts.get('skip6'):
                            gla_tile(ci - 4)
                wo = wcur
                for j in range(0 if opts.get('skip6') else 4):
                    gmlp_tile(j)
                for cg in range(0 if opts.get('skip7') else 4):
                    wnext = None
                    if cg + 1 < 4:
                        wnext = load_w(wo_src[:, :, (cg + 1) * 512:(cg + 2) * 512], 512)
                    for j in range(4):
                        tile = t0 + j
                        s = (cg * 4 + j) % 2
                        DMA("sp", xp[s].a[:, :], xsrc.ap()[tile * 128:(tile + 1) * 128, cg * 512:(cg + 1) * 512],
                            r=[dbuf(xsrc.name, tile)], w=[xp[s]])
                        bk = PSB[2 + j % 2]
                        for mc in range(DC):
                            MM(bk.a[:, :], ymT.a[:, mc, j * 128:(j + 1) * 128], wo.a[:, mc, :],
                               start=(mc == 0), stop=(mc == DC - 1), r=[ymT, wo], w=[bk])
                        TT("dve", xo[s].a[:, :], bk.a[:, :], GTR.a[:, cg * 512:(cg + 1) * 512], ALU.mult,
                           r=[bk, GTR], w=[xo[s]])
                        TT("pool", xo[s].a[:, :], xo[s].a[:, :], xp[s].a[:, :], ALU.add, r=[xo[s], xp[s]], w=[xo[s]])
                        DMA("sp", xdst.ap()[tile * 128:(tile + 1) * 128, cg * 512:(cg + 1) * 512], xo[s].a[:, :],
                            r=[xo[s]], w=[dbuf(xdst.name, tile)])
                    wo = wnext

        def peer_layer(l, xsrc, xdst):
            new_phase()
            ub = [AR.take([128, D], BF16) for _ in range(2)]
            uo = [AR.take([128, D], BF16) for _ in range(2)]
            vb_ = [AR.take([128, D], BF16) for _ in range(2)]
            wq_src = wq_d.ap()[l].rearrange("(dc p) c -> p dc c", p=128)
            wst = [AR.take([128, DC, 512], BF16) for _ in range(2)]
            for cg in range(4):
                DMA("pool", wst[cg % 2].a[:, :, :], wq_src[:, :, cg * 512:(cg + 1) * 512], w=[wst[cg % 2]])
                DMA("sp", wqb_d.ap()[cg], wst[cg % 2].a[:, :, :].rearrange("p a b -> p (a b)"),
                    r=[wst[cg % 2]], w=[dbuf("wqb", cg)])
            for i in range(128):
                s = i % 2
                DMA("pool", ub[s].a[:, :], pu_d.ap()[l, i * 128:(i + 1) * 128, :], w=[ub[s]])
                DMA("pool", vb_[s].a[:, :], pv_d.ap()[l, i * 128:(i + 1) * 128, :], w=[vb_[s]])
                DMA("sp", vb_d.ap()[i], vb_[s].a[:, :], r=[vb_[s]], w=[dbuf("vb", i)])
                for dc in range(DC):
                    bi = 4 + dc // 8 + 2 * s
                    TR(psbf(bi)[:, (dc % 8) * 128:(dc % 8 + 1) * 128],
                       ub[s].a[:, dc * 128:(dc + 1) * 128], identb.a[:], r=[ub[s], identb], w=[PSB[bi]])
                CP("act", uo[s].a[:, 0:1024], psbf(4 + 2 * s), r=[PSB[4 + 2 * s]], w=[uo[s]])
                CP("dve", uo[s].a[:, 1024:2048], psbf(5 + 2 * s), r=[PSB[5 + 2 * s]], w=[uo[s]])
                DMA("sp", ut_d.ap()[i], uo[s].a[:, :], r=[uo[s]], w=[dbuf("ut", i)])

            new_phase()
            k1T = AR.take([128, 8, 128])
            k2T = AR.take([128, 8, 128])
            h2T = AR.take([128, DC, 256], BF16)
            sc5T = AR.take([128, 5, 256])
            GA = AR.take([128, 128, 256], BF16)
            xblk = AR.take([128, 2, D])
            base = AR.off
            kin = X(xblk.a[:, 0, 0:1024].rearrange("p (a b) -> p a b", b=128))
            kin.b = xblk.b
            tr8(k1_d.ap()[l].rearrange("h k d -> k h d"), k1T, False, kin)
            tr8(k2_d.ap()[l].rearrange("h k d -> k h d"), k2T, False, kin)
            DMA("sp", GTR.a[:], modrow_d.ap()[l, 16:32, :].rearrange("a b -> (a b)").unsqueeze(0).to_broadcast([128, D]),
                r=[dbuf("modrow", l)], w=[GTR])

            for blk in range(HALF // 2 if (split and l == NL - 1) else 0, NT // 2):
                tb0 = blk * 2
                P.fence()
                AR.off = base
                xnb = AR.take([128, D], BF16)
                junk = AR.take([128, D], BF16)
                wcq = [AR.take([128, DC, 512], BF16) for _ in range(2)]
                qT = AR.take([128, 16, 256])
                ssb = [AR.take([128, 1024]) for _ in range(2)]
                v12 = AR.take([128, 2, 8, 16])
                idx = AR.take([128, 8, 16], mybir.dt.uint32)
                tmpA = AR.take([128, 256])
                cand = AR.take([128, 8, 256])
                sc = AR.take([128, 8, 24])
                tm5 = AR.take([128, 4, 128])
                sm = AR.take([128, 64])
                ex16 = AR.take([128, 8, 16])
                for ts in range(2):
                    tile = tb0 + ts
                    DMA("sp", xblk.a[:, ts, :], xsrc.ap()[tile * 128:(tile + 1) * 128, :],
                        r=[dbuf(xsrc.name, tile)], w=[xblk])
                for ts in range(2):
                    norm_T(xblk.a[:, ts, :], xblk, 1, 48, h2T,
                           lambda dc, ts=ts: h2T.a[:, dc, ts * 128:(ts + 1) * 128], xnb, junk)
                def load_wq(cg):
                    DMA("sp", wcq[cg % 2].a[:, :, :].rearrange("p a b -> p (a b)"), wqb_d.ap()[cg],
                        r=[dbuf("wqb", cg)], w=[wcq[cg % 2]])
                load_wq(0)
                for cg in range(4):
                    if cg + 1 < 4:
                        load_wq(cg + 1)
                    wq_ = wcq[cg % 2]
                    for cc in range(4):
                        bk = PSB[2 + cc % 2]
                        half = (cc // 2 % 2) * 256
                        for dc in range(DC):
                            MM(bk.a[:, half:half + 256], wq_.a[:, dc, cc * 128:(cc + 1) * 128], h2T.a[:, dc, :],
                               start=(dc == 0), stop=(dc == DC - 1), r=[wq_, h2T], w=[bk])
                        CP("act", qT.a[:, cg * 4 + cc, :], bk.a[:, half:half + 256], r=[bk], w=[qT])
                for ts in range(2):
                    tile = tb0 + ts
                    tk = slice(ts * 128, (ts + 1) * 128)
                    for side in range(2):
                        kT = k1T if side == 0 else k2T
                        for h in range(8):
                            bk = PSB[4 + side * 2 + h // 4]
                            MM(bk.a[:, (h % 4) * 128:(h % 4 + 1) * 128], qT.a[:, 2 * h + side, tk], kT.a[:, h, :],
                               r=[qT, kT], w=[bk])
                        for hb in range(2):
                            bk = PSB[4 + side * 2 + hb]
                            CP("act", ssb[side].a[:, hb * 512:(hb + 1) * 512], bk.a[:, :], r=[bk], w=[ssb[side]])
                        if side == 1:
                            DMA("sp", s2_d.ap()[tile * 128:(tile + 1) * 128],
                                ssb[1].a[:, :].unsqueeze(1).to_broadcast([128, 16, 1024]),
                                r=[ssb[1]], w=[dbuf("s2", tile)])
                        for h in range(8):
                            sv = ssb[side].a[:, h * 128:(h + 1) * 128]
                            MAX8(v12.a[:, side, h, 0:8], sv, r=[ssb[side]], w=[v12])
                            if side == 0:
                                MIDX(idx.a[:, h, 0:8], v12.a[:, 0, h, 0:8], sv, r=[ssb[0], v12], w=[idx])
                            MREP(tmpA.a[:, 0:128], v12.a[:, side, h, 0:8], sv, r=[ssb[side], v12], w=[tmpA])
                            MAX8(v12.a[:, side, h, 8:16], tmpA.a[:, 0:128], r=[tmpA], w=[v12])
                            if side == 0:
                                MIDX(idx.a[:, h, 8:16], v12.a[:, 0, h, 8:16], tmpA.a[:, 0:128], r=[tmpA, v12], w=[idx])
                    TT("dve", cand.a[:, :, :].rearrange("p h (a b) -> p h a b", b=16),
                       v12.a[:, 0, :, :].unsqueeze(3).to_broadcast([128, 8, 16, 16]),
                       v12.a[:, 1, :, :].unsqueeze(2).to_broadcast([128, 8, 16, 16]), ALU.add, r=[v12], w=[cand])
                    for h in range(8):
                        MAX8(sc.a[:, h, 0:8], cand.a[:, h, :], r=[cand], w=[sc])
                        MREP(tmpA.a[:, :], sc.a[:, h, 0:8], cand.a[:, h, :], r=[cand, sc], w=[tmpA])
                        MAX8(sc.a[:, h, 8:16], tmpA.a[:, :], r=[tmpA], w=[sc])
                        MREP(tmpA.a[:, :], sc.a[:, h, 8:16], tmpA.a[:, :], r=[tmpA, sc], w=[tmpA])
                        MAX8(sc.a[:, h, 16:24], tmpA.a[:, :], r=[tmpA], w=[sc])
                    TT("dve", sm.a[:, 0:8], sc.a[:, :, 15], sc.a[:, :, 16], ALU.add, r=[sc], w=[sm])
                    TS("dve", sm.a[:, 0:8], sm.a[:, 0:8], 0.5, None, ALU.mult, r=[sm], w=[sm])
                    TT("dve", ex16.a[:, :, :], sc.a[:, :, 0:16], sc.a[:, :, 0:1].to_broadcast([128, 8, 16]),
                       ALU.subtract, r=[sc], w=[ex16])
                    ACT(ex16.a[:, :, :], ex16.a[:, :, :], AF.Exp, r=[ex16], w=[ex16])
                    RED(sm.a[:, 8:16], ex16.a[:, :, :], ALU.add, r=[ex16], w=[sm])
                    RECIP(sm.a[:, 16:24], sm.a[:, 8:16], r=[sm], w=[sm])
                    v1v = v12.a[:, 0, :, :]
                    t4 = lambda k: tm5.a[:, k, :].rearrange("p (a h) -> p h a", h=8)
                    CP("dve", t4(0), idx.a[:, :, :], r=[idx], w=[tm5])
                    TT("dve", t4(1), v1v, v12.a[:, 0, :, 0:1].to_broadcast([128, 8, 16]), ALU.subtract,
                       r=[v12], w=[tm5])
                    ACT(t4(1), t4(1), AF.Exp, r=[tm5], w=[tm5])
                    TT("dve", t4(1), t4(1), sm.a[:, 16:24].unsqueeze(2).to_broadcast([128, 8, 16]), ALU.mult,
                       r=[tm5, sm], w=[tm5])
                    TT("dve", t4(2), sm.a[:, 0:8].unsqueeze(2).to_broadcast([128, 8, 16]), v1v, ALU.subtract,
                       r=[sm, v12], w=[tm5])
                    TS("dve", t4(3), v12.a[:, 1, :, 0:1].to_broadcast([128, 8, 16]), -1.0, None, ALU.mult,
                       r=[v12], w=[tm5])
                    for k in range(4):
                        TR(PSB[0].a[:, k * 128:(k + 1) * 128], tm5.a[:, k, :], ident, r=[tm5, cst], w=[PSB[0]])
                    CP("act", sc5T.a[:, 0:4, tk], v3(PSB[0].a[:, :], 128), r=[PSB[0]], w=[sc5T])
                P.fence()
                AR.off = base
                NSR = 3
                SR = [AR.take([128, 16, 128]) for _ in range(NSR)]
                Pt = [AR.take([128, 128], BF16) for _ in range(4)]
                Et = [AR.take([128, 128]) for _ in range(4)]
                Rt = [AR.take([128, 128], BF16) for _ in range(4)]

                def load_sr(bi):
                    sl = bi % NSR
                    tok0 = tb0 * 128 + bi * 16
                    tile = tok0 // 128
                    src = bass.AP(s2_d, tok0 * 16384, [[128, 128], [16384, 16], [1, 128]])
                    DMA("sp", SR[sl].a[:, :, :], src, r=[dbuf("s2", tile)], w=[SR[sl]])
                load_sr(0)
                load_sr(1)
                for bi in range(0 if opts.get('skipB') else 16):
                    if bi + 2 < 16:
                        load_sr(bi + 2)
                    sl = bi % NSR
                    for tt in range(16):
                        t = bi * 16 + tt
                        k2 = t % 4
                        col = lambda k, t=t: sc5T.a[:, k, t:t + 1]
                        TS("dve", Pt[k2].a[:, :], iota, col(0), col(1), ALU.is_equal, ALU.mult,
                           r=[cst, sc5T], w=[Pt[k2]])
                        ACT(Et[k2].a[:, :], SR[sl].a[:, tt, :], AF.Exp, bias=col(3), scale=1.0,
                            r=[SR[sl], sc5T], w=[Et[k2]])
                        STT(Rt[k2].a[:, :], SR[sl].a[:, tt, :], col(2), Et[k2].a[:, :], ALU.is_ge, ALU.mult,
                            r=[SR[sl], sc5T, Et[k2]], w=[Rt[k2]])
                        bk = PSB[4 + (t // 4) % 4]
                        MM(bk.a[:, (t % 4) * 128:(t % 4 + 1) * 128], Rt[k2].a[:, :], Pt[k2].a[:, :],
                           r=[Rt[k2], Pt[k2]], w=[bk])
                        if t % 4 == 3 and t >= 11:
                            te = t - 8
                            bke = PSB[4 + (te // 4) % 4]
                            CP("act", GA.a[:, :, te - 3:te + 1], bke.a[:, :].rearrange("p (t i) -> p i t", i=128),
                               r=[bke], w=[GA])
                if not opts.get('skipB'):
                    for te in (251, 255):
                        bke = PSB[4 + (te // 4) % 4]
                        CP("act", GA.a[:, :, te - 3:te + 1], bke.a[:, :].rearrange("p (t i) -> p i t", i=128),
                           r=[bke], w=[GA])
                P.fence()
                AR.off = base
                NUT, NVV = 3, 4
                gz = [AR.take([128, 256], BF16) for _ in range(4)]
                utb = [AR.take([128, 2, DC * 128], BF16) for _ in range(NUT)]
                vvb = [AR.take([128, 2, 1024], BF16) for _ in range(NVV)]
                yo = [AR.take([128, 1024]) for _ in range(2)]
                ZS = [X(PSB[4 + k].a[:, 0:256]) for k in range(4)]

                def load_ut(pi):
                    DMA("sp", utb[pi % NUT].a[:, :, :],
                        ut_d.ap()[2 * pi:2 * pi + 2].rearrange("i p f -> p i f"),
                        r=[dbuf("ut", 2 * pi), dbuf("ut", 2 * pi + 1)], w=[utb[pi % NUT]])
                if not opts.get('skipC'):
                    load_ut(0)
                    load_ut(1)
                for i in range(0 if opts.get('skipC') else 128):
                    if i % 2 == 0 and i // 2 + 2 < 64:
                        load_ut(i // 2 + 2)
                    ub_ = utb[(i // 2) % NUT]
                    zs = ZS[i % 4]
                    for dc in range(DC):
                        MM(zs.a, ub_.a[:, i % 2, dc * 128:(dc + 1) * 128], h2T.a[:, dc, :],
                           start=(dc == 0), stop=(dc == DC - 1), r=[ub_, h2T], w=[zs])
                    ACT(gz[i % 4].a[:, :], zs.a, AF.Gelu_apprx_tanh, r=[zs], w=[gz[i % 4]])
                    TT("dve", GA.a[:, i, :], gz[i % 4].a[:, :], GA.a[:, i, :], ALU.mult, r=[gz[i % 4], GA], w=[GA])
                for dh in range(0 if opts.get('skipC') else 2):
                    def load_v(pi, dh=dh):
                        DMA("sp", vvb[pi % NVV].a[:, :, :],
                            vb_d.ap()[2 * pi:2 * pi + 2, :, dh * 1024:(dh + 1) * 1024].rearrange("i p f -> p i f"),
                            r=[dbuf("vb", 2 * pi), dbuf("vb", 2 * pi + 1)], w=[vvb[pi % NVV]])
                    load_v(0)
                    load_v(1)
                    load_v(2)
                    for i in range(128):
                        if i % 2 == 0 and i // 2 + 3 < 64:
                            load_v(i // 2 + 3)
                        vv_ = vvb[(i // 2) % NVV]
                        for ts in range(2):
                            for cq in range(2):
                                bk = PSB[ts * 2 + cq]
                                MM(bk.a[:, :], GA.a[:, i, ts * 128:(ts + 1) * 128],
                                   vv_.a[:, i % 2, cq * 512:(cq + 1) * 512],
                                   start=(i == 0), stop=(i == 127), r=[GA, vv_], w=[bk])
                    for ts in range(2):
                        tile = tb0 + ts
                        y_ = yo[ts]
                        for cq in range(2):
                            bk = PSB[ts * 2 + cq]
                            c0 = dh * 1024 + cq * 512
                            TT("dve", y_.a[:, cq * 512:(cq + 1) * 512], bk.a[:, :], GTR.a[:, c0:c0 + 512], ALU.mult,
                               r=[bk, GTR], w=[y_])
                        TT("pool", y_.a[:, :], y_.a[:, :], xblk.a[:, ts, dh * 1024:(dh + 1) * 1024], ALU.add,
                           r=[y_, xblk], w=[y_])
                        DMA("sp", xdst.ap()[tile * 128:(tile + 1) * 128, dh * 1024:(dh + 1) * 1024], y_.a[:, :],
                            r=[y_], w=[dbuf(xdst.name, tile)])

        def tail(xsrc, norm):
            new_phase()
            xt = [AR.take([128, D]) for _ in range(2)]
            junk = AR.take([128, D], BF16)
            if norm:
                DMA("sp", GTR.a[:], fg_d.ap().to_broadcast([128, D]), w=[GTR])
            for tile in range(HALF, NT):
                s = tile % 2
                DMA("sp", xt[s].a[:, :], xsrc.ap()[tile * 128:(tile + 1) * 128, :], r=[dbuf(xsrc.name, tile)], w=[xt[s]])
                if norm:
                    ACT(junk.a[:, :], xt[s].a[:, :], AF.Square, accum=st8.a[:, 0:1], r=[xt[s]], w=[junk, st8])
                    rstd_of(st8.a[:, 0:1], 1.0 / D, st8.a[:, 1:2], r=[st8], w=[st8])
                    STT(xt[s].a[:, :], xt[s].a[:, :], st8.a[:, 1:2], GTR.a[:], ALU.mult, ALU.mult,
                        r=[xt[s], st8, GTR], w=[xt[s]])
                DMA("sp", out_d.ap()[(tile - HALF) * 128:(tile - HALF + 1) * 128, :], xt[s].a[:, :], r=[xt[s]],
                    w=[dbuf("out", tile)])

        done = False
        cur = x_d
        for l in range(NL):
            setup_layer(l)
            mixer_layer(l, cur, xa_d)
            if stop_after == ("mixer", l):
                tail(xa_d, False)
                done = True
                break
            peer_layer(l, xa_d, xb_d)
            cur = xb_d
            if stop_after == ("peer", l):
                tail(xb_d, False)
                done = True
                break
        if not done:
            tail(cur, True)
        P.finish("sp")
        P.emit()
    return nc


_CONSTS = None


def _consts():
    global _CONSTS
    if _CONSTS is None:
        c = np.zeros((128, 5, 128), np.float32)
        c[:, 4, :] = np.arange(128, dtype=np.float32)[None, :]
        c[:, 0, :] = np.eye(128, dtype=np.float32)
        tri = np.triu(np.ones((128, 128), np.float32))
        c[:, 1, :] = tri
        c[:, 2, :] = -tri / 16.0
        c[:, 3, :] = -(1.0 - tri) / 16.0
        _CONSTS = c
    return _CONSTS


def make_in_maps(inputs, ncores, ntok, split=False):
    f = lambda a: np.ascontiguousarray(np.asarray(a, dtype=np.float32))
    shared = {
        "ada_w": f(inputs["ada_w"]),
        "ada_b": f(inputs["ada_b"]).reshape(DEPTH, 96, 128),
        "norm1_g": f(inputs["norm1_g"]).reshape(DEPTH, 16, 128),
        "w_in": f(inputs["w_in"]),
        "gla_w_a2": f(inputs["gla_w_a2"]),
        "gla_b_a": f(inputs["gla_b_a"]).reshape(DEPTH, 1, 512),
        "gla_norm_g": f(inputs["gla_norm_g"]).reshape(DEPTH, 1, 1024),
        "gmlp_vnorm_g": f(inputs["gmlp_vnorm_g"]).reshape(DEPTH, 1, 1024),
        "gmlp_ws": f(inputs["gmlp_ws"]),
        "gmlp_b": f(inputs["gmlp_b"]),
        "gmlp_out_g": f(inputs["gmlp_out_g"]).reshape(DEPTH, 1, 1024),
        "w_out": f(inputs["w_out"]),
        "norm2_g": f(inputs["norm2_g"]).reshape(DEPTH, 16, 128),
        "peer_wq": f(inputs["peer_wq"]),
        "peer_k1": f(inputs["peer_k1"]),
        "peer_k2": f(inputs["peer_k2"]),
        "peer_u": f(inputs["peer_u"]),
        "peer_v": f(inputs["peer_v"]),
        "final_g": f(inputs["final_g"]).reshape(1, D),
        "consts": _consts(),
    }
    x = f(inputs["x"])
    c = f(inputs["c"])
    maps = []
    if not split:
        for b in range(ncores):
            m = dict(shared)
            m["x"] = np.ascontiguousarray(x[b, :ntok, :])
            m["c"] = np.ascontiguousarray(c[b].reshape(16, 128))
            m["flag"] = np.ones((128, 16), np.float32)
            maps.append(m)
        return maps
    hn = ntok // 2
    for cid in range(ncores):
        b, second = cid // 2, cid % 2
        m = dict(shared)
        if second:
            m["x"] = np.ascontiguousarray(x[b, :ntok, :])
        else:
            m["x"] = np.ascontiguousarray(np.concatenate([x[b, hn:ntok, :], x[b, :hn, :]], axis=0))
        m["c"] = np.ascontiguousarray(c[b].reshape(16, 128))
        m["flag"] = np.full((128, 16), float(second), np.float32)
        maps.append(m)
    return maps


_NC_CACHE = {}


def kernel(**inputs):
    key = "full"
    if key not in _NC_CACHE:
        _NC_CACHE[key] = build(NT=SEQ // 128, NL=DEPTH, split=True)
    nc = _NC_CACHE[key]
    ncores = 2 * BATCH
    in_maps = make_in_maps(inputs, ncores, SEQ, split=True)
    res = run_bass_kernel_spmd(nc, in_maps, core_ids=list(range(ncores)))
    out = np.empty((BATCH, SEQ, D), np.float32)
    hn = SEQ // 2
    for cid in range(ncores):
        b, second = cid // 2, cid % 2
        out[b, second * hn:(second + 1) * hn, :] = np.asarray(res.results[cid]["out"], dtype=np.float32)
    return out
```
